# Optimizing a Trainium2 kernel written in Bass

```python
import jax, jax.numpy as jnp
from jax import lax
import numpy as np

D_MODEL = 1024
BATCH = 4
SEQ = 4096
DEPTH = 4

HEAD_DIM = 64
N_FOX = 6
N_DSA = 5
N_SB = 5
N_IDX_HEADS = 8
IDX_DIM = 64
TOPK_MAX = 256
N_MEM = 256
N_CA_HEADS = 4
CA_HEAD_DIM = 128
D_FF = 2816
ROPE_THETA = 10000.0
Q_BLOCK = 128
NORM_EPS = 1e-6
N_BRANCH = 3
HALF_STEP = 0.5
FGATE_BIAS_OFFSET = 3.0

FOX_W = N_FOX * HEAD_DIM
DSA_W = N_DSA * HEAD_DIM
SB_W = N_SB * HEAD_DIM
CA_W = N_CA_HEADS * CA_HEAD_DIM
IN_SPLITS = (3 * FOX_W, N_FOX, 3 * DSA_W, N_IDX_HEADS * IDX_DIM, IDX_DIM, N_IDX_HEADS, 3 * SB_W, N_BRANCH * D_MODEL)
D_IN = 3 * FOX_W + N_FOX + 3 * DSA_W + N_IDX_HEADS * IDX_DIM + IDX_DIM + N_IDX_HEADS + 3 * SB_W + N_BRANCH * D_MODEL

kernel_name = 'hybrid_fox_dsa_stickbreak_macaron_trunk'


def rms_norm(x, g):
    xf = x.astype(jnp.float32)
    y = xf * lax.rsqrt(jnp.mean(xf * xf, axis=-1, keepdims=True) + NORM_EPS) * g.astype(jnp.float32)
    return y.astype(x.dtype)


def apply_rope(x, positions):
    d = x.shape[-1]
    half = d // 2
    inv_freq = jnp.power(ROPE_THETA, -jnp.arange(half, dtype=jnp.float32) * (2.0 / d))
    ang = positions.astype(jnp.float32)[..., None] * inv_freq
    cos = jnp.cos(ang)[:, :, None, :]
    sin = jnp.sin(ang)[:, :, None, :]
    xf = x.astype(jnp.float32)
    x1, x2 = xf[..., :half], xf[..., half:]
    return jnp.concatenate([x1 * cos - x2 * sin, x2 * cos + x1 * sin], axis=-1).astype(x.dtype)


def swiglu(h, w_gu, w_down):
    g, u = jnp.split(h @ w_gu, 2, axis=-1)
    return (jax.nn.silu(g) * u) @ w_down


def sweep_query_blocks(block_fn, seq_len):
    n_blocks = seq_len // Q_BLOCK
    out = lax.map(block_fn, jnp.arange(n_blocks, dtype=jnp.int32) * Q_BLOCK)
    out = jnp.moveaxis(out, 0, 1)
    return out.reshape((out.shape[0], seq_len) + out.shape[3:])


def fox_attention(q, k, v, log_f):
    seq_len, d = q.shape[1], q.shape[-1]
    scale = d ** -0.5
    c = jnp.cumsum(log_f, axis=1).transpose(0, 2, 1)
    key_pos = jnp.arange(seq_len)

    def block(start):
        qb = lax.dynamic_slice_in_dim(q, start, Q_BLOCK, axis=1)
        cb = lax.dynamic_slice_in_dim(c, start, Q_BLOCK, axis=2)
        logits = jnp.einsum('bqhd,bkhd->bhqk', qb, k).astype(jnp.float32) * scale
        logits = logits + cb[..., :, None] - c[:, :, None, :]
        qpos = start + jnp.arange(Q_BLOCK)
        causal = key_pos[None, :] <= qpos[:, None]
        p = jax.nn.softmax(jnp.where(causal, logits, -jnp.inf), axis=-1)
        return jnp.einsum('bhqk,bkhd->bqhd', p.astype(v.dtype), v)

    return sweep_query_blocks(block, seq_len)


def stick_breaking_attention(q, k, v):
    seq_len, d = q.shape[1], q.shape[-1]
    scale = d ** -0.5
    key_pos = jnp.arange(seq_len)

    def block(start):
        qb = lax.dynamic_slice_in_dim(q, start, Q_BLOCK, axis=1)
        z = jnp.einsum('bqhd,bkhd->bhqk', qb, k).astype(jnp.float32) * scale
        qpos = start + jnp.arange(Q_BLOCK)
        strict = key_pos[None, :] < qpos[:, None]
        log_beta = jax.nn.log_sigmoid(z)
        log_1m_beta = jnp.where(strict, jax.nn.log_sigmoid(-z), 0.0)
        after = lax.cumsum(log_1m_beta, axis=3, reverse=True) - log_1m_beta
        a = jnp.where(strict, jnp.exp(log_beta + after), 0.0)
        return jnp.einsum('bhqk,bkhd->bqhd', a.astype(v.dtype), v)

    return sweep_query_blocks(block, seq_len)


def dsa_sparse_attention(q, k, v, q_idx, k_idx, w_idx, topk):
    seq_len, d = q.shape[1], q.shape[-1]
    scale = d ** -0.5
    key_pos = jnp.arange(seq_len)

    def block(start):
        qb = lax.dynamic_slice_in_dim(q, start, Q_BLOCK, axis=1)
        qib = lax.dynamic_slice_in_dim(q_idx, start, Q_BLOCK, axis=1)
        wb = lax.dynamic_slice_in_dim(w_idx, start, Q_BLOCK, axis=1)
        head_scores = jax.nn.relu(jnp.einsum('bqhd,bkd->bqhk', qib, k_idx).astype(jnp.float32))
        score = jnp.einsum('bqhk,bqh->bqk', head_scores, wb)
        qpos = start + jnp.arange(Q_BLOCK)
        causal = key_pos[None, :] <= qpos[:, None]
        score = jnp.where(causal[None], score, -jnp.inf)
        _, idx = lax.top_k(score, topk)
        valid = idx <= qpos[None, :, None]
        gather = jax.vmap(lambda arr, ii: arr[ii])
        k_sel = gather(k, idx)
        v_sel = gather(v, idx)
        logits = jnp.einsum('bqhd,bqkhd->bhqk', qb, k_sel).astype(jnp.float32) * scale
        logits = jnp.where(valid[:, None], logits, -jnp.inf)
        p = jax.nn.softmax(logits, axis=-1)
        return jnp.einsum('bhqk,bqkhd->bqhd', p.astype(v_sel.dtype), v_sel)

    return sweep_query_blocks(block, seq_len)


def hybrid_mixer(h, positions, w_in, b_fgate, w_fox_out, w_dsa_out, w_sb_out, w_out):
    b, s, _ = h.shape
    proj = h @ w_in
    offsets = np.cumsum(np.array(IN_SPLITS))[:-1].tolist()
    fox_qkv, fox_f, dsa_qkv, idx_q, idx_k, idx_w, sb_qkv, gates = jnp.split(proj, offsets, axis=-1)

    fq, fk, fv = [t[:, :, 0] for t in jnp.split(fox_qkv.reshape(b, s, 3, N_FOX, HEAD_DIM), 3, axis=2)]
    log_f = jax.nn.log_sigmoid(fox_f.astype(jnp.float32) + b_fgate.astype(jnp.float32))
    o_fox = fox_attention(fq, fk, fv, log_f).reshape(b, s, FOX_W)

    dq, dk, dv = [t[:, :, 0] for t in jnp.split(dsa_qkv.reshape(b, s, 3, N_DSA, HEAD_DIM), 3, axis=2)]
    dq = apply_rope(dq, positions)
    dk = apply_rope(dk, positions)
    q_idx = apply_rope(idx_q.reshape(b, s, N_IDX_HEADS, IDX_DIM), positions)
    k_idx = apply_rope(idx_k.reshape(b, s, 1, IDX_DIM), positions)[:, :, 0]
    w_idx = idx_w.astype(jnp.float32) * (N_IDX_HEADS ** -0.5) * (IDX_DIM ** -0.5)
    topk = min(TOPK_MAX, s // 4)
    o_dsa = dsa_sparse_attention(dq, dk, dv, q_idx, k_idx, w_idx, topk).reshape(b, s, DSA_W)

    sq, sk, sv = [t[:, :, 0] for t in jnp.split(sb_qkv.reshape(b, s, 3, N_SB, HEAD_DIM), 3, axis=2)]
    o_sb = stick_breaking_attention(sq, sk, sv).reshape(b, s, SB_W)

    g = jax.nn.sigmoid(gates.reshape(b, s, N_BRANCH, D_MODEL))
    merged = g[:, :, 0] * (o_fox @ w_fox_out) + g[:, :, 1] * (o_dsa @ w_dsa_out) + g[:, :, 2] * (o_sb @ w_sb_out)
    return merged @ w_out


def memory_cross_attention(h, mem_n, w_q, w_kv, w_o):
    b, s, _ = h.shape
    q = (h @ w_q).reshape(b, s, N_CA_HEADS, CA_HEAD_DIM)
    k, v = jnp.split((mem_n @ w_kv).reshape(b, mem_n.shape[1], 2, N_CA_HEADS, CA_HEAD_DIM), 2, axis=2)
    k, v = k[:, :, 0], v[:, :, 0]
    logits = jnp.einsum('bshd,bmhd->bhsm', q, k).astype(jnp.float32) * (CA_HEAD_DIM ** -0.5)
    p = jax.nn.softmax(logits, axis=-1)
    o = jnp.einsum('bhsm,bmhd->bshd', p.astype(v.dtype), v).reshape(b, s, CA_W)
    return o @ w_o


def setup_inputs(seed: int = 0) -> dict:
    key = jax.random.key(seed)
    ks = jax.random.split(key, 24)
    f32 = jnp.float32

    def dense(k, shape):
        return jax.random.normal(k, shape, f32) * (shape[-2] ** -0.5)

    def gain(k, shape):
        return 1.0 + 0.02 * jax.random.normal(k, shape, f32)

    return {
        'x': jax.random.normal(ks[0], (BATCH, SEQ, D_MODEL), f32),
        'mem': jax.random.normal(ks[1], (BATCH, N_MEM, D_MODEL), f32),
        'positions': jnp.broadcast_to(jnp.arange(SEQ, dtype=jnp.int32), (BATCH, SEQ)),
        'ffn1_norm': gain(ks[2], (DEPTH, D_MODEL)),
        'ffn1_w_gu': dense(ks[3], (DEPTH, D_MODEL, 2 * D_FF)),
        'ffn1_w_down': dense(ks[4], (DEPTH, D_FF, D_MODEL)),
        'mix_norm': gain(ks[5], (DEPTH, D_MODEL)),
        'w_in': dense(ks[6], (DEPTH, D_MODEL, D_IN)),
        'b_fgate': FGATE_BIAS_OFFSET + 0.1 * jax.random.normal(ks[7], (DEPTH, N_FOX), f32),
        'w_fox_out': dense(ks[8], (DEPTH, FOX_W, D_MODEL)),
        'w_dsa_out': dense(ks[9], (DEPTH, DSA_W, D_MODEL)),
        'w_sb_out': dense(ks[10], (DEPTH, SB_W, D_MODEL)),
        'w_out': dense(ks[11], (DEPTH, D_MODEL, D_MODEL)),
        'ca_norm': gain(ks[12], (DEPTH, D_MODEL)),
        'mem_norm': gain(ks[13], (DEPTH, D_MODEL)),
        'ca_w_q': dense(ks[14], (DEPTH, D_MODEL, CA_W)),
        'ca_w_kv': dense(ks[15], (DEPTH, D_MODEL, 2 * CA_W)),
        'ca_w_o': dense(ks[16], (DEPTH, CA_W, D_MODEL)),
        'ffn2_norm': gain(ks[17], (DEPTH, D_MODEL)),
        'ffn2_w_gu': dense(ks[18], (DEPTH, D_MODEL, 2 * D_FF)),
        'ffn2_w_down': dense(ks[19], (DEPTH, D_FF, D_MODEL)),
        'final_norm': gain(ks[20], (D_MODEL,)),
    }


def reference(x, mem, positions, ffn1_norm, ffn1_w_gu, ffn1_w_down, mix_norm, w_in, b_fgate, w_fox_out, w_dsa_out, w_sb_out, w_out, ca_norm, mem_norm, ca_w_q, ca_w_kv, ca_w_o, ffn2_norm, ffn2_w_gu, ffn2_w_down, final_norm):
    for l in range(DEPTH):
        x = x + HALF_STEP * swiglu(rms_norm(x, ffn1_norm[l]), ffn1_w_gu[l], ffn1_w_down[l])
        x = x + hybrid_mixer(rms_norm(x, mix_norm[l]), positions, w_in[l], b_fgate[l], w_fox_out[l], w_dsa_out[l], w_sb_out[l], w_out[l])
        x = x + memory_cross_attention(rms_norm(x, ca_norm[l]), rms_norm(mem, mem_norm[l]), ca_w_q[l], ca_w_kv[l], ca_w_o[l])
        x = x + HALF_STEP * swiglu(rms_norm(x, ffn2_norm[l]), ffn2_w_gu[l], ffn2_w_down[l])
    return rms_norm(x, final_norm)
```

```python
import contextlib
import math
import numpy as np
import concourse.bass as bass
import concourse.mybir as mybir
from concourse.bass_utils import run_bass_kernel_spmd

F32 = mybir.dt.float32
BF16 = mybir.dt.bfloat16
I32 = mybir.dt.int32
ALU = mybir.AluOpType
AF = mybir.ActivationFunctionType

S = 4096
D = 1024
NT = 32
NQ = 8
DFF = 2816
NJ = 22
DIN = 6734
NEG = -1.0e30
SB_H = 5
SB_NQ = 8
SB_DBG = 5
SB_V = 'bc'
C_FQ, C_FK, C_FV, C_FF = 0, 384, 768, 1152
C_DQ, C_DK, C_DV = 1158, 1478, 1798
C_IQ, C_IK, C_IW = 2118, 2630, 2694
C_SQ, C_SK, C_SV = 2702, 3022, 3342
C_G = 3662
R_FQ, R_FK, R_DQ, R_DK, R_IQ, R_IK, R_SQ, R_SK = 0, 384, 768, 1088, 1408, 1920, 1984, 2304
PJ_ROWS = 2624
V_F, V_D, V_S = 0, 390, 715
V_COLS = 1040
O_F, O_D, O_S = 0, 384, 768
O_ROWS = 1152


class Sem:
    __slots__ = ("h", "count", "is_dma")

    def __init__(self, h, is_dma):
        self.h = h
        self.count = 0
        self.is_dma = is_dma


class Buf:
    __slots__ = ("name", "w", "r", "dsem")

    def __init__(self, name):
        self.name = name
        self.w = None
        self.r = {}
        self.dsem = None


class MK:
    SAME_ENGINE_SYNC = True

    def __init__(self, nc, es):
        self.nc = nc
        self.es = es
        self.eng = {}
        self.esem = {}
        self.waited = {}
        self.dsems = []
        self.phase_sems = []
        self.free_sems = []
        self.uid = 0
        for name in ("tensor", "vector", "scalar", "gpsimd", "sync"):
            self.eng[name] = getattr(nc, name)
            self.esem[name] = Sem(es.enter_context(nc.semaphore("s_" + name)), False)
            self.waited[name] = {}

    def buf(self, name="b"):
        return Buf(name)

    def _dsem(self, b):
        if b.dsem is None:
            if self.free_sems:
                b.dsem = self.free_sems.pop()
            else:
                self.uid += 1
                b.dsem = Sem(self.es.enter_context(self.nc.semaphore("d%d" % self.uid)), True)
                self.dsems.append(b.dsem)
            self.phase_sems.append(b.dsem)
        return b.dsem

    def keep(self):
        self.phase_sems = []

    def _waits(self, en, reads, writes, skip_same):
        need = {}
        for b in reads:
            if b.w is not None:
                s, v = b.w
                if need.get(s, 0) < v:
                    need[s] = v
        for b in writes:
            if b.w is not None:
                s, v = b.w
                if need.get(s, 0) < v:
                    need[s] = v
            for s, v in b.r.items():
                if need.get(s, 0) < v:
                    need[s] = v
        wd = self.waited[en]
        own = self.esem[en]
        for s, v in need.items():
            if s.is_dma:
                v = s.count
            if s is own and (skip_same or not self.SAME_ENGINE_SYNC):
                continue
            if wd.get(s, 0) >= v:
                continue
            self.eng[en].wait_ge(s.h, v)
            wd[s] = v

    def op(self, en, fn, reads=(), writes=(), skip_same=False):
        self._waits(en, reads, writes, skip_same)
        inst = fn(self.eng[en])
        s = self.esem[en]
        s.count += 1
        inst.then_inc(s.h, 1)
        for b in reads:
            b.r[s] = s.count
        ev = (s, s.count)
        for b in writes:
            b.w = ev
            b.r = {}
        return inst

    def dma(self, q, out, in_, reads=(), writes=(), semb=None):
        self._waits(q, reads, writes, True)
        if semb is None:
            semb = (list(writes) + list(reads))[0]
        s = self._dsem(semb)
        inst = self.eng[q].dma_start(out=out, in_=in_)
        s.count += 16
        inst.then_inc(s.h, 16)
        for b in reads:
            b.r[s] = s.count
        ev = (s, s.count)
        for b in writes:
            b.w = ev
            b.r = {}
        return inst

    def barrier(self, recycle=False):
        if recycle:
            self.free_sems.extend(self.phase_sems)
            self.phase_sems = []
        for en in self.eng:
            wd = self.waited[en]
            for on, s in self.esem.items():
                if on == en or s.count == 0:
                    continue
                if wd.get(s, 0) < s.count:
                    self.eng[en].wait_ge(s.h, s.count)
                    wd[s] = s.count
            for s in self.dsems:
                if s.count and wd.get(s, 0) < s.count:
                    self.eng[en].wait_ge(s.h, s.count)
                    wd[s] = s.count


class Rot:
    def __init__(self, items):
        self.items = items
        self.i = 0

    def next(self):
        it = self.items[self.i % len(self.items)]
        self.i += 1
        return it


def host_consts():
    c = {}
    p = np.arange(128)[:, None]
    f = np.arange(128)[None, :]
    c["ident"] = (p == f).astype(np.float32)
    rot = np.zeros((128, 128), np.float32)
    for m in range(128):
        if (m % 64) < 32:
            rot[m + 32, m] = -1.0
        else:
            rot[m - 32, m] = 1.0
    c["rot"] = rot
    c["trile"] = (p <= f).astype(np.float32)
    c["trige"] = (p >= f).astype(np.float32)
    ff = np.arange(512)[None, None, :]
    dd = np.arange(4)[None, :, None]
    pp = np.arange(128)[:, None, None]
    c["cmask"] = ((128 * dd + pp) <= ff).astype(np.float32).reshape(128, 2048)
    c["smask"] = ((128 * dd + pp) < ff).astype(np.float32).reshape(128, 2048)
    c["negm"] = np.where(f <= p, 0.0, NEG).astype(np.float32)
    c["invf"] = (10000.0 ** (-(np.arange(128) % 32).astype(np.float64) / 32.0)).astype(np.float32).reshape(128, 1)
    return c


CONST_SHAPES = {"ident": 128, "rot": 128, "trile": 128, "trige": 128, "cmask": 2048, "smask": 2048,
                "negm": 128, "invf": 1}

WSPEC = [("ffn1_norm", [4, 1024]), ("ffn1_w_gu", [4, 1024, 5632]), ("ffn1_w_down", [4, 2816, 1024]),
         ("mix_norm", [4, 1024]), ("w_in", [4, 1024, 6734]), ("b_fgate", [4, 6]),
         ("w_fox_out", [4, 384, 1024]), ("w_dsa_out", [4, 320, 1024]), ("w_sb_out", [4, 320, 1024]),
         ("w_out", [4, 1024, 1024]), ("ca_norm", [4, 1024]), ("mem_norm", [4, 1024]),
         ("ca_w_q", [4, 1024, 512]), ("ca_w_kv", [4, 1024, 1024]), ("ca_w_o", [4, 512, 1024]),
         ("ffn2_norm", [4, 1024]), ("ffn2_w_gu", [4, 1024, 5632]), ("ffn2_w_down", [4, 2816, 1024]),
         ("final_norm", [1, 1024])]


def build(n_layers=4, stages=("ffn1", "proj", "fox", "dsa", "sb", "merge", "ca", "ffn2"), debug=False, attn_probe=False):
    nc = bass.Bass("TRN2", target_bir_lowering=False)
    x_in = nc.dram_tensor("x", [S, D], F32, kind="ExternalInput").ap()
    mem_in = nc.dram_tensor("mem", [256, D], F32, kind="ExternalInput").ap()
    pos_in = nc.dram_tensor("pos", [1, S], I32, kind="ExternalInput").ap()
    W = {}
    for name, shp in ([] if attn_probe else WSPEC):
        shp = [min(shp[0], n_layers)] + list(shp[1:])
        W[name] = nc.dram_tensor(name, shp, F32, kind="ExternalInput").ap()
    CD = {}
    for name, n in CONST_SHAPES.items():
        CD[name] = nc.dram_tensor("c_" + name, [128, n], F32, kind="ExternalInput").ap()
    out_d = nc.dram_tensor("out", [S, D], F32, kind="ExternalOutput").ap()
    dbgk = "ExternalOutput" if debug else "Internal"
    X = nc.dram_tensor("Xs", [S, D], F32, kind="Internal").ap()
    AT = nc.dram_tensor("ATs", [DFF, S], BF16, kind="Internal").ap()
    PJT = nc.dram_tensor("PJTs", [PJ_ROWS, S], BF16, kind=("ExternalInput" if attn_probe else dbgk)).ap()
    VT = nc.dram_tensor("VTs", [S, V_COLS], BF16, kind=("ExternalInput" if attn_probe else "Internal")).ap()
    OT = nc.dram_tensor("OTs", [O_ROWS, S], BF16, kind=dbgk).ap()

    with contextlib.ExitStack() as es:
        mk = MK(nc, es)

        uid = [0]

        def sb(stack, name, shape, dt):
            uid[0] += 1
            t = stack.enter_context(nc.sbuf_tensor("%s_%d" % (name, uid[0]), shape, dt))
            return t, Buf(name)

        Xb = [Buf("X%d" % t) for t in range(NT)]
        ATb = Buf("AT")
        PJb = Buf("PJT")
        VTb = Buf("VT")
        OTb = Buf("OT")
        xstate = {"src": x_in}

        PS = []
        for i in range(7):
            PS.append((es.enter_context(nc.psum_tensor("ps%d" % i, [128, 512], F32)), Buf("ps%d" % i)))
        psT, psTb = es.enter_context(nc.psum_tensor("psT", [128, 1024], BF16)), Buf("psT")

        def cload(name, dt):
            n = CONST_SHAPES[name]
            t, b = sb(es, "k_" + name, [128, n], dt)
            mk.dma("gpsimd", t[:], CD[name][:, :], writes=[b])
            return t, b

        ident, identb = cload("ident", BF16)
        rotm, rotb = cload("rot", BF16)
        trile, trileb = cload("trile", F32)
        trige, trigeb = cload("trige", BF16)
        cmask, cmaskb = cload("cmask", BF16)
        smask, smaskb = cload("smask", BF16)
        negm, negmb = cload("negm", F32)
        invf, invfb = cload("invf", F32)
        onesb, onesbb = sb(es, "onesb", [128, 128], BF16)
        onesf, onesfb = sb(es, "onesf", [128, 128], F32)
        mk.op("vector", lambda e: e.memset(onesb[:], 1.0), writes=[onesbb])
        mk.op("vector", lambda e: e.memset(onesf[:], 1.0), writes=[onesfb])
        Cc, Ccb = sb(es, "Cc", [128, NT, 6], F32)
        Ee, Eeb = sb(es, "Ee", [128, NT, 6], F32)
        widx, widxb = sb(es, "widx", [128, NT, 8], F32)
        gB, gBb = sb(es, "gB", [128, D], F32)
        small, smallb = sb(es, "small", [128, 8], F32)
        mk.keep()

        def rstd_of(xt, xtb, junk, junkb):
            mk.op("scalar", lambda e: e.activation(out=junk[:], in_=xt[:], func=AF.Square, accum_out=small[:, 0:1]),
                  reads=[xtb], writes=[junkb, smallb])
            mk.op("vector", lambda e: e.tensor_scalar(out=small[:, 1:2], in0=small[:, 0:1], scalar1=1.0 / D, scalar2=1e-6,
                                                      op0=ALU.mult, op1=ALU.add), reads=[smallb], writes=[smallb])
            mk.op("scalar", lambda e: e.activation(out=small[:, 1:2], in_=small[:, 1:2], func=AF.Sqrt),
                  reads=[smallb], writes=[smallb])
            mk.op("vector", lambda e: e.reciprocal(out=small[:, 2:3], in_=small[:, 1:2]), reads=[smallb], writes=[smallb])

        def load_gain(gain_ap):
            mk.dma("sync", gB[:], gain_ap.to_broadcast([128, D]), writes=[gBb])

        def norm_pass(st_unused, gain_ap, hT, hTb, t0=0, nt=NT):
            load_gain(gain_ap)
            with contextlib.ExitStack() as st:
                xr = Rot([sb(st, "nx%d" % i, [128, D], F32) for i in range(3)])
                hn = Rot([sb(st, "nh%d" % i, [128, D], BF16) for i in range(2)])
                junk, junkb = sb(st, "njunk", [128, D], BF16)
                src = xstate["src"]
                for t in range(t0, t0 + nt):
                    xt, xtb = xr.next()
                    mk.dma("sync", xt[:], src[t * 128:(t + 1) * 128, :], reads=[Xb[t]], writes=[xtb])
                    rstd_of(xt, xtb, junk, junkb)
                    h, hb = hn.next()
                    mk.op("vector", lambda e: e.scalar_tensor_tensor(out=h[:], in0=xt[:], scalar=small[:, 2:3], in1=gB[:],
                                                                     op0=ALU.mult, op1=ALU.mult),
                          reads=[xtb, smallb, gBb], writes=[hb])
                    for kc in range(8):
                        mk.op("tensor", lambda e: e.transpose(psT[:, kc * 128:(kc + 1) * 128], h[:, kc * 128:(kc + 1) * 128], ident[:]),
                              reads=[hb, identb], writes=[psTb], skip_same=True)
                    mk.op("scalar", lambda e: e.activation(out=hT[:, :, (t - t0) * 128:(t - t0 + 1) * 128],
                                                           in_=psT[:, :].rearrange("p (k t) -> p k t", k=8), func=AF.Identity),
                          reads=[psTb], writes=[hTb])
                mk.barrier()

        def wchunk(w2d, c0, n, dst, dstb, kcs=8, q="gpsimd"):
            mk.dma(q, dst, w2d[:, c0:c0 + n].rearrange("(k p) n -> p k n", p=128), writes=[dstb])

        psrot = Rot(PS[0:4])

        def ffn(l, which):
            gain = W["ffn%d_norm" % which][l:l + 1, :]
            wgu = W["ffn%d_w_gu" % which][l]
            wdn = W["ffn%d_w_down" % which][l]
            with contextlib.ExitStack() as st:
                hT, hTb = sb(st, "hT", [128, 8, S], BF16)
                norm_pass(st, gain, hT, hTb)
                wr = Rot([sb(st, "wgu%d" % i, [128, 2, 8, 128], BF16) for i in range(3)])
                ar = Rot([sb(st, "aTj%d" % i, [128, S], BF16) for i in range(2)])
                sr = Rot([sb(st, "sg%d" % i, [128, 512], F32) for i in range(2)])
                wl = []

                def loadw(j):
                    w, wb = wr.next()
                    wchunk(wgu, j * 128, 128, w[:, 0], wb)
                    mk.dma("gpsimd", w[:, 1], wgu[:, DFF + j * 128:DFF + (j + 1) * 128].rearrange("(k p) n -> p k n", p=128),
                           writes=[wb])
                    wl.append((w, wb))
                loadw(0)
                loadw(1)
                for j in range(NJ):
                    if j + 2 < NJ:
                        loadw(j + 2)
                    w, wb = wl[j]
                    a, ab = ar.next()
                    for Q in range(NQ):
                        pg, pgb = psrot.next()
                        pu, pub = psrot.next()
                        for kc in range(8):
                            mk.op("tensor", lambda e: e.matmul(pg[:, :], lhsT=w[:, 0, kc, :], rhs=hT[:, kc, Q * 512:(Q + 1) * 512],
                                                               start=(kc == 0), stop=(kc == 7)),
                                  reads=[wb, hTb], writes=[pgb], skip_same=True)
                        for kc in range(8):
                            mk.op("tensor", lambda e: e.matmul(pu[:, :], lhsT=w[:, 1, kc, :], rhs=hT[:, kc, Q * 512:(Q + 1) * 512],
                                                               start=(kc == 0), stop=(kc == 7)),
                                  reads=[wb, hTb], writes=[pub], skip_same=True)
                        sg, sgb = sr.next()
                        mk.op("scalar", lambda e: e.activation(out=sg[:], in_=pg[:, :], func=AF.Silu), reads=[pgb], writes=[sgb])
                        mk.op("vector", lambda e: e.tensor_tensor(out=a[:, Q * 512:(Q + 1) * 512], in0=sg[:], in1=pu[:, :], op=ALU.mult),
                              reads=[sgb, pub], writes=[ab])
                    mk.dma("sync", AT[j * 128:(j + 1) * 128, :], a[:], reads=[ab], writes=[ATb], semb=ab)
                mk.barrier(recycle=True)
            with contextlib.ExitStack() as st:
                wd, wdb = sb(st, "wd", [128, NJ, D], BF16)
                for j in range(NJ):
                    mk.dma("gpsimd", wd[:, j, :], wdn[j * 128:(j + 1) * 128, :], writes=[wdb])
                aq = Rot([sb(st, "aq%d" % i, [128, NJ, 512], BF16) for i in range(2)])
                xr = Rot([sb(st, "dx%d" % i, [128, D], F32) for i in range(3)])
                src = xstate["src"]
                for Q in range(NQ):
                    a, ab = aq.next()
                    mk.dma("sync", a[:], AT[:, Q * 512:(Q + 1) * 512].rearrange("(j p) t -> p j t", p=128), reads=[ATb], writes=[ab])
                    for sub in range(4):
                        t = Q * 4 + sub
                        xt, xtb = xr.next()
                        mk.dma("sync", xt[:], src[t * 128:(t + 1) * 128, :], reads=[Xb[t]], writes=[xtb])
                        for half in range(2):
                            ps, psb = psrot.next()
                            for j in range(NJ):
                                mk.op("tensor", lambda e: e.matmul(ps[:, :], lhsT=a[:, j, sub * 128:(sub + 1) * 128],
                                                                   rhs=wd[:, j, half * 512:(half + 1) * 512],
                                                                   start=(j == 0), stop=(j == NJ - 1)),
                                      reads=[ab, wdb], writes=[psb], skip_same=True)
                            mk.op("vector", lambda e: e.scalar_tensor_tensor(out=xt[:, half * 512:(half + 1) * 512], in0=ps[:, :], scalar=0.5,
                                                                             in1=xt[:, half * 512:(half + 1) * 512],
                                                                             op0=ALU.mult, op1=ALU.add),
                                  reads=[psb, xtb], writes=[xtb])
                        mk.dma("sync", X[t * 128:(t + 1) * 128, :], xt[:], reads=[xtb], writes=[Xb[t]], semb=xtb)
                xstate["src"] = X
                mk.barrier(recycle=True)

        def proj(l):
            win = W["w_in"][l]
            with contextlib.ExitStack() as st:
                hT, hTb = sb(st, "hT", [128, 8, S], BF16)
                norm_pass(st, W["mix_norm"][l:l + 1, :], hT, hTb)
                cosT, cosb = sb(st, "cosT", [128, S], F32)
                sinT, sinb = sb(st, "sinT", [128, S], F32)
                st_r = contextlib.ExitStack()
                ang, angb = sb(st_r, "ang", [128, S], F32)
                tmpf, tmpfb = sb(st_r, "tmpf", [128, S], F32)
                posi, posib = sb(st_r, "posi", [128, S], I32)
                mk.dma("sync", posi[:], pos_in.to_broadcast([128, S]), writes=[posib])
                mk.op("vector", lambda e: e.tensor_copy(out=ang[:], in_=posi[:]), reads=[posib], writes=[angb])
                mk.op("vector", lambda e: e.tensor_scalar(out=ang[:], in0=ang[:], scalar1=invf[:, 0:1], scalar2=None, op0=ALU.mult),
                      reads=[angb, invfb], writes=[angb])
                TWO_PI = 2.0 * math.pi
                C1 = 6.28125
                C2 = TWO_PI - C1
                for (dst, dstb, shift) in ((sinT, sinb, 0.0), (cosT, cosb, math.pi / 2)):
                    mk.op("vector", lambda e: e.tensor_scalar(out=tmpf[:], in0=ang[:], scalar1=shift, scalar2=1.0 / TWO_PI,
                                                              op0=ALU.add, op1=ALU.mult), reads=[angb], writes=[tmpfb])
                    mk.op("vector", lambda e: e.tensor_copy(out=posi[:], in_=tmpf[:]), reads=[tmpfb], writes=[posib])
                    mk.op("vector", lambda e: e.tensor_copy(out=tmpf[:], in_=posi[:]), reads=[posib], writes=[tmpfb])
                    mk.op("vector", lambda e: e.scalar_tensor_tensor(out=dst[:], in0=tmpf[:], scalar=-C1, in1=ang[:],
                                                                     op0=ALU.mult, op1=ALU.add), reads=[tmpfb, angb], writes=[dstb])
                    mk.op("vector", lambda e: e.scalar_tensor_tensor(out=dst[:], in0=tmpf[:], scalar=-C2, in1=dst[:],
                                                                     op0=ALU.mult, op1=ALU.add), reads=[tmpfb, dstb], writes=[dstb])
                    mk.op("vector", lambda e: e.tensor_scalar(out=dst[:], in0=dst[:], scalar1=shift, scalar2=math.pi,
                                                              op0=ALU.add, op1=ALU.min), reads=[dstb], writes=[dstb])
                    mk.op("vector", lambda e: e.tensor_scalar(out=dst[:], in0=dst[:], scalar1=-math.pi, scalar2=None,
                                                              op0=ALU.max), reads=[dstb], writes=[dstb])
                    mk.op("scalar", lambda e: e.activation(out=dst[:], in_=dst[:], func=AF.Sin), reads=[dstb], writes=[dstb])
                mk.barrier()
                st_r.close()
                chunks = []
                def add_group(c0, r0, width, rope):
                    o = 0
                    while o < width:
                        m = min(128, width - o)
                        chunks.append((c0 + o, r0 + o, m, rope))
                        o += m
                add_group(C_FQ, R_FQ, 384, False)
                add_group(C_FK, R_FK, 384, False)
                add_group(C_DQ, R_DQ, 320, True)
                add_group(C_DK, R_DK, 320, True)
                add_group(C_IQ, R_IQ, 512, True)
                add_group(C_IK, R_IK, 64, True)
                add_group(C_SQ, R_SQ, 320, False)
                add_group(C_SK, R_SK, 320, False)
                wr = Rot([sb(st, "pw%d" % i, [128, 8, 128], BF16) for i in range(3)])
                sr = Rot([sb(st, "pst%d" % i, [128, S], BF16) for i in range(2)])
                xbr = Rot([sb(st, "pxb%d" % i, [128, 512], BF16) for i in range(2)])
                t1r = Rot([sb(st, "pt1%d" % i, [128, 512], F32) for i in range(2)])
                t2r = Rot([sb(st, "pt2%d" % i, [128, 512], F32) for i in range(2)])
                wl = []

                def loadw(i):
                    c0, r0, m, rope = chunks[i]
                    w, wb = wr.next()
                    wchunk(win, c0, m, w[:, :, 0:m], wb)
                    wl.append((w, wb))
                loadw(0)
                loadw(1)
                for i, (c0, r0, m, rope) in enumerate(chunks):
                    if i + 2 < len(chunks):
                        loadw(i + 2)
                    w, wb = wl[i]
                    stg, stgb = sr.next()
                    for Q in range(NQ):
                        qs = slice(Q * 512, (Q + 1) * 512)
                        ps, psb = psrot.next()
                        for kc in range(8):
                            mk.op("tensor", lambda e: e.matmul(ps[0:m, :], lhsT=w[:, kc, 0:m], rhs=hT[:, kc, qs],
                                                               start=(kc == 0), stop=(kc == 7)),
                                  reads=[wb, hTb], writes=[psb], skip_same=True)
                        if not rope:
                            mk.op("scalar", lambda e: e.activation(out=stg[0:m, qs], in_=ps[0:m, :], func=AF.Identity),
                                  reads=[psb], writes=[stgb])
                        else:
                            xb_, xbb = xbr.next()
                            mk.op("scalar", lambda e: e.activation(out=xb_[0:m, :], in_=ps[0:m, :], func=AF.Identity),
                                  reads=[psb], writes=[xbb])
                            pr, prb = psrot.next()
                            mk.op("tensor", lambda e: e.matmul(pr[0:m, :], lhsT=rotm[0:m, 0:m], rhs=xb_[0:m, :], start=True, stop=True),
                                  reads=[rotb, xbb], writes=[prb], skip_same=True)
                            t1, t1b = t1r.next()
                            t2, t2b = t2r.next()
                            mk.op("gpsimd", lambda e: e.tensor_tensor(out=t1[0:m, :], in0=xb_[0:m, :], in1=cosT[0:m, qs], op=ALU.mult),
                                  reads=[xbb, cosb], writes=[t1b])
                            mk.op("vector", lambda e: e.tensor_tensor(out=t2[0:m, :], in0=pr[0:m, :], in1=sinT[0:m, qs], op=ALU.mult),
                                  reads=[prb, sinb], writes=[t2b])
                            mk.op("gpsimd", lambda e: e.tensor_tensor(out=stg[0:m, qs], in0=t1[0:m, :], in1=t2[0:m, :], op=ALU.add),
                                  reads=[t1b, t2b], writes=[stgb])
                    mk.dma("sync", PJT[r0:r0 + m, :], stg[0:m, :], reads=[stgb], writes=[PJb], semb=stgb)
                wv, wvb = sb(st, "wv", [128, 8, 1024], BF16)
                wchunk(win, C_FV, 384, wv[:, :, 0:384], wvb)
                wchunk(win, C_DV, 320, wv[:, :, 384:704], wvb)
                wchunk(win, C_SV, 320, wv[:, :, 704:1024], wvb)
                wf, wfb = sb(st, "wf", [128, 8, 16], BF16)
                mk.op("vector", lambda e: e.memset(wf[:], 0.0), writes=[wfb])
                wchunk(win, C_FF, 6, wf[:, :, 0:6], wfb)
                wchunk(win, C_IW, 8, wf[:, :, 8:16], wfb)
                vst = Rot([sb(st, "vst%d" % i, [128, 16, 65], BF16) for i in range(2)])
                fw, fwb = sb(st, "fw", [128, NT, 16], F32)
                for t in range(NT):
                    ts_ = slice(t * 128, (t + 1) * 128)
                    v, vb = vst.next()
                    mk.op("gpsimd", lambda e: e.memset(v[:], 1.0), writes=[vb])
                    for half in range(2):
                        ps, psb = psrot.next()
                        for kc in range(8):
                            mk.op("tensor", lambda e: e.matmul(ps[:, :], lhsT=hT[:, kc, ts_], rhs=wv[:, kc, half * 512:(half + 1) * 512],
                                                               start=(kc == 0), stop=(kc == 7)),
                                  reads=[hTb, wvb], writes=[psb], skip_same=True)
                        mk.op("scalar", lambda e: e.activation(out=v[:, half * 8:(half + 1) * 8, 0:64],
                                                               in_=ps[:, :].rearrange("p (h d) -> p h d", h=8), func=AF.Identity),
                              reads=[psb], writes=[vb])
                    mk.dma("sync", VT[ts_, :], v[:].rearrange("p h d -> p (h d)"), reads=[vb], writes=[VTb], semb=vb)
                    ps, psb = psrot.next()
                    for kc in range(8):
                        mk.op("tensor", lambda e: e.matmul(ps[:, 0:16], lhsT=hT[:, kc, ts_], rhs=wf[:, kc, :],
                                                           start=(kc == 0), stop=(kc == 7)),
                              reads=[hTb, wfb], writes=[psb], skip_same=True)
                    mk.op("vector", lambda e: e.tensor_copy(out=fw[:, t, :], in_=ps[:, 0:16]), reads=[psb], writes=[fwb])
                bt, btb = sb(st, "bt", [128, 6], F32)
                mk.dma("sync", bt[:], W["b_fgate"][l:l + 1, :].to_broadcast([128, 6]), writes=[btb])
                lf, lfb = sb(st, "lf", [128, NT, 6], F32)
                for h in range(6):
                    mk.op("vector", lambda e: e.tensor_scalar(out=lf[:, :, h], in0=fw[:, :, h], scalar1=bt[:, h:h + 1], scalar2=None,
                                                              op0=ALU.add), reads=[fwb, btb], writes=[lfb])
                mk.op("scalar", lambda e: e.activation(out=lf[:], in_=lf[:], func=AF.Exp, scale=-1.0), reads=[lfb], writes=[lfb])
                mk.op("scalar", lambda e: e.activation(out=lf[:], in_=lf[:], func=AF.Ln, bias=1.0), reads=[lfb], writes=[lfb])
                p1, p1b = psrot.next()
                p2, p2b = psrot.next()
                lf2 = lf[:].rearrange("p t h -> p (t h)")
                mk.op("tensor", lambda e: e.matmul(p1[:, 0:192], lhsT=trile[:], rhs=lf2, start=True, stop=True),
                      reads=[trileb, lfb], writes=[p1b], skip_same=True)
                mk.op("tensor", lambda e: e.matmul(p2[:, 0:192], lhsT=onesf[:], rhs=lf2, start=True, stop=True),
                      reads=[onesfb, lfb], writes=[p2b], skip_same=True)
                tot, totb = sb(st, "tot", [128, NT, 6], F32)
                mk.op("vector", lambda e: e.tensor_copy(out=tot[:].rearrange("p t h -> p (t h)"), in_=p2[:, 0:192]),
                      reads=[p2b], writes=[totb])
                mk.op("vector", lambda e: e.tensor_copy(out=Ee[:, 0, :], in_=tot[:, 0, :]), reads=[totb], writes=[Eeb])
                for t in range(1, NT):
                    mk.op("vector", lambda e: e.tensor_tensor(out=Ee[:, t, :], in0=Ee[:, t - 1, :], in1=tot[:, t, :], op=ALU.add),
                          reads=[Eeb, totb], writes=[Eeb])
                mk.op("vector", lambda e: e.tensor_tensor(out=Cc[:].rearrange("p t h -> p (t h)"), in0=p1[:, 0:192],
                                                          in1=Ee[:].rearrange("p t h -> p (t h)"), op=ALU.add),
                      reads=[p1b, Eeb], writes=[Ccb])
                mk.op("vector", lambda e: e.tensor_tensor(out=Cc[:], in0=Cc[:], in1=tot[:], op=ALU.subtract),
                      reads=[Ccb, totb], writes=[Ccb])
                mk.op("vector", lambda e: e.tensor_scalar(out=widx[:], in0=fw[:, :, 8:16], scalar1=(8.0 ** -0.5) * (64.0 ** -0.5),
                                                          scalar2=None, op0=ALU.mult), reads=[fwb], writes=[widxb])
                mk.barrier(recycle=True)

        def load_kv(st, r_k, v_off, nh):
            kT, kTb = sb(st, "kT", [128, 3, S], BF16)
            nfull = (nh * 64) // 128
            if nfull:
                mk.dma("sync", kT[:, 0:nfull, :], PJT[r_k:r_k + nfull * 128, :].rearrange("(c p) t -> p c t", p=128),
                       reads=[PJb], writes=[kTb])
            if nh % 2:
                mk.dma("sync", kT[0:64, nfull, :], PJT[r_k + nfull * 128:r_k + nfull * 128 + 64, :], reads=[PJb], writes=[kTb])
            vt, vtb = sb(st, "vt", [128, NT, nh, 65], BF16)
            for g in range(4):
                mk.dma("sync", vt[:, g * 8:(g + 1) * 8].rearrange("p t h d -> p t (h d)"),
                       VT[g * 1024:(g + 1) * 1024, v_off:v_off + nh * 65].rearrange("(t p) c -> p t c", p=128),
                       reads=[VTb], writes=[vtb])
            return kT, kTb, vt, vtb

        def load_q(st, name, r_q, nh, Q=None):
            n = S if Q is None else 512
            cs = slice(0, S) if Q is None else slice(Q * 512, (Q + 1) * 512)
            return n, cs

        def attn_softmax_head(st_tiles, qT, qTb, qoff, kT, kTb, vt, vtb, h, Q, bias_fn, mask_fn, ostg, ostgb):
            ptr, rcs, nbs = st_tiles
            ck, pb = h // 2, (h % 2) * 64
            nkb = 4 * Q + 4
            pO, pOb = PS[4 + (Q % 2)]
            pss = [None] * nkb

            def issue_s(kb):
                ps, psb = psrot.next()
                mk.op("tensor", lambda e: e.matmul(ps[:, :], lhsT=kT[pb:pb + 64, ck, kb * 128:(kb + 1) * 128],
                                                   rhs=qT[pb:pb + 64, ck, qoff:qoff + 512], start=True, stop=True),
                      reads=[kTb, qTb], writes=[psb], skip_same=True)
                pss[kb] = (ps, psb)
            issue_s(0)
            for kb in range(nkb):
                if kb + 1 < nkb:
                    issue_s(kb + 1)
                ps, psb = pss[kb]
                pt, ptb = ptr.next()
                bias_ap, bias_bufs = bias_fn(kb)
                if bias_ap is None:
                    mk.op("scalar", lambda e: e.activation(out=pt[:], in_=ps[:, :], func=AF.Exp, scale=0.125), reads=[psb], writes=[ptb])
                else:
                    mk.op("scalar", lambda e: e.activation(out=pt[:], in_=ps[:, :], func=AF.Exp, scale=0.125, bias=bias_ap),
                          reads=[psb] + bias_bufs, writes=[ptb])
                m = mask_fn(kb)
                if m is not None:
                    m_ap, m_bufs = m
                    mk.op("gpsimd", lambda e: e.tensor_tensor(out=pt[:], in0=pt[:], in1=m_ap, op=ALU.mult),
                          reads=[ptb] + m_bufs, writes=[ptb])
                mk.op("tensor", lambda e: e.matmul(pO[0:65, :], lhsT=vt[:, kb, h, :], rhs=pt[:], start=(kb == 0), stop=(kb == nkb - 1)),
                      reads=[vtb, ptb], writes=[pOb], skip_same=True)
            rc, rcb = rcs.next()
            nb, nbb = nbs.next()
            mk.op("vector", lambda e: e.reciprocal(out=rc[64:65, :], in_=pO[64:65, :]), reads=[pOb], writes=[rcb])
            pB, pBb = PS[6]
            mk.op("tensor", lambda e: e.matmul(pB[0:64, :], lhsT=onesf[64:65, 0:64], rhs=rc[64:65, :], start=True, stop=True),
                  reads=[onesfb, rcb], writes=[pBb], skip_same=True)
            mk.op("scalar", lambda e: e.activation(out=nb[0:64, :], in_=pB[0:64, :], func=AF.Identity), reads=[pBb], writes=[nbb])
            mk.op("vector", lambda e: e.tensor_tensor(out=ostg[0:64, Q * 512:(Q + 1) * 512], in0=pO[0:64, :], in1=nb[0:64, :], op=ALU.mult),
                  reads=[pOb, nbb], writes=[ostgb])

        def attn_tiles(st):
            ptr = Rot([sb(st, "pt%d" % i, [128, 512], BF16) for i in range(4)])
            rcs = Rot([sb(st, "rc%d" % i, [128, 512], F32) for i in range(2)])
            nbs = Rot([sb(st, "nb%d" % i, [64, 512], F32) for i in range(2)])
            return ptr, rcs, nbs

        def fox(l):
            with contextlib.ExitStack() as st:
                kT, kTb, vt, vtb = load_kv(st, R_FK, V_F, 6)
                qT, qTb = sb(st, "qT", [128, 3, S], BF16)
                mk.dma("sync", qT[:], PJT[R_FQ:R_FQ + 384, :].rearrange("(c p) t -> p c t", p=128), reads=[PJb], writes=[qTb])
                tiles = attn_tiles(st)
                osr = Rot([sb(st, "ostg%d" % i, [64, S], BF16) for i in range(2)])
                bqr = Rot([sb(st, "bq%d" % i, [128, NT], F32) for i in range(2)])
                for h in range(6):
                    ostg, ostgb = osr.next()
                    for Q in range(NQ):
                        nkb = 4 * Q + 4
                        bq, bqb = bqr.next()
                        mk.op("vector", lambda e: e.tensor_scalar(out=bq[:, 0:nkb], in0=Cc[:, 0:nkb, h], scalar1=Ee[:, 4 * Q + 3, h:h + 1],
                                                                  scalar2=None, op0=ALU.subtract), reads=[Ccb, Eeb], writes=[bqb])
                        attn_softmax_head(tiles, qT, qTb, Q * 512, kT, kTb, vt, vtb, h, Q,
                                          lambda kb: (bq[:, kb:kb + 1], [bqb]),
                                          lambda kb: ((cmask[:, (kb - 4 * Q) * 512:(kb - 4 * Q + 1) * 512], [cmaskb]) if kb >= 4 * Q else None),
                                          ostg, ostgb)
                    mk.dma("sync", OT[O_F + h * 64:O_F + (h + 1) * 64, :], ostg[:], reads=[ostgb], writes=[OTb], semb=ostgb)
                mk.barrier(recycle=True)

        def dsa(l):
            with contextlib.ExitStack() as st:
                kT, kTb, vt, vtb = load_kv(st, R_DK, V_D, 5)
                kix, kixb = sb(st, "kix", [128, S], BF16)
                mk.dma("sync", kix[0:64, :], PJT[R_IK:R_IK + 64, :], reads=[PJb], writes=[kixb])
                mk.dma("sync", kix[64:128, :], PJT[R_IK:R_IK + 64, :], reads=[PJb], writes=[kixb])
                tiles = attn_tiles(st)
                qr = Rot([sb(st, "dq%d" % i, [128, 3, 512], BF16) for i in range(2)])
                qir = Rot([sb(st, "dqi%d" % i, [128, 4, 512], BF16) for i in range(2)])
                sc, scb = sb(st, "sc", [128, S], F32)
                wk, wkb = sb(st, "wk", [128, S], F32)
                m8, m8b = sb(st, "m8", [128, 8], F32)
                mqr = Rot([sb(st, "mq%d" % i, [128, S], BF16) for i in range(2)])
                mT, mTb = sb(st, "mT", [128, NT, 512], BF16)
                rr = Rot([sb(st, "rl%d" % i, [128, 512], F32) for i in range(3)])
                osr = Rot([sb(st, "ostg%d" % i, [64, 5, 512], BF16) for i in range(2)])
                for Q in range(NQ):
                    qs = slice(Q * 512, (Q + 1) * 512)
                    q, qb_ = qr.next()
                    mk.dma("sync", q[:, 0:2, :], PJT[R_DQ:R_DQ + 256, qs].rearrange("(c p) t -> p c t", p=128), reads=[PJb], writes=[qb_])
                    mk.dma("sync", q[0:64, 2, :], PJT[R_DQ + 256:R_DQ + 320, qs], reads=[PJb], writes=[qb_])
                    qi, qib = qir.next()
                    mk.dma("sync", qi[:], PJT[R_IQ:R_IQ + 512, qs].rearrange("(c p) t -> p c t", p=128), reads=[PJb], writes=[qib])
                    mk.op("gpsimd", lambda e: e.memset(mT[:, 4 * Q:4 * Q + 4, :], 0.0), writes=[mTb])
                    for qsub in range(4):
                        qb = 4 * Q + qsub
                        nk = (qb + 1) * 128
                        for ks in range((nk + 511) // 512):
                            w_ = min(512, nk - ks * 512)
                            for hi in range(8):
                                ps, psb = psrot.next()
                                pbi = (hi % 2) * 64
                                mk.op("tensor", lambda e: e.matmul(ps[:, 0:w_], lhsT=qi[pbi:pbi + 64, hi // 2, qsub * 128:(qsub + 1) * 128],
                                                                   rhs=kix[pbi:pbi + 64, ks * 512:ks * 512 + w_], start=True, stop=True),
                                      reads=[qib, kixb], writes=[psb], skip_same=True)
                                r, rb = rr.next()
                                mk.op("scalar", lambda e: e.activation(out=r[:, 0:w_], in_=ps[:, 0:w_], func=AF.Relu), reads=[psb], writes=[rb])
                                if hi == 0:
                                    mk.op("vector", lambda e: e.tensor_scalar(out=sc[:, ks * 512:ks * 512 + w_], in0=r[:, 0:w_],
                                                                              scalar1=widx[:, qb, 0:1], scalar2=None, op0=ALU.mult),
                                          reads=[rb, widxb], writes=[scb])
                                else:
                                    mk.op("vector", lambda e: e.scalar_tensor_tensor(out=sc[:, ks * 512:ks * 512 + w_], in0=r[:, 0:w_],
                                                                                     scalar=widx[:, qb, hi:hi + 1],
                                                                                     in1=sc[:, ks * 512:ks * 512 + w_],
                                                                                     op0=ALU.mult, op1=ALU.add),
                                          reads=[rb, widxb, scb], writes=[scb])
                        mk.op("gpsimd", lambda e: e.tensor_tensor(out=sc[:, qb * 128:nk], in0=sc[:, qb * 128:nk], in1=negm[:], op=ALU.add),
                              reads=[scb, negmb], writes=[scb])
                        mq, mqb = mqr.next()
                        if qb >= 2:
                            src = sc
                            for it in range(32):
                                mk.op("vector", lambda e: e.max(out=m8[:], in_=src[:, 0:nk]), reads=[scb, wkb], writes=[m8b])
                                if it < 31:
                                    mk.op("vector", lambda e: e.match_replace(out=wk[:, 0:nk], in_to_replace=m8[:], in_values=src[:, 0:nk],
                                                                              imm_value=NEG), reads=[scb, wkb, m8b], writes=[wkb])
                                    src = wk
                            mk.op("vector", lambda e: e.tensor_scalar(out=mq[:, 0:nk], in0=sc[:, 0:nk], scalar1=m8[:, 7:8], scalar2=None,
                                                                      op0=ALU.is_ge), reads=[scb, m8b], writes=[mqb])
                        else:
                            mk.op("vector", lambda e: e.tensor_scalar(out=mq[:, 0:nk], in0=sc[:, 0:nk], scalar1=-1.0e29, scalar2=None,
                                                                      op0=ALU.is_ge), reads=[scb], writes=[mqb])
                        for g0 in range(0, qb + 1, 8):
                            g1 = min(qb + 1, g0 + 8)
                            for kb in range(g0, g1):
                                mk.op("tensor", lambda e: e.transpose(psT[:, (kb - g0) * 128:(kb - g0 + 1) * 128], mq[:, kb * 128:(kb + 1) * 128], ident[:]),
                                      reads=[mqb, identb], writes=[psTb], skip_same=True)
                            mk.op("scalar", lambda e: e.activation(out=mT[:, g0:g1, qsub * 128:(qsub + 1) * 128],
                                                                   in_=psT[:, 0:(g1 - g0) * 128].rearrange("p (k t) -> p k t", k=g1 - g0),
                                                                   func=AF.Identity), reads=[psTb], writes=[mTb])
                    ostg, ostgb = osr.next()
                    for h in range(5):
                        attn_softmax_head_dsa(tiles, q, qb_, kT, kTb, vt, vtb, h, Q, mT, mTb, ostg, ostgb)
                    mk.dma("sync", OT[O_D:O_D + 320, qs].rearrange("(h p) t -> p h t", p=64), ostg[:], reads=[ostgb], writes=[OTb], semb=ostgb)
                mk.barrier(recycle=True)

        def attn_softmax_head_dsa(tiles, q, qb_, kT, kTb, vt, vtb, h, Q, mT, mTb, ostg, ostgb):
            class V:
                def __getitem__(self, idx):
                    return ostg[idx[0], h, :]
            attn_softmax_head(tiles, q, qb_, 0, kT, kTb, vt, vtb, h, Q,
                              lambda kb: (None, []),
                              lambda kb: (mT[:, kb, :], [mTb]),
                              V(), ostgb)

        def sbk(l):
            mk.SAME_ENGINE_SYNC = False
            try:
                _sbk(l)
            finally:
                mk.SAME_ENGINE_SYNC = True

        def _sbk(l):
            DBG = SB_DBG
            with contextlib.ExitStack() as st:
                kT, kTb, vt, vtb = load_kv(st, R_SK, V_S, 5)
                qT, qTb = sb(st, "qT", [128, 3, S], BF16)
                mk.dma("sync", qT[:, 0:2, :], PJT[R_SQ:R_SQ + 256, :].rearrange("(c p) t -> p c t", p=128), reads=[PJb], writes=[qTb])
                mk.dma("sync", qT[0:64, 2, :], PJT[R_SQ + 256:R_SQ + 320, :], reads=[PJb], writes=[qTb])
                etr = Rot([sb(st, "et%d" % i, [128, 512], F32) for i in range(2)])
                ltr = Rot([sb(st, "lt%d" % i, [128, 512], BF16) for i in range(3)])
                t1r = Rot([sb(st, "st1%d" % i, [128, 512], F32) for i in range(2)])
                t2r = Rot([sb(st, "st2%d" % i, [128, 512], F32) for i in range(2)])
                atr = Rot([sb(st, "at%d" % i, [128, 512], BF16) for i in range(3)])
                rsr = Rot([sb(st, "rs%d" % i, [128, 512], F32) for i in range(2)])
                osr = Rot([sb(st, "ostg%d" % i, [64, S], BF16) for i in range(2)])
                for h in range(SB_H):
                    ck, pb = h // 2, (h % 2) * 64
                    ostg, ostgb = osr.next()
                    for Q in range(SB_NQ):
                        qs = slice(Q * 512, (Q + 1) * 512)
                        nkb = 4 * Q + 4
                        rs, rsb = rsr.next()
                        mk.op("gpsimd", lambda e: e.memset(rs[:], 0.0), writes=[rsb])
                        pO, pOb = PS[4 + (Q % 2)]
                        for kb in range(nkb - 1, -1, -1):
                            d = kb - 4 * Q
                            pz, pzb = psrot.next()
                            mk.op("tensor", lambda e: e.matmul(pz[:, :], lhsT=kT[pb:pb + 64, ck, kb * 128:(kb + 1) * 128],
                                                               rhs=qT[pb:pb + 64, ck, qs], start=True, stop=True),
                                  reads=[kTb, qTb], writes=[pzb], skip_same=True)
                            et, etb = etr.next()
                            lt, ltb = ltr.next()
                            mk.op("scalar", lambda e: e.activation(out=et[:], in_=pz[:, :], func=AF.Exp, scale=0.125), reads=[pzb], writes=[etb])
                            if 'a' in SB_V:
                                mk.op("scalar", lambda e: e.activation(out=lt[:], in_=et[:], func=AF.Identity), reads=[etb], writes=[ltb])
                            else:
                                mk.op("scalar", lambda e: e.activation(out=lt[:], in_=et[:], func=AF.Ln, bias=1.0), reads=[etb], writes=[ltb])
                            at, atb = lt, ltb
                            if DBG >= 2:
                                if d >= 0:
                                    mk.op("gpsimd", lambda e: e.tensor_tensor(out=lt[:], in0=lt[:], in1=smask[:, d * 512:(d + 1) * 512], op=ALU.mult),
                                          reads=[ltb, smaskb], writes=[ltb])
                                pc, pcb = psrot.next()
                                mk.op("tensor", lambda e: e.matmul(pc[:, :], lhsT=trige[:], rhs=lt[:], start=True, stop=True),
                                      reads=[trigeb, ltb], writes=[pcb], skip_same=True)
                            if DBG >= 3:
                                t1, t1b = t1r.next()
                                if 'c' in SB_V:
                                    mk.op("scalar", lambda e: e.activation(out=t1[:], in_=pz[:, :], func=AF.Identity, scale=0.125),
                                          reads=[pzb], writes=[t1b])
                                    mk.op("gpsimd", lambda e: e.tensor_tensor(out=t1[:], in0=t1[:], in1=rs[:], op=ALU.subtract),
                                          reads=[t1b, rsb], writes=[t1b])
                                else:
                                    mk.op("vector", lambda e: e.scalar_tensor_tensor(out=t1[:], in0=pz[:, :], scalar=0.125, in1=rs[:],
                                                                                     op0=ALU.mult, op1=ALU.subtract),
                                          reads=[pzb, rsb] + ([etb] if 'e' in SB_V else []), writes=[t1b])
                                t2, t2b = t2r.next()
                                if 'd' in SB_V:
                                    t2, t2b = t1, t1b
                                else:
                                    mk.op("vector", lambda e: e.tensor_tensor(out=t2[:], in0=t1[:], in1=pc[:, :], op=ALU.subtract),
                                          reads=[t1b, pcb], writes=[t2b])
                                at, atb = atr.next()
                                mk.op("scalar", lambda e: e.activation(out=at[:], in_=t2[:], func=AF.Exp), reads=[t2b], writes=[atb])
                            if DBG >= 4:
                                if d >= 0:
                                    mk.op("gpsimd", lambda e: e.tensor_tensor(out=at[:], in0=at[:], in1=smask[:, d * 512:(d + 1) * 512], op=ALU.mult),
                                          reads=[atb, smaskb], writes=[atb])
                            if DBG >= 5 and kb > 0:
                                pr, prb = psrot.next()
                                mk.op("tensor", lambda e: e.matmul(pr[:, :], lhsT=onesb[:], rhs=lt[:], start=True, stop=True),
                                      reads=[onesbb, ltb], writes=[prb], skip_same=True)
                                mk.op("vector", lambda e: e.tensor_tensor(out=rs[:], in0=pr[:, :], in1=rs[:], op=ALU.add),
                                      reads=[rsb, prb], writes=[rsb])
                            NV = 65 if 'b' in SB_V else 64
                            mk.op("tensor", lambda e: e.matmul(pO[0:NV, :], lhsT=vt[:, kb, h, 0:NV], rhs=at[:],
                                                               start=(kb == nkb - 1), stop=(kb == 0)),
                                  reads=[vtb, atb], writes=[pOb], skip_same=True)
                        mk.op("scalar", lambda e: e.activation(out=ostg[0:64, qs], in_=pO[0:64, :], func=AF.Identity), reads=[pOb], writes=[ostgb])
                    mk.dma("sync", OT[O_S + h * 64:O_S + (h + 1) * 64, :], ostg[:], reads=[ostgb], writes=[OTb], semb=ostgb)
                mk.barrier(recycle=True)

        def merge(l):
            win = W["w_in"][l]
            with contextlib.ExitStack() as st:
                hT, hTb = sb(st, "hT2", [128, 8, S // 2], BF16)
                gw, gwb = sb(st, "gw", [128, 8, 3072], BF16)
                for b in range(3):
                    wchunk(win, C_G + b * 1024, 1024, gw[:, :, b * 1024:(b + 1) * 1024], gwb)
                wbo, wbob = sb(st, "wbo", [128, 9, D], BF16)
                mk.dma("gpsimd", wbo[:, 0:3, :], W["w_fox_out"][l].rearrange("(c p) n -> p c n", p=128), writes=[wbob])
                for b, nm in ((1, "w_dsa_out"), (2, "w_sb_out")):
                    mk.dma("gpsimd", wbo[:, 3 * b:3 * b + 2, :], W[nm][l][0:256, :].rearrange("(c p) n -> p c n", p=128), writes=[wbob])
                    mk.dma("gpsimd", wbo[0:64, 3 * b + 2, :], W[nm][l][256:320, :], writes=[wbob])
                wo, wob = sb(st, "wo", [128, 8, D], BF16)
                wchunk(W["w_out"][l], 0, D, wo[:], wob)
                oqr = Rot([sb(st, "oq%d" % i, [128, 9, 512], BF16) for i in range(1)])
                sgr = Rot([sb(st, "msg%d" % i, [128, 512], F32) for i in range(3)])
                tmr = Rot([sb(st, "mtm%d" % i, [128, 512], F32) for i in range(3)])
                mTr = Rot([sb(st, "mmT%d" % i, [128, 8, 512], BF16) for i in range(2)])
                xr = Rot([sb(st, "mx%d" % i, [128, D], F32) for i in range(2)])
                KK = [128, 128, 128, 128, 128, 64, 128, 128, 64]
                for Q in range(NQ):
                    if Q % 4 == 0:
                        norm_pass(st, W["mix_norm"][l:l + 1, :], hT, hTb, t0=(Q // 4) * 16, nt=16)
                    qs = slice(Q * 512, (Q + 1) * 512)
                    ql = slice((Q % 4) * 512, (Q % 4 + 1) * 512)
                    oq, oqb = oqr.next()
                    mk.dma("sync", oq[:, 0:5, :], OT[0:640, qs].rearrange("(c p) t -> p c t", p=128), reads=[OTb], writes=[oqb])
                    mk.dma("sync", oq[0:64, 5, :], OT[640:704, qs], reads=[OTb], writes=[oqb])
                    mk.dma("sync", oq[:, 6:8, :], OT[768:1024, qs].rearrange("(c p) t -> p c t", p=128), reads=[OTb], writes=[oqb])
                    mk.dma("sync", oq[0:64, 8, :], OT[1024:1088, qs], reads=[OTb], writes=[oqb])
                    mT, mTb = mTr.next()
                    for fc in range(8):
                        tms = []
                        for b in range(3):
                            pg, pgb = psrot.next()
                            for kc in range(8):
                                mk.op("tensor", lambda e: e.matmul(pg[:, :], lhsT=gw[:, kc, b * 1024 + fc * 128:b * 1024 + (fc + 1) * 128],
                                                                   rhs=hT[:, kc, ql], start=(kc == 0), stop=(kc == 7)),
                                      reads=[gwb, hTb], writes=[pgb], skip_same=True)
                            sg, sgb = sgr.next()
                            mk.op("scalar", lambda e: e.activation(out=sg[:], in_=pg[:, :], func=AF.Sigmoid), reads=[pgb], writes=[sgb])
                            pp, ppb = psrot.next()
                            for c in range(3):
                                kk = KK[3 * b + c]
                                mk.op("tensor", lambda e: e.matmul(pp[:, :], lhsT=wbo[0:kk, 3 * b + c, fc * 128:(fc + 1) * 128],
                                                                   rhs=oq[0:kk, 3 * b + c, :], start=(c == 0), stop=(c == 2)),
                                      reads=[wbob, oqb], writes=[ppb], skip_same=True)
                            tm, tmb = tmr.next()
                            mk.op("vector", lambda e: e.tensor_tensor(out=tm[:], in0=sg[:], in1=pp[:, :], op=ALU.mult),
                                  reads=[sgb, ppb], writes=[tmb])
                            tms.append((tm, tmb))
                        (a0, a0b), (a1, a1b), (a2, a2b) = tms
                        mk.op("gpsimd", lambda e: e.tensor_tensor(out=a0[:], in0=a0[:], in1=a1[:], op=ALU.add), reads=[a0b, a1b], writes=[a0b])
                        mk.op("gpsimd", lambda e: e.tensor_tensor(out=mT[:, fc, :], in0=a0[:], in1=a2[:], op=ALU.add),
                              reads=[a0b, a2b], writes=[mTb])
                    for sub in range(4):
                        t = Q * 4 + sub
                        xt, xtb = xr.next()
                        mk.dma("sync", xt[:], X[t * 128:(t + 1) * 128, :], reads=[Xb[t]], writes=[xtb])
                        for half in range(2):
                            ps, psb = psrot.next()
                            for kc in range(8):
                                mk.op("tensor", lambda e: e.matmul(ps[:, :], lhsT=mT[:, kc, sub * 128:(sub + 1) * 128],
                                                                   rhs=wo[:, kc, half * 512:(half + 1) * 512], start=(kc == 0), stop=(kc == 7)),
                                      reads=[mTb, wob], writes=[psb], skip_same=True)
                            mk.op("vector", lambda e: e.tensor_tensor(out=xt[:, half * 512:(half + 1) * 512], in0=ps[:, :],
                                                                      in1=xt[:, half * 512:(half + 1) * 512], op=ALU.add),
                                  reads=[psb, xtb], writes=[xtb])
                        mk.dma("sync", X[t * 128:(t + 1) * 128, :], xt[:], reads=[xtb], writes=[Xb[t]], semb=xtb)
                mk.barrier(recycle=True)

        def ca(l):
            with contextlib.ExitStack() as st:
                hT, hTb = sb(st, "hT", [128, 8, S], BF16)
                norm_pass(st, W["ca_norm"][l:l + 1, :], hT, hTb)
                load_gain(W["mem_norm"][l:l + 1, :])
                memT, memTb = sb(st, "memT", [128, 8, 256], BF16)
                mx, mxb = sb(st, "cmx", [128, D], F32)
                mh, mhb = sb(st, "cmh", [128, D], BF16)
                junk, junkb = sb(st, "cjunk", [128, D], BF16)
                for mb in range(2):
                    mk.dma("sync", mx[:], mem_in[mb * 128:(mb + 1) * 128, :], writes=[mxb])
                    rstd_of(mx, mxb, junk, junkb)
                    mk.op("vector", lambda e: e.scalar_tensor_tensor(out=mh[:], in0=mx[:], scalar=small[:, 2:3], in1=gB[:],
                                                                     op0=ALU.mult, op1=ALU.mult), reads=[mxb, smallb, gBb], writes=[mhb])
                    for kc in range(8):
                        mk.op("tensor", lambda e: e.transpose(psT[:, kc * 128:(kc + 1) * 128], mh[:, kc * 128:(kc + 1) * 128], ident[:]),
                              reads=[mhb, identb], writes=[psTb], skip_same=True)
                    mk.op("scalar", lambda e: e.activation(out=memT[:, :, mb * 128:(mb + 1) * 128],
                                                           in_=psT[:, :].rearrange("p (k t) -> p k t", k=8), func=AF.Identity),
                          reads=[psTb], writes=[memTb])
                wkv, wkvb = sb(st, "wkv", [128, 8, D], BF16)
                wchunk(W["ca_w_kv"][l], 0, D, wkv[:], wkvb)
                wq, wqb = sb(st, "wq", [128, 8, 512], BF16)
                wchunk(W["ca_w_q"][l], 0, 512, wq[:], wqb)
                wo, wob = sb(st, "cwo", [128, 4, D], BF16)
                mk.dma("gpsimd", wo[:], W["ca_w_o"][l].rearrange("(c p) n -> p c n", p=128), writes=[wob])
                kTm, kTmb = sb(st, "kTm", [128, 4, 256], BF16)
                vm, vmb = sb(st, "vm", [128, 2, 512], BF16)
                for h in range(4):
                    ps, psb = psrot.next()
                    for kc in range(8):
                        mk.op("tensor", lambda e: e.matmul(ps[:, 0:256], lhsT=wkv[:, kc, h * 128:(h + 1) * 128], rhs=memT[:, kc, :],
                                                           start=(kc == 0), stop=(kc == 7)), reads=[wkvb, memTb], writes=[psb], skip_same=True)
                    mk.op("scalar", lambda e: e.activation(out=kTm[:, h, :], in_=ps[:, 0:256], func=AF.Identity), reads=[psb], writes=[kTmb])
                for mb in range(2):
                    ps, psb = psrot.next()
                    for kc in range(8):
                        mk.op("tensor", lambda e: e.matmul(ps[:, :], lhsT=memT[:, kc, mb * 128:(mb + 1) * 128], rhs=wkv[:, kc, 512:1024],
                                                           start=(kc == 0), stop=(kc == 7)), reads=[wkvb, memTb], writes=[psb], skip_same=True)
                    mk.op("scalar", lambda e: e.activation(out=vm[:, mb, :], in_=ps[:, :], func=AF.Identity), reads=[psb], writes=[vmb])
                qcr = Rot([sb(st, "qc%d" % i, [128, 512], BF16) for i in range(2)])
                ptr = Rot([sb(st, "cpt%d" % i, [128, 512], BF16) for i in range(3)])
                rdr = Rot([sb(st, "crd%d" % i, [128, 512], F32) for i in range(2)])
                ocr = Rot([sb(st, "oc%d" % i, [128, 4, 512], BF16) for i in range(2)])
                xr = Rot([sb(st, "cx%d" % i, [128, D], F32) for i in range(3)])
                sc_ = 128.0 ** -0.5
                for Q in range(NQ):
                    qs = slice(Q * 512, (Q + 1) * 512)
                    oc, ocb = ocr.next()
                    for h in range(4):
                        ps, psb = psrot.next()
                        for kc in range(8):
                            mk.op("tensor", lambda e: e.matmul(ps[:, :], lhsT=wq[:, kc, h * 128:(h + 1) * 128], rhs=hT[:, kc, qs],
                                                               start=(kc == 0), stop=(kc == 7)), reads=[wqb, hTb], writes=[psb], skip_same=True)
                        qc, qcb = qcr.next()
                        mk.op("scalar", lambda e: e.activation(out=qc[:], in_=ps[:, :], func=AF.Identity), reads=[psb], writes=[qcb])
                        pO, pOb = PS[4]
                        pD, pDb = PS[5]
                        for mb in range(2):
                            pS, pSb = psrot.next()
                            mk.op("tensor", lambda e: e.matmul(pS[:, :], lhsT=kTm[:, h, mb * 128:(mb + 1) * 128], rhs=qc[:], start=True, stop=True),
                                  reads=[kTmb, qcb], writes=[pSb], skip_same=True)
                            pt, ptb = ptr.next()
                            mk.op("scalar", lambda e: e.activation(out=pt[:], in_=pS[:, :], func=AF.Exp, scale=sc_), reads=[pSb], writes=[ptb])
                            mk.op("tensor", lambda e: e.matmul(pO[:, :], lhsT=vm[:, mb, h * 128:(h + 1) * 128], rhs=pt[:], start=(mb == 0), stop=(mb == 1)),
                                  reads=[vmb, ptb], writes=[pOb], skip_same=True)
                            mk.op("tensor", lambda e: e.matmul(pD[:, :], lhsT=onesb[:], rhs=pt[:], start=(mb == 0), stop=(mb == 1)),
                                  reads=[onesbb, ptb], writes=[pDb], skip_same=True)
                        rd, rdb = rdr.next()
                        mk.op("vector", lambda e: e.reciprocal(out=rd[:], in_=pD[:, :]), reads=[pDb], writes=[rdb])
                        mk.op("vector", lambda e: e.tensor_tensor(out=oc[:, h, :], in0=pO[:, :], in1=rd[:], op=ALU.mult),
                              reads=[pOb, rdb], writes=[ocb])
                    for sub in range(4):
                        t = Q * 4 + sub
                        xt, xtb = xr.next()
                        mk.dma("sync", xt[:], X[t * 128:(t + 1) * 128, :], reads=[Xb[t]], writes=[xtb])
                        for half in range(2):
                            ps, psb = psrot.next()
                            for h in range(4):
                                mk.op("tensor", lambda e: e.matmul(ps[:, :], lhsT=oc[:, h, sub * 128:(sub + 1) * 128],
                                                                   rhs=wo[:, h, half * 512:(half + 1) * 512], start=(h == 0), stop=(h == 3)),
                                      reads=[ocb, wob], writes=[psb], skip_same=True)
                            mk.op("vector", lambda e: e.tensor_tensor(out=xt[:, half * 512:(half + 1) * 512], in0=ps[:, :],
                                                                      in1=xt[:, half * 512:(half + 1) * 512], op=ALU.add),
                                  reads=[psb, xtb], writes=[xtb])
                        mk.dma("sync", X[t * 128:(t + 1) * 128, :], xt[:], reads=[xtb], writes=[Xb[t]], semb=xtb)
                mk.barrier(recycle=True)

        def final(raw):
            with contextlib.ExitStack() as st:
                xr = Rot([sb(st, "fx%d" % i, [128, D], F32) for i in range(3)])
                junk, junkb = sb(st, "fjunk", [128, D], BF16)
                if not raw:
                    load_gain(W["final_norm"][0:1, :])
                src = xstate["src"]
                outb = Buf("out")
                for t in range(NT):
                    xt, xtb = xr.next()
                    mk.dma("sync", xt[:], src[t * 128:(t + 1) * 128, :], reads=[Xb[t]], writes=[xtb])
                    if not raw:
                        rstd_of(xt, xtb, junk, junkb)
                        mk.op("vector", lambda e: e.scalar_tensor_tensor(out=xt[:], in0=xt[:], scalar=small[:, 2:3], in1=gB[:],
                                                                         op0=ALU.mult, op1=ALU.mult),
                              reads=[xtb, smallb, gBb], writes=[xtb])
                    mk.dma("sync", out_d[t * 128:(t + 1) * 128, :], xt[:], reads=[xtb], writes=[outb], semb=xtb)
                mk.barrier(recycle=True)

        for l in range(n_layers):
            if "ffn1" in stages:
                ffn(l, 1)
            if "proj" in stages:
                proj(l)
            if "fox" in stages:
                fox(l)
            if "dsa" in stages:
                dsa(l)
            if "sb" in stages:
                sbk(l)
            if "merge" in stages:
                merge(l)
            if "ca" in stages:
                ca(l)
            if "ffn2" in stages:
                ffn(l, 2)
        if not attn_probe:
            final(raw=debug)
        else:
            mk.barrier()
    return nc


def make_in_maps(inputs, n_layers=4):
    consts = host_consts()
    maps = []
    shared = {}
    for name, shp in WSPEC:
        shared[name] = np.ascontiguousarray(np.asarray(inputs[name], dtype=np.float32).reshape(shp)[:n_layers])
    for k, v in consts.items():
        shared["c_" + k] = np.ascontiguousarray(v.astype(np.float32))
    for c in range(8):
        b = c // 2
        m = dict(shared)
        m["x"] = np.ascontiguousarray(np.asarray(inputs["x"][b], dtype=np.float32))
        m["mem"] = np.ascontiguousarray(np.asarray(inputs["mem"][b], dtype=np.float32))
        m["pos"] = np.ascontiguousarray(np.asarray(inputs["positions"][b], dtype=np.int32).reshape(1, S))
        maps.append(m)
    return maps


def kernel(**inputs):
    nc = build()
    res = run_bass_kernel_spmd(nc, make_in_maps(inputs), core_ids=list(range(8)))
    out = np.stack([np.asarray(res.results[2 * b]["out"], dtype=np.float32) for b in range(4)], axis=0)
    return out
```

```python
import contextlib
import math
import numpy as np
import concourse.bass as bass
import concourse.mybir as mybir
from concourse.bass_utils import run_bass_kernel_spmd

F32 = mybir.dt.float32
BF16 = mybir.dt.bfloat16
I32 = mybir.dt.int32
ALU = mybir.AluOpType
AF = mybir.ActivationFunctionType

S = 4096
D = 1024
NT = 32
NQ = 8
DFF = 2816
NJ = 22
DIN = 6734
NEG = -1.0e30
SB_H = 5
SB_NQ = 8
SB_DBG = 5
SB_V = 'bc'
C_FQ, C_FK, C_FV, C_FF = 0, 384, 768, 1152
C_DQ, C_DK, C_DV = 1158, 1478, 1798
C_IQ, C_IK, C_IW = 2118, 2630, 2694
C_SQ, C_SK, C_SV = 2702, 3022, 3342
C_G = 3662
R_FQ, R_FK, R_DQ, R_DK, R_IQ, R_IK, R_SQ, R_SK = 0, 384, 768, 1088, 1408, 1920, 1984, 2304
PJ_ROWS = 2624
V_F, V_D, V_S = 0, 390, 715
V_COLS = 1040
O_F, O_D, O_S = 0, 384, 768
O_ROWS = 1152


class Sem:
    __slots__ = ("h", "count", "is_dma")

    def __init__(self, h, is_dma):
        self.h = h
        self.count = 0
        self.is_dma = is_dma


class Buf:
    __slots__ = ("name", "w", "r", "dsem")

    def __init__(self, name):
        self.name = name
        self.w = None
        self.r = {}
        self.dsem = None


class MK:
    SAME_ENGINE_SYNC = False

    @contextlib.contextmanager
    def small(self):
        prev = self.SAME_ENGINE_SYNC
        self.SAME_ENGINE_SYNC = True
        try:
            yield
        finally:
            self.SAME_ENGINE_SYNC = prev

    def __init__(self, nc, es):
        self.nc = nc
        self.es = es
        self.eng = {}
        self.esem = {}
        self.waited = {}
        self.dsems = []
        self.phase_sems = []
        self.free_sems = []
        self.uid = 0
        for name in ("tensor", "vector", "scalar", "gpsimd", "sync"):
            self.eng[name] = getattr(nc, name)
            self.esem[name] = Sem(es.enter_context(nc.semaphore("s_" + name)), False)
            self.waited[name] = {}

    def buf(self, name="b"):
        return Buf(name)

    def _dsem(self, b):
        if b.dsem is None:
            if self.free_sems:
                b.dsem = self.free_sems.pop()
            else:
                self.uid += 1
                b.dsem = Sem(self.es.enter_context(self.nc.semaphore("d%d" % self.uid)), True)
                self.dsems.append(b.dsem)
            self.phase_sems.append(b.dsem)
        return b.dsem

    def keep(self):
        self.phase_sems = []

    def _waits(self, en, reads, writes, skip_same):
        need = {}
        for b in reads:
            if b.w is not None:
                s, v = b.w
                if need.get(s, 0) < v:
                    need[s] = v
        for b in writes:
            if b.w is not None:
                s, v = b.w
                if need.get(s, 0) < v:
                    need[s] = v
            for s, v in b.r.items():
                if need.get(s, 0) < v:
                    need[s] = v
        wd = self.waited[en]
        own = self.esem[en]
        for s, v in need.items():
            if s.is_dma:
                v = s.count
            if s is own and (skip_same or not self.SAME_ENGINE_SYNC):
                continue
            if wd.get(s, 0) >= v:
                continue
            self.eng[en].wait_ge(s.h, v)
            wd[s] = v

    def op(self, en, fn, reads=(), writes=(), skip_same=False):
        self._waits(en, reads, writes, skip_same)
        inst = fn(self.eng[en])
        s = self.esem[en]
        s.count += 1
        inst.then_inc(s.h, 1)
        for b in reads:
            b.r[s] = s.count
        ev = (s, s.count)
        for b in writes:
            b.w = ev
            b.r = {}
        return inst

    def dma(self, q, out, in_, reads=(), writes=(), semb=None):
        self._waits(q, reads, writes, True)
        if semb is None:
            semb = (list(writes) + list(reads))[0]
        s = self._dsem(semb)
        inst = self.eng[q].dma_start(out=out, in_=in_)
        s.count += 16
        inst.then_inc(s.h, 16)
        for b in reads:
            b.r[s] = s.count
        ev = (s, s.count)
        for b in writes:
            b.w = ev
            b.r = {}
        return inst

    def barrier(self, recycle=False):
        if recycle:
            self.free_sems.extend(self.phase_sems)
            self.phase_sems = []
        for en in self.eng:
            wd = self.waited[en]
            for on, s in self.esem.items():
                if on == en or s.count == 0:
                    continue
                if wd.get(s, 0) < s.count:
                    self.eng[en].wait_ge(s.h, s.count)
                    wd[s] = s.count
            for s in self.dsems:
                if s.count and wd.get(s, 0) < s.count:
                    self.eng[en].wait_ge(s.h, s.count)
                    wd[s] = s.count


class Rot:
    def __init__(self, items):
        self.items = items
        self.i = 0

    def next(self):
        it = self.items[self.i % len(self.items)]
        self.i += 1
        return it


def host_consts():
    c = {}
    p = np.arange(128)[:, None]
    f = np.arange(128)[None, :]
    c["ident"] = (p == f).astype(np.float32)
    rot = np.zeros((128, 128), np.float32)
    for m in range(128):
        if (m % 64) < 32:
            rot[m + 32, m] = -1.0
        else:
            rot[m - 32, m] = 1.0
    c["rot"] = rot
    c["trile"] = (p <= f).astype(np.float32)
    c["trige"] = (p >= f).astype(np.float32)
    ff = np.arange(512)[None, None, :]
    dd = np.arange(4)[None, :, None]
    pp = np.arange(128)[:, None, None]
    c["cmask"] = ((128 * dd + pp) <= ff).astype(np.float32).reshape(128, 2048)
    c["smask"] = ((128 * dd + pp) < ff).astype(np.float32).reshape(128, 2048)
    c["negm"] = np.where(f <= p, 0.0, NEG).astype(np.float32)
    c["invf"] = (10000.0 ** (-(np.arange(128) % 32).astype(np.float64) / 32.0)).astype(np.float32).reshape(128, 1)
    return c


CONST_SHAPES = {"ident": 128, "rot": 128, "trile": 128, "trige": 128, "cmask": 2048, "smask": 2048,
                "negm": 128, "invf": 1}

WSPEC = [("ffn1_norm", [4, 1024]), ("ffn1_w_gu", [4, 1024, 5632]), ("ffn1_w_down", [4, 2816, 1024]),
         ("mix_norm", [4, 1024]), ("w_in", [4, 1024, 6734]), ("b_fgate", [4, 6]),
         ("w_fox_out", [4, 384, 1024]), ("w_dsa_out", [4, 320, 1024]), ("w_sb_out", [4, 320, 1024]),
         ("w_out", [4, 1024, 1024]), ("ca_norm", [4, 1024]), ("mem_norm", [4, 1024]),
         ("ca_w_q", [4, 1024, 512]), ("ca_w_kv", [4, 1024, 1024]), ("ca_w_o", [4, 512, 1024]),
         ("ffn2_norm", [4, 1024]), ("ffn2_w_gu", [4, 1024, 5632]), ("ffn2_w_down", [4, 2816, 1024]),
         ("final_norm", [1, 1024])]


def build(n_layers=4, stages=("ffn1", "proj", "fox", "dsa", "sb", "merge", "ca", "ffn2"), debug=False, attn_probe=False):
    nc = bass.Bass("TRN2", target_bir_lowering=False)
    x_in = nc.dram_tensor("x", [S, D], F32, kind="ExternalInput").ap()
    mem_in = nc.dram_tensor("mem", [256, D], F32, kind="ExternalInput").ap()
    pos_in = nc.dram_tensor("pos", [1, S], I32, kind="ExternalInput").ap()
    W = {}
    for name, shp in ([] if attn_probe else WSPEC):
        shp = [min(shp[0], n_layers)] + list(shp[1:])
        W[name] = nc.dram_tensor(name, shp, F32, kind="ExternalInput").ap()
    CD = {}
    for name, n in CONST_SHAPES.items():
        CD[name] = nc.dram_tensor("c_" + name, [128, n], F32, kind="ExternalInput").ap()
    out_d = nc.dram_tensor("out", [S, D], F32, kind="ExternalOutput").ap()
    dbgk = "ExternalOutput" if debug else "Internal"
    X = nc.dram_tensor("Xs", [S, D], F32, kind="Internal").ap()
    AT = nc.dram_tensor("ATs", [DFF, S], BF16, kind="Internal").ap()
    PJT = nc.dram_tensor("PJTs", [PJ_ROWS, S], BF16, kind=("ExternalInput" if attn_probe else dbgk)).ap()
    VT = nc.dram_tensor("VTs", [S, V_COLS], BF16, kind=("ExternalInput" if attn_probe else "Internal")).ap()
    OT = nc.dram_tensor("OTs", [O_ROWS, S], BF16, kind=dbgk).ap()

    with contextlib.ExitStack() as es:
        mk = MK(nc, es)

        uid = [0]

        def sb(stack, name, shape, dt):
            uid[0] += 1
            t = stack.enter_context(nc.sbuf_tensor("%s_%d" % (name, uid[0]), shape, dt))
            return t, Buf(name)

        Xb = [Buf("X%d" % t) for t in range(NT)]
        ATb = Buf("AT")
        PJb = Buf("PJT")
        VTb = Buf("VT")
        OTb = Buf("OT")
        xstate = {"src": x_in}

        PS = []
        for i in range(7):
            PS.append((es.enter_context(nc.psum_tensor("ps%d" % i, [128, 512], F32)), Buf("ps%d" % i)))
        psT, psTb = es.enter_context(nc.psum_tensor("psT", [128, 1024], BF16)), Buf("psT")

        def cload(name, dt):
            n = CONST_SHAPES[name]
            t, b = sb(es, "k_" + name, [128, n], dt)
            mk.dma("gpsimd", t[:], CD[name][:, :], writes=[b])
            return t, b

        ident, identb = cload("ident", BF16)
        rotm, rotb = cload("rot", BF16)
        trile, trileb = cload("trile", F32)
        trige, trigeb = cload("trige", BF16)
        cmask, cmaskb = cload("cmask", BF16)
        smask, smaskb = cload("smask", BF16)
        negm, negmb = cload("negm", F32)
        invf, invfb = cload("invf", F32)
        onesb, onesbb = sb(es, "onesb", [128, 128], BF16)
        onesf, onesfb = sb(es, "onesf", [128, 128], F32)
        mk.op("vector", lambda e: e.memset(onesb[:], 1.0), writes=[onesbb])
        mk.op("vector", lambda e: e.memset(onesf[:], 1.0), writes=[onesfb])
        Cc, Ccb = sb(es, "Cc", [128, NT, 6], F32)
        Ee, Eeb = sb(es, "Ee", [128, NT, 6], F32)
        widx, widxb = sb(es, "widx", [128, NT, 8], F32)
        gB, gBb = sb(es, "gB", [128, D], F32)
        small, smallb = sb(es, "small", [128, 8], F32)
        mk.keep()

        def rstd_of(xt, xtb, junk, junkb):
            with mk.small():
                mk.op("scalar", lambda e: e.activation(out=junk[:], in_=xt[:], func=AF.Square, accum_out=small[:, 0:1]),
                      reads=[xtb], writes=[junkb, smallb])
                mk.op("vector", lambda e: e.tensor_scalar(out=small[:, 1:2], in0=small[:, 0:1], scalar1=1.0 / D, scalar2=1e-6,
                                                          op0=ALU.mult, op1=ALU.add), reads=[smallb], writes=[smallb])
                mk.op("scalar", lambda e: e.activation(out=small[:, 1:2], in_=small[:, 1:2], func=AF.Sqrt),
                      reads=[smallb], writes=[smallb])
                mk.op("vector", lambda e: e.reciprocal(out=small[:, 2:3], in_=small[:, 1:2]), reads=[smallb], writes=[smallb])

        def load_gain(gain_ap):
            mk.dma("sync", gB[:], gain_ap.to_broadcast([128, D]), writes=[gBb])

        def norm_pass(st_unused, gain_ap, hT, hTb, t0=0, nt=NT):
            load_gain(gain_ap)
            with contextlib.ExitStack() as st:
                xr = Rot([sb(st, "nx%d" % i, [128, D], F32) for i in range(3)])
                hn = Rot([sb(st, "nh%d" % i, [128, D], BF16) for i in range(2)])
                junk, junkb = sb(st, "njunk", [128, D], BF16)
                src = xstate["src"]
                for t in range(t0, t0 + nt):
                    xt, xtb = xr.next()
                    mk.dma("sync", xt[:], src[t * 128:(t + 1) * 128, :], reads=[Xb[t]], writes=[xtb])
                    rstd_of(xt, xtb, junk, junkb)
                    h, hb = hn.next()
                    with mk.small():
                        mk.op("vector", lambda e: e.scalar_tensor_tensor(out=h[:], in0=xt[:], scalar=small[:, 2:3], in1=gB[:],
                                                                         op0=ALU.mult, op1=ALU.mult),
                              reads=[xtb, smallb, gBb], writes=[hb])
                    for kc in range(8):
                        mk.op("tensor", lambda e: e.transpose(psT[:, kc * 128:(kc + 1) * 128], h[:, kc * 128:(kc + 1) * 128], ident[:]),
                              reads=[hb, identb], writes=[psTb], skip_same=True)
                    mk.op("scalar", lambda e: e.activation(out=hT[:, :, (t - t0) * 128:(t - t0 + 1) * 128],
                                                           in_=psT[:, :].rearrange("p (k t) -> p k t", k=8), func=AF.Identity),
                          reads=[psTb], writes=[hTb])
                mk.barrier()

        def wchunk(w2d, c0, n, dst, dstb, kcs=8, q="gpsimd"):
            mk.dma(q, dst, w2d[:, c0:c0 + n].rearrange("(k p) n -> p k n", p=128), writes=[dstb])

        psrot = Rot(PS[0:4])

        def ffn(l, which):
            gain = W["ffn%d_norm" % which][l:l + 1, :]
            wgu = W["ffn%d_w_gu" % which][l]
            wdn = W["ffn%d_w_down" % which][l]
            with contextlib.ExitStack() as st:
                hT, hTb = sb(st, "hT", [128, 8, S], BF16)
                norm_pass(st, gain, hT, hTb)
                wr = Rot([sb(st, "wgu%d" % i, [128, 2, 8, 128], BF16) for i in range(3)])
                ar = Rot([sb(st, "aTj%d" % i, [128, S], BF16) for i in range(2)])
                sr = Rot([sb(st, "sg%d" % i, [128, 512], F32) for i in range(2)])
                wl = []

                def loadw(j):
                    w, wb = wr.next()
                    wchunk(wgu, j * 128, 128, w[:, 0], wb)
                    mk.dma("gpsimd", w[:, 1], wgu[:, DFF + j * 128:DFF + (j + 1) * 128].rearrange("(k p) n -> p k n", p=128),
                           writes=[wb])
                    wl.append((w, wb))
                loadw(0)
                loadw(1)
                for j in range(NJ):
                    if j + 2 < NJ:
                        loadw(j + 2)
                    w, wb = wl[j]
                    a, ab = ar.next()
                    for Q in range(NQ):
                        pg, pgb = psrot.next()
                        pu, pub = psrot.next()
                        for kc in range(8):
                            mk.op("tensor", lambda e: e.matmul(pg[:, :], lhsT=w[:, 0, kc, :], rhs=hT[:, kc, Q * 512:(Q + 1) * 512],
                                                               start=(kc == 0), stop=(kc == 7)),
                                  reads=[wb, hTb], writes=[pgb], skip_same=True)
                        for kc in range(8):
                            mk.op("tensor", lambda e: e.matmul(pu[:, :], lhsT=w[:, 1, kc, :], rhs=hT[:, kc, Q * 512:(Q + 1) * 512],
                                                               start=(kc == 0), stop=(kc == 7)),
                                  reads=[wb, hTb], writes=[pub], skip_same=True)
                        sg, sgb = sr.next()
                        mk.op("scalar", lambda e: e.activation(out=sg[:], in_=pg[:, :], func=AF.Silu), reads=[pgb], writes=[sgb])
                        mk.op("vector", lambda e: e.tensor_tensor(out=a[:, Q * 512:(Q + 1) * 512], in0=sg[:], in1=pu[:, :], op=ALU.mult),
                              reads=[sgb, pub], writes=[ab])
                    mk.dma("sync", AT[j * 128:(j + 1) * 128, :], a[:], reads=[ab], writes=[ATb], semb=ab)
                mk.barrier(recycle=True)
            with contextlib.ExitStack() as st:
                wd, wdb = sb(st, "wd", [128, NJ, D], BF16)
                for j in range(NJ):
                    mk.dma("gpsimd", wd[:, j, :], wdn[j * 128:(j + 1) * 128, :], writes=[wdb])
                aq = Rot([sb(st, "aq%d" % i, [128, NJ, 512], BF16) for i in range(2)])
                xr = Rot([sb(st, "dx%d" % i, [128, D], F32) for i in range(3)])
                src = xstate["src"]
                for Q in range(NQ):
                    a, ab = aq.next()
                    mk.dma("sync", a[:], AT[:, Q * 512:(Q + 1) * 512].rearrange("(j p) t -> p j t", p=128), reads=[ATb], writes=[ab])
                    for sub in range(4):
                        t = Q * 4 + sub
                        xt, xtb = xr.next()
                        mk.dma("sync", xt[:], src[t * 128:(t + 1) * 128, :], reads=[Xb[t]], writes=[xtb])
                        for half in range(2):
                            ps, psb = psrot.next()
                            for j in range(NJ):
                                mk.op("tensor", lambda e: e.matmul(ps[:, :], lhsT=a[:, j, sub * 128:(sub + 1) * 128],
                                                                   rhs=wd[:, j, half * 512:(half + 1) * 512],
                                                                   start=(j == 0), stop=(j == NJ - 1)),
                                      reads=[ab, wdb], writes=[psb], skip_same=True)
                            mk.op("vector", lambda e: e.scalar_tensor_tensor(out=xt[:, half * 512:(half + 1) * 512], in0=ps[:, :], scalar=0.5,
                                                                             in1=xt[:, half * 512:(half + 1) * 512],
                                                                             op0=ALU.mult, op1=ALU.add),
                                  reads=[psb, xtb], writes=[xtb])
                        mk.dma("sync", X[t * 128:(t + 1) * 128, :], xt[:], reads=[xtb], writes=[Xb[t]], semb=xtb)
                xstate["src"] = X
                mk.barrier(recycle=True)

        def proj(l):
            win = W["w_in"][l]
            with contextlib.ExitStack() as st:
                hT, hTb = sb(st, "hT", [128, 8, S], BF16)
                norm_pass(st, W["mix_norm"][l:l + 1, :], hT, hTb)
                cosT, cosb = sb(st, "cosT", [128, S], F32)
                sinT, sinb = sb(st, "sinT", [128, S], F32)
                st_r = contextlib.ExitStack()
                ang, angb = sb(st_r, "ang", [128, S], F32)
                tmpf, tmpfb = sb(st_r, "tmpf", [128, S], F32)
                posi, posib = sb(st_r, "posi", [128, S], I32)
                with mk.small():
                    mk.dma("sync", posi[:], pos_in.to_broadcast([128, S]), writes=[posib])
                    mk.op("vector", lambda e: e.tensor_copy(out=ang[:], in_=posi[:]), reads=[posib], writes=[angb])
                    mk.op("vector", lambda e: e.tensor_scalar(out=ang[:], in0=ang[:], scalar1=invf[:, 0:1], scalar2=None, op0=ALU.mult),
                          reads=[angb, invfb], writes=[angb])
                    TWO_PI = 2.0 * math.pi
                    C1 = 6.28125
                    C2 = TWO_PI - C1
                    for (dst, dstb, shift) in ((sinT, sinb, 0.0), (cosT, cosb, math.pi / 2)):
                        mk.op("vector", lambda e: e.tensor_scalar(out=tmpf[:], in0=ang[:], scalar1=shift, scalar2=1.0 / TWO_PI,
                                                                  op0=ALU.add, op1=ALU.mult), reads=[angb], writes=[tmpfb])
                        mk.op("vector", lambda e: e.tensor_copy(out=posi[:], in_=tmpf[:]), reads=[tmpfb], writes=[posib])
                        mk.op("vector", lambda e: e.tensor_copy(out=tmpf[:], in_=posi[:]), reads=[posib], writes=[tmpfb])
                        mk.op("vector", lambda e: e.scalar_tensor_tensor(out=dst[:], in0=tmpf[:], scalar=-C1, in1=ang[:],
                                                                         op0=ALU.mult, op1=ALU.add), reads=[tmpfb, angb], writes=[dstb])
                        mk.op("vector", lambda e: e.scalar_tensor_tensor(out=dst[:], in0=tmpf[:], scalar=-C2, in1=dst[:],
                                                                         op0=ALU.mult, op1=ALU.add), reads=[tmpfb, dstb], writes=[dstb])
                        mk.op("vector", lambda e: e.tensor_scalar(out=dst[:], in0=dst[:], scalar1=shift, scalar2=math.pi,
                                                                  op0=ALU.add, op1=ALU.min), reads=[dstb], writes=[dstb])
                        mk.op("vector", lambda e: e.tensor_scalar(out=dst[:], in0=dst[:], scalar1=-math.pi, scalar2=None,
                                                                  op0=ALU.max), reads=[dstb], writes=[dstb])
                        mk.op("scalar", lambda e: e.activation(out=dst[:], in_=dst[:], func=AF.Sin), reads=[dstb], writes=[dstb])
                mk.barrier()
                st_r.close()
                chunks = []
                def add_group(c0, r0, width, rope):
                    o = 0
                    while o < width:
                        m = min(128, width - o)
                        chunks.append((c0 + o, r0 + o, m, rope))
                        o += m
                add_group(C_FQ, R_FQ, 384, False)
                add_group(C_FK, R_FK, 384, False)
                add_group(C_DQ, R_DQ, 320, True)
                add_group(C_DK, R_DK, 320, True)
                add_group(C_IQ, R_IQ, 512, True)
                add_group(C_IK, R_IK, 64, True)
                add_group(C_SQ, R_SQ, 320, False)
                add_group(C_SK, R_SK, 320, False)
                wr = Rot([sb(st, "pw%d" % i, [128, 8, 128], BF16) for i in range(3)])
                sr = Rot([sb(st, "pst%d" % i, [128, S], BF16) for i in range(2)])
                xbr = Rot([sb(st, "pxb%d" % i, [128, 512], BF16) for i in range(2)])
                t1r = Rot([sb(st, "pt1%d" % i, [128, 512], F32) for i in range(2)])
                t2r = Rot([sb(st, "pt2%d" % i, [128, 512], F32) for i in range(2)])
                wl = []

                def loadw(i):
                    c0, r0, m, rope = chunks[i]
                    w, wb = wr.next()
                    wchunk(win, c0, m, w[:, :, 0:m], wb)
                    wl.append((w, wb))
                loadw(0)
                loadw(1)
                for i, (c0, r0, m, rope) in enumerate(chunks):
                    if i + 2 < len(chunks):
                        loadw(i + 2)
                    w, wb = wl[i]
                    stg, stgb = sr.next()
                    for Q in range(NQ):
                        qs = slice(Q * 512, (Q + 1) * 512)
                        ps, psb = psrot.next()
                        for kc in range(8):
                            mk.op("tensor", lambda e: e.matmul(ps[0:m, :], lhsT=w[:, kc, 0:m], rhs=hT[:, kc, qs],
                                                               start=(kc == 0), stop=(kc == 7)),
                                  reads=[wb, hTb], writes=[psb], skip_same=True)
                        if not rope:
                            mk.op("scalar", lambda e: e.activation(out=stg[0:m, qs], in_=ps[0:m, :], func=AF.Identity),
                                  reads=[psb], writes=[stgb])
                        else:
                            xb_, xbb = xbr.next()
                            mk.op("scalar", lambda e: e.activation(out=xb_[0:m, :], in_=ps[0:m, :], func=AF.Identity),
                                  reads=[psb], writes=[xbb])
                            pr, prb = psrot.next()
                            mk.op("tensor", lambda e: e.matmul(pr[0:m, :], lhsT=rotm[0:m, 0:m], rhs=xb_[0:m, :], start=True, stop=True),
                                  reads=[rotb, xbb], writes=[prb], skip_same=True)
                            t1, t1b = t1r.next()
                            t2, t2b = t2r.next()
                            mk.op("gpsimd", lambda e: e.tensor_tensor(out=t1[0:m, :], in0=xb_[0:m, :], in1=cosT[0:m, qs], op=ALU.mult),
                                  reads=[xbb, cosb], writes=[t1b])
                            mk.op("vector", lambda e: e.tensor_tensor(out=t2[0:m, :], in0=pr[0:m, :], in1=sinT[0:m, qs], op=ALU.mult),
                                  reads=[prb, sinb], writes=[t2b])
                            mk.op("gpsimd", lambda e: e.tensor_tensor(out=stg[0:m, qs], in0=t1[0:m, :], in1=t2[0:m, :], op=ALU.add),
                                  reads=[t1b, t2b], writes=[stgb])
                    mk.dma("sync", PJT[r0:r0 + m, :], stg[0:m, :], reads=[stgb], writes=[PJb], semb=stgb)
                wv, wvb = sb(st, "wv", [128, 8, 1024], BF16)
                wchunk(win, C_FV, 384, wv[:, :, 0:384], wvb)
                wchunk(win, C_DV, 320, wv[:, :, 384:704], wvb)
                wchunk(win, C_SV, 320, wv[:, :, 704:1024], wvb)
                wf, wfb = sb(st, "wf", [128, 8, 16], BF16)
                mk.op("vector", lambda e: e.memset(wf[:], 0.0), writes=[wfb])
                wchunk(win, C_FF, 6, wf[:, :, 0:6], wfb)
                wchunk(win, C_IW, 8, wf[:, :, 8:16], wfb)
                vst = Rot([sb(st, "vst%d" % i, [128, 16, 65], BF16) for i in range(2)])
                fw, fwb = sb(st, "fw", [128, NT, 16], F32)
                for t in range(NT):
                    ts_ = slice(t * 128, (t + 1) * 128)
                    v, vb = vst.next()
                    mk.op("gpsimd", lambda e: e.memset(v[:], 1.0), writes=[vb])
                    for half in range(2):
                        ps, psb = psrot.next()
                        for kc in range(8):
                            mk.op("tensor", lambda e: e.matmul(ps[:, :], lhsT=hT[:, kc, ts_], rhs=wv[:, kc, half * 512:(half + 1) * 512],
                                                               start=(kc == 0), stop=(kc == 7)),
                                  reads=[hTb, wvb], writes=[psb], skip_same=True)
                        mk.op("scalar", lambda e: e.activation(out=v[:, half * 8:(half + 1) * 8, 0:64],
                                                               in_=ps[:, :].rearrange("p (h d) -> p h d", h=8), func=AF.Identity),
                              reads=[psb], writes=[vb])
                    mk.dma("sync", VT[ts_, :], v[:].rearrange("p h d -> p (h d)"), reads=[vb], writes=[VTb], semb=vb)
                    ps, psb = psrot.next()
                    for kc in range(8):
                        mk.op("tensor", lambda e: e.matmul(ps[:, 0:16], lhsT=hT[:, kc, ts_], rhs=wf[:, kc, :],
                                                           start=(kc == 0), stop=(kc == 7)),
                              reads=[hTb, wfb], writes=[psb], skip_same=True)
                    mk.op("vector", lambda e: e.tensor_copy(out=fw[:, t, :], in_=ps[:, 0:16]), reads=[psb], writes=[fwb])
                with mk.small():
                    bt, btb = sb(st, "bt", [128, 6], F32)
                    mk.dma("sync", bt[:], W["b_fgate"][l:l + 1, :].to_broadcast([128, 6]), writes=[btb])
                    lf, lfb = sb(st, "lf", [128, NT, 6], F32)
                    for h in range(6):
                        mk.op("vector", lambda e: e.tensor_scalar(out=lf[:, :, h], in0=fw[:, :, h], scalar1=bt[:, h:h + 1], scalar2=None,
                                                                  op0=ALU.add), reads=[fwb, btb], writes=[lfb])
                    mk.op("scalar", lambda e: e.activation(out=lf[:], in_=lf[:], func=AF.Exp, scale=-1.0), reads=[lfb], writes=[lfb])
                    mk.op("scalar", lambda e: e.activation(out=lf[:], in_=lf[:], func=AF.Ln, bias=1.0), reads=[lfb], writes=[lfb])
                    p1, p1b = psrot.next()
                    p2, p2b = psrot.next()
                    lf2 = lf[:].rearrange("p t h -> p (t h)")
                    mk.op("tensor", lambda e: e.matmul(p1[:, 0:192], lhsT=trile[:], rhs=lf2, start=True, stop=True),
                          reads=[trileb, lfb], writes=[p1b], skip_same=True)
                    mk.op("tensor", lambda e: e.matmul(p2[:, 0:192], lhsT=onesf[:], rhs=lf2, start=True, stop=True),
                          reads=[onesfb, lfb], writes=[p2b], skip_same=True)
                    tot, totb = sb(st, "tot", [128, NT, 6], F32)
                    mk.op("vector", lambda e: e.tensor_copy(out=tot[:].rearrange("p t h -> p (t h)"), in_=p2[:, 0:192]),
                          reads=[p2b], writes=[totb])
                    mk.op("vector", lambda e: e.tensor_copy(out=Ee[:, 0, :], in_=tot[:, 0, :]), reads=[totb], writes=[Eeb])
                    for t in range(1, NT):
                        mk.op("vector", lambda e: e.tensor_tensor(out=Ee[:, t, :], in0=Ee[:, t - 1, :], in1=tot[:, t, :], op=ALU.add),
                              reads=[Eeb, totb], writes=[Eeb])
                    mk.op("vector", lambda e: e.tensor_tensor(out=Cc[:].rearrange("p t h -> p (t h)"), in0=p1[:, 0:192],
                                                              in1=Ee[:].rearrange("p t h -> p (t h)"), op=ALU.add),
                          reads=[p1b, Eeb], writes=[Ccb])
                    mk.op("vector", lambda e: e.tensor_tensor(out=Cc[:], in0=Cc[:], in1=tot[:], op=ALU.subtract),
                          reads=[Ccb, totb], writes=[Ccb])
                    mk.op("vector", lambda e: e.tensor_scalar(out=widx[:], in0=fw[:, :, 8:16], scalar1=(8.0 ** -0.5) * (64.0 ** -0.5),
                                                              scalar2=None, op0=ALU.mult), reads=[fwb], writes=[widxb])
                mk.barrier(recycle=True)

        def load_kv(st, r_k, v_off, nh):
            kT, kTb = sb(st, "kT", [128, 3, S], BF16)
            nfull = (nh * 64) // 128
            if nfull:
                mk.dma("sync", kT[:, 0:nfull, :], PJT[r_k:r_k + nfull * 128, :].rearrange("(c p) t -> p c t", p=128),
                       reads=[PJb], writes=[kTb])
            if nh % 2:
                mk.dma("sync", kT[0:64, nfull, :], PJT[r_k + nfull * 128:r_k + nfull * 128 + 64, :], reads=[PJb], writes=[kTb])
            vt, vtb = sb(st, "vt", [128, NT, nh, 65], BF16)
            for g in range(4):
                mk.dma("sync", vt[:, g * 8:(g + 1) * 8].rearrange("p t h d -> p t (h d)"),
                       VT[g * 1024:(g + 1) * 1024, v_off:v_off + nh * 65].rearrange("(t p) c -> p t c", p=128),
                       reads=[VTb], writes=[vtb])
            return kT, kTb, vt, vtb

        def load_q(st, name, r_q, nh, Q=None):
            n = S if Q is None else 512
            cs = slice(0, S) if Q is None else slice(Q * 512, (Q + 1) * 512)
            return n, cs

        def attn_softmax_head(st_tiles, qT, qTb, qoff, kT, kTb, vt, vtb, h, Q, bias_fn, mask_fn, ostg, ostgb):
            ptr, rcs, nbs = st_tiles
            ck, pb = h // 2, (h % 2) * 64
            nkb = 4 * Q + 4
            pO, pOb = PS[4 + (Q % 2)]
            pss = [None] * nkb

            def issue_s(kb):
                ps, psb = psrot.next()
                mk.op("tensor", lambda e: e.matmul(ps[:, :], lhsT=kT[pb:pb + 64, ck, kb * 128:(kb + 1) * 128],
                                                   rhs=qT[pb:pb + 64, ck, qoff:qoff + 512], start=True, stop=True),
                      reads=[kTb, qTb], writes=[psb], skip_same=True)
                pss[kb] = (ps, psb)
            issue_s(0)
            for kb in range(nkb):
                if kb + 1 < nkb:
                    issue_s(kb + 1)
                ps, psb = pss[kb]
                pt, ptb = ptr.next()
                bias_ap, bias_bufs = bias_fn(kb)
                if bias_ap is None:
                    mk.op("scalar", lambda e: e.activation(out=pt[:], in_=ps[:, :], func=AF.Exp, scale=0.125), reads=[psb], writes=[ptb])
                else:
                    mk.op("scalar", lambda e: e.activation(out=pt[:], in_=ps[:, :], func=AF.Exp, scale=0.125, bias=bias_ap),
                          reads=[psb] + bias_bufs, writes=[ptb])
                m = mask_fn(kb)
                if m is not None:
                    m_ap, m_bufs = m
                    mk.op("gpsimd", lambda e: e.tensor_tensor(out=pt[:], in0=pt[:], in1=m_ap, op=ALU.mult),
                          reads=[ptb] + m_bufs, writes=[ptb])
                mk.op("tensor", lambda e: e.matmul(pO[0:65, :], lhsT=vt[:, kb, h, :], rhs=pt[:], start=(kb == 0), stop=(kb == nkb - 1)),
                      reads=[vtb, ptb], writes=[pOb], skip_same=True)
            rc, rcb = rcs.next()
            nb, nbb = nbs.next()
            mk.op("vector", lambda e: e.reciprocal(out=rc[64:65, :], in_=pO[64:65, :]), reads=[pOb], writes=[rcb])
            pB, pBb = PS[6]
            mk.op("tensor", lambda e: e.matmul(pB[0:64, :], lhsT=onesf[64:65, 0:64], rhs=rc[64:65, :], start=True, stop=True),
                  reads=[onesfb, rcb], writes=[pBb], skip_same=True)
            mk.op("scalar", lambda e: e.activation(out=nb[0:64, :], in_=pB[0:64, :], func=AF.Identity), reads=[pBb], writes=[nbb])
            mk.op("vector", lambda e: e.tensor_tensor(out=ostg[0:64, Q * 512:(Q + 1) * 512], in0=pO[0:64, :], in1=nb[0:64, :], op=ALU.mult),
                  reads=[pOb, nbb], writes=[ostgb])

        def attn_tiles(st):
            ptr = Rot([sb(st, "pt%d" % i, [128, 512], BF16) for i in range(4)])
            rcs = Rot([sb(st, "rc%d" % i, [128, 512], F32) for i in range(2)])
            nbs = Rot([sb(st, "nb%d" % i, [64, 512], F32) for i in range(2)])
            return ptr, rcs, nbs

        def fox(l):
            with contextlib.ExitStack() as st:
                kT, kTb, vt, vtb = load_kv(st, R_FK, V_F, 6)
                qT, qTb = sb(st, "qT", [128, 3, S], BF16)
                mk.dma("sync", qT[:], PJT[R_FQ:R_FQ + 384, :].rearrange("(c p) t -> p c t", p=128), reads=[PJb], writes=[qTb])
                tiles = attn_tiles(st)
                osr = Rot([sb(st, "ostg%d" % i, [64, S], BF16) for i in range(2)])
                bqr = Rot([sb(st, "bq%d" % i, [128, NT], F32) for i in range(2)])
                for h in range(6):
                    ostg, ostgb = osr.next()
                    for Q in range(NQ):
                        nkb = 4 * Q + 4
                        bq, bqb = bqr.next()
                        mk.op("vector", lambda e: e.tensor_scalar(out=bq[:, 0:nkb], in0=Cc[:, 0:nkb, h], scalar1=Ee[:, 4 * Q + 3, h:h + 1],
                                                                  scalar2=None, op0=ALU.subtract), reads=[Ccb, Eeb], writes=[bqb])
                        attn_softmax_head(tiles, qT, qTb, Q * 512, kT, kTb, vt, vtb, h, Q,
                                          lambda kb: (bq[:, kb:kb + 1], [bqb]),
                                          lambda kb: ((cmask[:, (kb - 4 * Q) * 512:(kb - 4 * Q + 1) * 512], [cmaskb]) if kb >= 4 * Q else None),
                                          ostg, ostgb)
                    mk.dma("sync", OT[O_F + h * 64:O_F + (h + 1) * 64, :], ostg[:], reads=[ostgb], writes=[OTb], semb=ostgb)
                mk.barrier(recycle=True)

        def dsa(l):
            with contextlib.ExitStack() as st:
                kT, kTb, vt, vtb = load_kv(st, R_DK, V_D, 5)
                kix, kixb = sb(st, "kix", [128, S], BF16)
                mk.dma("sync", kix[0:64, :], PJT[R_IK:R_IK + 64, :], reads=[PJb], writes=[kixb])
                mk.dma("sync", kix[64:128, :], PJT[R_IK:R_IK + 64, :], reads=[PJb], writes=[kixb])
                tiles = attn_tiles(st)
                qr = Rot([sb(st, "dq%d" % i, [128, 3, 512], BF16) for i in range(2)])
                qir = Rot([sb(st, "dqi%d" % i, [128, 4, 512], BF16) for i in range(2)])
                sc, scb = sb(st, "sc", [128, S], F32)
                wk, wkb = sb(st, "wk", [128, S], F32)
                m8, m8b = sb(st, "m8", [128, 8], F32)
                mqr = Rot([sb(st, "mq%d" % i, [128, S], BF16) for i in range(2)])
                mT, mTb = sb(st, "mT", [128, NT, 512], BF16)
                rr = Rot([sb(st, "rl%d" % i, [128, 512], F32) for i in range(3)])
                osr = Rot([sb(st, "ostg%d" % i, [64, 5, 512], BF16) for i in range(2)])
                for Q in range(NQ):
                    qs = slice(Q * 512, (Q + 1) * 512)
                    q, qb_ = qr.next()
                    mk.dma("sync", q[:, 0:2, :], PJT[R_DQ:R_DQ + 256, qs].rearrange("(c p) t -> p c t", p=128), reads=[PJb], writes=[qb_])
                    mk.dma("sync", q[0:64, 2, :], PJT[R_DQ + 256:R_DQ + 320, qs], reads=[PJb], writes=[qb_])
                    qi, qib = qir.next()
                    mk.dma("sync", qi[:], PJT[R_IQ:R_IQ + 512, qs].rearrange("(c p) t -> p c t", p=128), reads=[PJb], writes=[qib])
                    mk.op("gpsimd", lambda e: e.memset(mT[:, 4 * Q:4 * Q + 4, :], 0.0), writes=[mTb])
                    for qsub in range(4):
                        qb = 4 * Q + qsub
                        nk = (qb + 1) * 128
                        for ks in range((nk + 511) // 512):
                            w_ = min(512, nk - ks * 512)
                            for hi in range(8):
                                ps, psb = psrot.next()
                                pbi = (hi % 2) * 64
                                mk.op("tensor", lambda e: e.matmul(ps[:, 0:w_], lhsT=qi[pbi:pbi + 64, hi // 2, qsub * 128:(qsub + 1) * 128],
                                                                   rhs=kix[pbi:pbi + 64, ks * 512:ks * 512 + w_], start=True, stop=True),
                                      reads=[qib, kixb], writes=[psb], skip_same=True)
                                r, rb = rr.next()
                                mk.op("scalar", lambda e: e.activation(out=r[:, 0:w_], in_=ps[:, 0:w_], func=AF.Relu), reads=[psb], writes=[rb])
                                if hi == 0:
                                    mk.op("vector", lambda e: e.tensor_scalar(out=sc[:, ks * 512:ks * 512 + w_], in0=r[:, 0:w_],
                                                                              scalar1=widx[:, qb, 0:1], scalar2=None, op0=ALU.mult),
                                          reads=[rb, widxb], writes=[scb])
                                else:
                                    mk.op("vector", lambda e: e.scalar_tensor_tensor(out=sc[:, ks * 512:ks * 512 + w_], in0=r[:, 0:w_],
                                                                                     scalar=widx[:, qb, hi:hi + 1],
                                                                                     in1=sc[:, ks * 512:ks * 512 + w_],
                                                                                     op0=ALU.mult, op1=ALU.add),
                                          reads=[rb, widxb, scb], writes=[scb])
                        mk.op("gpsimd", lambda e: e.tensor_tensor(out=sc[:, qb * 128:nk], in0=sc[:, qb * 128:nk], in1=negm[:], op=ALU.add),
                              reads=[scb, negmb], writes=[scb])
                        with mk.small():
                            mq, mqb = mqr.next()
                            if qb >= 2:
                                src = sc
                                for it in range(32):
                                    mk.op("vector", lambda e: e.max(out=m8[:], in_=src[:, 0:nk]), reads=[scb, wkb], writes=[m8b])
                                    if it < 31:
                                        mk.op("vector", lambda e: e.match_replace(out=wk[:, 0:nk], in_to_replace=m8[:], in_values=src[:, 0:nk],
                                                                                  imm_value=NEG), reads=[scb, wkb, m8b], writes=[wkb])
                                        src = wk
                                mk.op("vector", lambda e: e.tensor_scalar(out=mq[:, 0:nk], in0=sc[:, 0:nk], scalar1=m8[:, 7:8], scalar2=None,
                                                                          op0=ALU.is_ge), reads=[scb, m8b], writes=[mqb])
                            else:
                                mk.op("vector", lambda e: e.tensor_scalar(out=mq[:, 0:nk], in0=sc[:, 0:nk], scalar1=-1.0e29, scalar2=None,
                                                                          op0=ALU.is_ge), reads=[scb], writes=[mqb])
                        for g0 in range(0, qb + 1, 8):
                            g1 = min(qb + 1, g0 + 8)
                            for kb in range(g0, g1):
                                mk.op("tensor", lambda e: e.transpose(psT[:, (kb - g0) * 128:(kb - g0 + 1) * 128], mq[:, kb * 128:(kb + 1) * 128], ident[:]),
                                      reads=[mqb, identb], writes=[psTb], skip_same=True)
                            mk.op("scalar", lambda e: e.activation(out=mT[:, g0:g1, qsub * 128:(qsub + 1) * 128],
                                                                   in_=psT[:, 0:(g1 - g0) * 128].rearrange("p (k t) -> p k t", k=g1 - g0),
                                                                   func=AF.Identity), reads=[psTb], writes=[mTb])
                    ostg, ostgb = osr.next()
                    for h in range(5):
                        attn_softmax_head_dsa(tiles, q, qb_, kT, kTb, vt, vtb, h, Q, mT, mTb, ostg, ostgb)
                    mk.dma("sync", OT[O_D:O_D + 320, qs].rearrange("(h p) t -> p h t", p=64), ostg[:], reads=[ostgb], writes=[OTb], semb=ostgb)
                mk.barrier(recycle=True)

        def attn_softmax_head_dsa(tiles, q, qb_, kT, kTb, vt, vtb, h, Q, mT, mTb, ostg, ostgb):
            class V:
                def __getitem__(self, idx):
                    return ostg[idx[0], h, :]
            attn_softmax_head(tiles, q, qb_, 0, kT, kTb, vt, vtb, h, Q,
                              lambda kb: (None, []),
                              lambda kb: (mT[:, kb, :], [mTb]),
                              V(), ostgb)

        def sbk(l):
            prev = mk.SAME_ENGINE_SYNC
            mk.SAME_ENGINE_SYNC = False
            try:
                _sbk(l)
            finally:
                mk.SAME_ENGINE_SYNC = prev

        def _sbk(l):
            with contextlib.ExitStack() as st:
                kT, kTb, vt, vtb = load_kv(st, R_SK, V_S, 5)
                qT, qTb = sb(st, "qT", [128, 3, S], BF16)
                mk.dma("sync", qT[:, 0:2, :], PJT[R_SQ:R_SQ + 256, :].rearrange("(c p) t -> p c t", p=128), reads=[PJb], writes=[qTb])
                mk.dma("sync", qT[0:64, 2, :], PJT[R_SQ + 256:R_SQ + 320, :], reads=[PJb], writes=[qTb])
                etr = Rot([sb(st, "et%d" % i, [128, 512], F32) for i in range(2)])
                ltr = Rot([sb(st, "lt%d" % i, [128, 512], BF16) for i in range(3)])
                t1r = Rot([sb(st, "st1%d" % i, [128, 512], F32) for i in range(3)])
                t2r = Rot([sb(st, "st2%d" % i, [128, 512], F32) for i in range(2)])
                atr = Rot([sb(st, "at%d" % i, [128, 512], BF16) for i in range(3)])
                rsr = Rot([sb(st, "rs%d" % i, [128, 512], F32) for i in range(2)])
                osr = Rot([sb(st, "ostg%d" % i, [64, S], BF16) for i in range(2)])
                rot5 = Rot(PS[0:4] + [PS[6]])
                for h in range(SB_H):
                    ck, pb = h // 2, (h % 2) * 64
                    ostg, ostgb = osr.next()
                    for Q in range(SB_NQ):
                        qs = slice(Q * 512, (Q + 1) * 512)
                        nkb = 4 * Q + 4
                        rs, rsb = rsr.next()
                        mk.op("gpsimd", lambda e: e.memset(rs[:], 0.0), writes=[rsb])
                        pO, pOb = PS[4 + (Q % 2)]
                        stA = {}

                        def stage_a(kb):
                            d = kb - 4 * Q
                            pz, pzb = rot5.next()
                            mk.op("tensor", lambda e: e.matmul(pz[:, :], lhsT=kT[pb:pb + 64, ck, kb * 128:(kb + 1) * 128],
                                                               rhs=qT[pb:pb + 64, ck, qs], start=True, stop=True),
                                  reads=[kTb, qTb], writes=[pzb], skip_same=True)
                            et, etb = etr.next()
                            lt, ltb = ltr.next()
                            mk.op("scalar", lambda e: e.activation(out=et[:], in_=pz[:, :], func=AF.Exp, scale=0.125), reads=[pzb], writes=[etb])
                            mk.op("scalar", lambda e: e.activation(out=lt[:], in_=et[:], func=AF.Ln, bias=1.0), reads=[etb], writes=[ltb])
                            if d >= 0:
                                mk.op("gpsimd", lambda e: e.tensor_tensor(out=lt[:], in0=lt[:], in1=smask[:, d * 512:(d + 1) * 512], op=ALU.mult),
                                      reads=[ltb, smaskb], writes=[ltb])
                            pc, pcb = rot5.next()
                            mk.op("tensor", lambda e: e.matmul(pc[:, :], lhsT=trige[:], rhs=lt[:], start=True, stop=True),
                                  reads=[trigeb, ltb], writes=[pcb], skip_same=True)
                            prt = None
                            if kb > 0:
                                pr, prb = rot5.next()
                                mk.op("tensor", lambda e: e.matmul(pr[:, :], lhsT=onesb[:], rhs=lt[:], start=True, stop=True),
                                      reads=[onesbb, ltb], writes=[prb], skip_same=True)
                                prt = (pr, prb)
                            t1, t1b = t1r.next()
                            mk.op("scalar", lambda e: e.activation(out=t1[:], in_=pz[:, :], func=AF.Identity, scale=0.125),
                                  reads=[pzb], writes=[t1b])
                            stA[kb] = (d, pc, pcb, prt, t1, t1b)

                        def stage_b(kb):
                            d, pc, pcb, prt, t1, t1b = stA.pop(kb)
                            mk.op("gpsimd", lambda e: e.tensor_tensor(out=t1[:], in0=t1[:], in1=rs[:], op=ALU.subtract),
                                  reads=[t1b, rsb], writes=[t1b])
                            t2, t2b = t2r.next()
                            mk.op("vector", lambda e: e.tensor_tensor(out=t2[:], in0=t1[:], in1=pc[:, :], op=ALU.subtract),
                                  reads=[t1b, pcb], writes=[t2b])
                            at, atb = atr.next()
                            mk.op("scalar", lambda e: e.activation(out=at[:], in_=t2[:], func=AF.Exp), reads=[t2b], writes=[atb])
                            if d >= 0:
                                mk.op("gpsimd", lambda e: e.tensor_tensor(out=at[:], in0=at[:], in1=smask[:, d * 512:(d + 1) * 512], op=ALU.mult),
                                      reads=[atb, smaskb], writes=[atb])
                            if prt is not None:
                                pr, prb = prt
                                mk.op("vector", lambda e: e.tensor_tensor(out=rs[:], in0=pr[:, :], in1=rs[:], op=ALU.add),
                                      reads=[rsb, prb], writes=[rsb])
                            mk.op("tensor", lambda e: e.matmul(pO[0:65, :], lhsT=vt[:, kb, h, 0:65], rhs=at[:],
                                                               start=(kb == nkb - 1), stop=(kb == 0)),
                                  reads=[vtb, atb], writes=[pOb], skip_same=True)

                        kbs = list(range(nkb - 1, -1, -1))
                        stage_a(kbs[0])
                        for i, kb in enumerate(kbs):
                            if i + 1 < len(kbs):
                                stage_a(kbs[i + 1])
                            stage_b(kb)
                        mk.op("scalar", lambda e: e.activation(out=ostg[0:64, qs], in_=pO[0:64, :], func=AF.Identity), reads=[pOb], writes=[ostgb])
                    mk.dma("sync", OT[O_S + h * 64:O_S + (h + 1) * 64, :], ostg[:], reads=[ostgb], writes=[OTb], semb=ostgb)
                mk.barrier(recycle=True)

        def merge(l):
            win = W["w_in"][l]
            with contextlib.ExitStack() as st:
                hT, hTb = sb(st, "hT2", [128, 8, S // 2], BF16)
                gw, gwb = sb(st, "gw", [128, 8, 3072], BF16)
                for b in range(3):
                    wchunk(win, C_G + b * 1024, 1024, gw[:, :, b * 1024:(b + 1) * 1024], gwb)
                wbo, wbob = sb(st, "wbo", [128, 9, D], BF16)
                mk.dma("gpsimd", wbo[:, 0:3, :], W["w_fox_out"][l].rearrange("(c p) n -> p c n", p=128), writes=[wbob])
                for b, nm in ((1, "w_dsa_out"), (2, "w_sb_out")):
                    mk.dma("gpsimd", wbo[:, 3 * b:3 * b + 2, :], W[nm][l][0:256, :].rearrange("(c p) n -> p c n", p=128), writes=[wbob])
                    mk.dma("gpsimd", wbo[0:64, 3 * b + 2, :], W[nm][l][256:320, :], writes=[wbob])
                wo, wob = sb(st, "wo", [128, 8, D], BF16)
                wchunk(W["w_out"][l], 0, D, wo[:], wob)
                oqr = Rot([sb(st, "oq%d" % i, [128, 9, 512], BF16) for i in range(1)])
                sgr = Rot([sb(st, "msg%d" % i, [128, 512], F32) for i in range(3)])
                tmr = Rot([sb(st, "mtm%d" % i, [128, 512], F32) for i in range(3)])
                mTr = Rot([sb(st, "mmT%d" % i, [128, 8, 512], BF16) for i in range(2)])
                xr = Rot([sb(st, "mx%d" % i, [128, D], F32) for i in range(2)])
                KK = [128, 128, 128, 128, 128, 64, 128, 128, 64]
                for Q in range(NQ):
                    if Q % 4 == 0:
                        norm_pass(st, W["mix_norm"][l:l + 1, :], hT, hTb, t0=(Q // 4) * 16, nt=16)
                    qs = slice(Q * 512, (Q + 1) * 512)
                    ql = slice((Q % 4) * 512, (Q % 4 + 1) * 512)
                    oq, oqb = oqr.next()
                    mk.dma("sync", oq[:, 0:5, :], OT[0:640, qs].rearrange("(c p) t -> p c t", p=128), reads=[OTb], writes=[oqb])
                    mk.dma("sync", oq[0:64, 5, :], OT[640:704, qs], reads=[OTb], writes=[oqb])
                    mk.dma("sync", oq[:, 6:8, :], OT[768:1024, qs].rearrange("(c p) t -> p c t", p=128), reads=[OTb], writes=[oqb])
                    mk.dma("sync", oq[0:64, 8, :], OT[1024:1088, qs], reads=[OTb], writes=[oqb])
                    mT, mTb = mTr.next()
                    for fc in range(8):
                        tms = []
                        for b in range(3):
                            pg, pgb = psrot.next()
                            for kc in range(8):
                                mk.op("tensor", lambda e: e.matmul(pg[:, :], lhsT=gw[:, kc, b * 1024 + fc * 128:b * 1024 + (fc + 1) * 128],
                                                                   rhs=hT[:, kc, ql], start=(kc == 0), stop=(kc == 7)),
                                      reads=[gwb, hTb], writes=[pgb], skip_same=True)
                            sg, sgb = sgr.next()
                            mk.op("scalar", lambda e: e.activation(out=sg[:], in_=pg[:, :], func=AF.Sigmoid), reads=[pgb], writes=[sgb])
                            pp, ppb = psrot.next()
                            for c in range(3):
                                kk = KK[3 * b + c]
                                mk.op("tensor", lambda e: e.matmul(pp[:, :], lhsT=wbo[0:kk, 3 * b + c, fc * 128:(fc + 1) * 128],
                                                                   rhs=oq[0:kk, 3 * b + c, :], start=(c == 0), stop=(c == 2)),
                                      reads=[wbob, oqb], writes=[ppb], skip_same=True)
                            tm, tmb = tmr.next()
                            mk.op("vector", lambda e: e.tensor_tensor(out=tm[:], in0=sg[:], in1=pp[:, :], op=ALU.mult),
                                  reads=[sgb, ppb], writes=[tmb])
                            tms.append((tm, tmb))
                        (a0, a0b), (a1, a1b), (a2, a2b) = tms
                        mk.op("gpsimd", lambda e: e.tensor_tensor(out=a0[:], in0=a0[:], in1=a1[:], op=ALU.add), reads=[a0b, a1b], writes=[a0b])
                        mk.op("gpsimd", lambda e: e.tensor_tensor(out=mT[:, fc, :], in0=a0[:], in1=a2[:], op=ALU.add),
                              reads=[a0b, a2b], writes=[mTb])
                    for sub in range(4):
                        t = Q * 4 + sub
                        xt, xtb = xr.next()
                        mk.dma("sync", xt[:], X[t * 128:(t + 1) * 128, :], reads=[Xb[t]], writes=[xtb])
                        for half in range(2):
                            ps, psb = psrot.next()
                            for kc in range(8):
                                mk.op("tensor", lambda e: e.matmul(ps[:, :], lhsT=mT[:, kc, sub * 128:(sub + 1) * 128],
                                                                   rhs=wo[:, kc, half * 512:(half + 1) * 512], start=(kc == 0), stop=(kc == 7)),
                                      reads=[mTb, wob], writes=[psb], skip_same=True)
                            mk.op("vector", lambda e: e.tensor_tensor(out=xt[:, half * 512:(half + 1) * 512], in0=ps[:, :],
                                                                      in1=xt[:, half * 512:(half + 1) * 512], op=ALU.add),
                                  reads=[psb, xtb], writes=[xtb])
                        mk.dma("sync", X[t * 128:(t + 1) * 128, :], xt[:], reads=[xtb], writes=[Xb[t]], semb=xtb)
                mk.barrier(recycle=True)

        def ca(l):
            with contextlib.ExitStack() as st:
                hT, hTb = sb(st, "hT", [128, 8, S], BF16)
                norm_pass(st, W["ca_norm"][l:l + 1, :], hT, hTb)
                load_gain(W["mem_norm"][l:l + 1, :])
                memT, memTb = sb(st, "memT", [128, 8, 256], BF16)
                mx, mxb = sb(st, "cmx", [128, D], F32)
                mh, mhb = sb(st, "cmh", [128, D], BF16)
                junk, junkb = sb(st, "cjunk", [128, D], BF16)
                for mb in range(2):
                    mk.dma("sync", mx[:], mem_in[mb * 128:(mb + 1) * 128, :], writes=[mxb])
                    rstd_of(mx, mxb, junk, junkb)
                    with mk.small():
                        mk.op("vector", lambda e: e.scalar_tensor_tensor(out=mh[:], in0=mx[:], scalar=small[:, 2:3], in1=gB[:],
                                                                         op0=ALU.mult, op1=ALU.mult), reads=[mxb, smallb, gBb], writes=[mhb])
                    for kc in range(8):
                        mk.op("tensor", lambda e: e.transpose(psT[:, kc * 128:(kc + 1) * 128], mh[:, kc * 128:(kc + 1) * 128], ident[:]),
                              reads=[mhb, identb], writes=[psTb], skip_same=True)
                    mk.op("scalar", lambda e: e.activation(out=memT[:, :, mb * 128:(mb + 1) * 128],
                                                           in_=psT[:, :].rearrange("p (k t) -> p k t", k=8), func=AF.Identity),
                          reads=[psTb], writes=[memTb])
                wkv, wkvb = sb(st, "wkv", [128, 8, D], BF16)
                wchunk(W["ca_w_kv"][l], 0, D, wkv[:], wkvb)
                wq, wqb = sb(st, "wq", [128, 8, 512], BF16)
                wchunk(W["ca_w_q"][l], 0, 512, wq[:], wqb)
                wo, wob = sb(st, "cwo", [128, 4, D], BF16)
                mk.dma("gpsimd", wo[:], W["ca_w_o"][l].rearrange("(c p) n -> p c n", p=128), writes=[wob])
                kTm, kTmb = sb(st, "kTm", [128, 4, 256], BF16)
                vm, vmb = sb(st, "vm", [128, 2, 512], BF16)
                for h in range(4):
                    ps, psb = psrot.next()
                    for kc in range(8):
                        mk.op("tensor", lambda e: e.matmul(ps[:, 0:256], lhsT=wkv[:, kc, h * 128:(h + 1) * 128], rhs=memT[:, kc, :],
                                                           start=(kc == 0), stop=(kc == 7)), reads=[wkvb, memTb], writes=[psb], skip_same=True)
                    mk.op("scalar", lambda e: e.activation(out=kTm[:, h, :], in_=ps[:, 0:256], func=AF.Identity), reads=[psb], writes=[kTmb])
                for mb in range(2):
                    ps, psb = psrot.next()
                    for kc in range(8):
                        mk.op("tensor", lambda e: e.matmul(ps[:, :], lhsT=memT[:, kc, mb * 128:(mb + 1) * 128], rhs=wkv[:, kc, 512:1024],
                                                           start=(kc == 0), stop=(kc == 7)), reads=[wkvb, memTb], writes=[psb], skip_same=True)
                    mk.op("scalar", lambda e: e.activation(out=vm[:, mb, :], in_=ps[:, :], func=AF.Identity), reads=[psb], writes=[vmb])
                qcr = Rot([sb(st, "qc%d" % i, [128, 512], BF16) for i in range(2)])
                ptr = Rot([sb(st, "cpt%d" % i, [128, 512], BF16) for i in range(3)])
                rdr = Rot([sb(st, "crd%d" % i, [128, 512], F32) for i in range(2)])
                ocr = Rot([sb(st, "oc%d" % i, [128, 4, 512], BF16) for i in range(2)])
                xr = Rot([sb(st, "cx%d" % i, [128, D], F32) for i in range(3)])
                sc_ = 128.0 ** -0.5
                for Q in range(NQ):
                    qs = slice(Q * 512, (Q + 1) * 512)
                    oc, ocb = ocr.next()
                    for h in range(4):
                        ps, psb = psrot.next()
                        for kc in range(8):
                            mk.op("tensor", lambda e: e.matmul(ps[:, :], lhsT=wq[:, kc, h * 128:(h + 1) * 128], rhs=hT[:, kc, qs],
                                                               start=(kc == 0), stop=(kc == 7)), reads=[wqb, hTb], writes=[psb], skip_same=True)
                        qc, qcb = qcr.next()
                        mk.op("scalar", lambda e: e.activation(out=qc[:], in_=ps[:, :], func=AF.Identity), reads=[psb], writes=[qcb])
                        pO, pOb = PS[4]
                        pD, pDb = PS[5]
                        for mb in range(2):
                            pS, pSb = psrot.next()
                            mk.op("tensor", lambda e: e.matmul(pS[:, :], lhsT=kTm[:, h, mb * 128:(mb + 1) * 128], rhs=qc[:], start=True, stop=True),
                                  reads=[kTmb, qcb], writes=[pSb], skip_same=True)
                            pt, ptb = ptr.next()
                            mk.op("scalar", lambda e: e.activation(out=pt[:], in_=pS[:, :], func=AF.Exp, scale=sc_), reads=[pSb], writes=[ptb])
                            mk.op("tensor", lambda e: e.matmul(pO[:, :], lhsT=vm[:, mb, h * 128:(h + 1) * 128], rhs=pt[:], start=(mb == 0), stop=(mb == 1)),
                                  reads=[vmb, ptb], writes=[pOb], skip_same=True)
                            mk.op("tensor", lambda e: e.matmul(pD[:, :], lhsT=onesb[:], rhs=pt[:], start=(mb == 0), stop=(mb == 1)),
                                  reads=[onesbb, ptb], writes=[pDb], skip_same=True)
                        rd, rdb = rdr.next()
                        mk.op("vector", lambda e: e.reciprocal(out=rd[:], in_=pD[:, :]), reads=[pDb], writes=[rdb])
                        mk.op("vector", lambda e: e.tensor_tensor(out=oc[:, h, :], in0=pO[:, :], in1=rd[:], op=ALU.mult),
                              reads=[pOb, rdb], writes=[ocb])
                    for sub in range(4):
                        t = Q * 4 + sub
                        xt, xtb = xr.next()
                        mk.dma("sync", xt[:], X[t * 128:(t + 1) * 128, :], reads=[Xb[t]], writes=[xtb])
                        for half in range(2):
                            ps, psb = psrot.next()
                            for h in range(4):
                                mk.op("tensor", lambda e: e.matmul(ps[:, :], lhsT=oc[:, h, sub * 128:(sub + 1) * 128],
                                                                   rhs=wo[:, h, half * 512:(half + 1) * 512], start=(h == 0), stop=(h == 3)),
                                      reads=[ocb, wob], writes=[psb], skip_same=True)
                            mk.op("vector", lambda e: e.tensor_tensor(out=xt[:, half * 512:(half + 1) * 512], in0=ps[:, :],
                                                                      in1=xt[:, half * 512:(half + 1) * 512], op=ALU.add),
                                  reads=[psb, xtb], writes=[xtb])
                        mk.dma("sync", X[t * 128:(t + 1) * 128, :], xt[:], reads=[xtb], writes=[Xb[t]], semb=xtb)
                mk.barrier(recycle=True)

        def final(raw):
            with contextlib.ExitStack() as st:
                xr = Rot([sb(st, "fx%d" % i, [128, D], F32) for i in range(3)])
                junk, junkb = sb(st, "fjunk", [128, D], BF16)
                if not raw:
                    load_gain(W["final_norm"][0:1, :])
                src = xstate["src"]
                outb = Buf("out")
                for t in range(NT):
                    xt, xtb = xr.next()
                    mk.dma("sync", xt[:], src[t * 128:(t + 1) * 128, :], reads=[Xb[t]], writes=[xtb])
                    if not raw:
                        rstd_of(xt, xtb, junk, junkb)
                        with mk.small():
                            mk.op("vector", lambda e: e.scalar_tensor_tensor(out=xt[:], in0=xt[:], scalar=small[:, 2:3], in1=gB[:],
                                                                             op0=ALU.mult, op1=ALU.mult),
                                  reads=[xtb, smallb, gBb], writes=[xtb])
                    mk.dma("sync", out_d[t * 128:(t + 1) * 128, :], xt[:], reads=[xtb], writes=[outb], semb=xtb)
                mk.barrier(recycle=True)

        for l in range(n_layers):
            if "ffn1" in stages:
                ffn(l, 1)
            if "proj" in stages:
                proj(l)
            if "fox" in stages:
                fox(l)
            if "dsa" in stages:
                dsa(l)
            if "sb" in stages:
                sbk(l)
            if "merge" in stages:
                merge(l)
            if "ca" in stages:
                ca(l)
            if "ffn2" in stages:
                ffn(l, 2)
        if not attn_probe:
            final(raw=debug)
        else:
            mk.barrier()
    return nc


def make_in_maps(inputs, n_layers=4):
    consts = host_consts()
    maps = []
    shared = {}
    for name, shp in WSPEC:
        shared[name] = np.ascontiguousarray(np.asarray(inputs[name], dtype=np.float32).reshape(shp)[:n_layers])
    for k, v in consts.items():
        shared["c_" + k] = np.ascontiguousarray(v.astype(np.float32))
    for c in range(8):
        b = c // 2
        m = dict(shared)
        m["x"] = np.ascontiguousarray(np.asarray(inputs["x"][b], dtype=np.float32))
        m["mem"] = np.ascontiguousarray(np.asarray(inputs["mem"][b], dtype=np.float32))
        m["pos"] = np.ascontiguousarray(np.asarray(inputs["positions"][b], dtype=np.int32).reshape(1, S))
        maps.append(m)
    return maps


def kernel(**inputs):
    nc = build()
    res = run_bass_kernel_spmd(nc, make_in_maps(inputs), core_ids=list(range(8)))
    out = np.stack([np.asarray(res.results[2 * b]["out"], dtype=np.float32) for b in range(4)], axis=0)
    return out
```

```python
import contextlib
import math
import numpy as np
import concourse.bass as bass
import concourse.mybir as mybir
from concourse.bass_utils import run_bass_kernel_spmd

F32 = mybir.dt.float32
BF16 = mybir.dt.bfloat16
I32 = mybir.dt.int32
ALU = mybir.AluOpType
AF = mybir.ActivationFunctionType

S = 4096
D = 1024
NT = 32
NQ = 8
DFF = 2816
NJ = 22
DIN = 6734
NEG = -1.0e30
TOPK_W = 128.0
TOPK_IT = 28
SB_H = 5
SB_NQ = 8
SB_DBG = 5
SB_V = 'bc'
C_FQ, C_FK, C_FV, C_FF = 0, 384, 768, 1152
C_DQ, C_DK, C_DV = 1158, 1478, 1798
C_IQ, C_IK, C_IW = 2118, 2630, 2694
C_SQ, C_SK, C_SV = 2702, 3022, 3342
C_G = 3662
R_FQ, R_FK, R_DQ, R_DK, R_IQ, R_IK, R_SQ, R_SK = 0, 384, 768, 1088, 1408, 1920, 1984, 2304
PJ_ROWS = 2624
V_F, V_D, V_S = 0, 390, 715
V_COLS = 1040
O_F, O_D, O_S = 0, 384, 768
O_ROWS = 1152


class Sem:
    __slots__ = ("h", "count", "is_dma")

    def __init__(self, h, is_dma):
        self.h = h
        self.count = 0
        self.is_dma = is_dma


class Buf:
    __slots__ = ("name", "w", "r", "dsem")

    def __init__(self, name):
        self.name = name
        self.w = None
        self.r = {}
        self.dsem = None


class MK:
    SAME_ENGINE_SYNC = False

    @contextlib.contextmanager
    def small(self):
        prev = self.SAME_ENGINE_SYNC
        self.SAME_ENGINE_SYNC = True
        try:
            yield
        finally:
            self.SAME_ENGINE_SYNC = prev

    def __init__(self, nc, es):
        self.nc = nc
        self.es = es
        self.eng = {}
        self.esem = {}
        self.waited = {}
        self.dsems = []
        self.phase_sems = []
        self.free_sems = []
        self.uid = 0
        for name in ("tensor", "vector", "scalar", "gpsimd", "sync"):
            self.eng[name] = getattr(nc, name)
            self.esem[name] = Sem(es.enter_context(nc.semaphore("s_" + name)), False)
            self.waited[name] = {}

    def buf(self, name="b"):
        return Buf(name)

    def _dsem(self, b):
        if b.dsem is None:
            if self.free_sems:
                b.dsem = self.free_sems.pop()
            else:
                self.uid += 1
                b.dsem = Sem(self.es.enter_context(self.nc.semaphore("d%d" % self.uid)), True)
                self.dsems.append(b.dsem)
            self.phase_sems.append(b.dsem)
        return b.dsem

    def keep(self):
        self.phase_sems = []

    def _waits(self, en, reads, writes, skip_same):
        need = {}
        for b in reads:
            if b.w is not None:
                s, v = b.w
                if need.get(s, 0) < v:
                    need[s] = v
        for b in writes:
            if b.w is not None:
                s, v = b.w
                if need.get(s, 0) < v:
                    need[s] = v
            for s, v in b.r.items():
                if need.get(s, 0) < v:
                    need[s] = v
        wd = self.waited[en]
        own = self.esem[en]
        for s, v in need.items():
            if s.is_dma:
                v = s.count
            if s is own and (skip_same or not self.SAME_ENGINE_SYNC):
                continue
            if wd.get(s, 0) >= v:
                continue
            self.eng[en].wait_ge(s.h, v)
            wd[s] = v

    def op(self, en, fn, reads=(), writes=(), skip_same=False):
        self._waits(en, reads, writes, skip_same)
        inst = fn(self.eng[en])
        s = self.esem[en]
        s.count += 1
        inst.then_inc(s.h, 1)
        for b in reads:
            b.r[s] = s.count
        ev = (s, s.count)
        for b in writes:
            b.w = ev
            b.r = {}
        return inst

    def dma(self, q, out, in_, reads=(), writes=(), semb=None):
        self._waits(q, reads, writes, True)
        if semb is None:
            semb = (list(writes) + list(reads))[0]
        s = self._dsem(semb)
        inst = self.eng[q].dma_start(out=out, in_=in_)
        s.count += 16
        inst.then_inc(s.h, 16)
        for b in reads:
            b.r[s] = s.count
        ev = (s, s.count)
        for b in writes:
            b.w = ev
            b.r = {}
        return inst

    def barrier(self, recycle=False):
        if recycle:
            self.free_sems.extend(self.phase_sems)
            self.phase_sems = []
        for en in self.eng:
            wd = self.waited[en]
            for on, s in self.esem.items():
                if on == en or s.count == 0:
                    continue
                if wd.get(s, 0) < s.count:
                    self.eng[en].wait_ge(s.h, s.count)
                    wd[s] = s.count
            for s in self.dsems:
                if s.count and wd.get(s, 0) < s.count:
                    self.eng[en].wait_ge(s.h, s.count)
                    wd[s] = s.count


class Rot:
    def __init__(self, items):
        self.items = items
        self.i = 0

    def next(self):
        it = self.items[self.i % len(self.items)]
        self.i += 1
        return it


def host_consts():
    c = {}
    p = np.arange(128)[:, None]
    f = np.arange(128)[None, :]
    c["ident"] = (p == f).astype(np.float32)
    rot = np.zeros((128, 128), np.float32)
    for m in range(128):
        if (m % 64) < 32:
            rot[m + 32, m] = -1.0
        else:
            rot[m - 32, m] = 1.0
    c["rot"] = rot
    c["trile"] = (p <= f).astype(np.float32)
    c["trige"] = (p >= f).astype(np.float32)
    ff = np.arange(512)[None, None, :]
    dd = np.arange(4)[None, :, None]
    pp = np.arange(128)[:, None, None]
    c["cmask"] = ((128 * dd + pp) <= ff).astype(np.float32).reshape(128, 2048)
    c["smask"] = ((128 * dd + pp) < ff).astype(np.float32).reshape(128, 2048)
    c["negm"] = np.where(f <= p, 0.0, NEG).astype(np.float32)
    c["invf"] = (10000.0 ** (-(np.arange(128) % 32).astype(np.float64) / 32.0)).astype(np.float32).reshape(128, 1)
    return c


CONST_SHAPES = {"ident": 128, "rot": 128, "trile": 128, "trige": 128, "cmask": 2048, "smask": 2048,
                "negm": 128, "invf": 1}

WSPEC = [("ffn1_norm", [4, 1024]), ("ffn1_w_gu", [4, 1024, 5632]), ("ffn1_w_down", [4, 2816, 1024]),
         ("mix_norm", [4, 1024]), ("w_in", [4, 1024, 6734]), ("b_fgate", [4, 6]),
         ("w_fox_out", [4, 384, 1024]), ("w_dsa_out", [4, 320, 1024]), ("w_sb_out", [4, 320, 1024]),
         ("w_out", [4, 1024, 1024]), ("ca_norm", [4, 1024]), ("mem_norm", [4, 1024]),
         ("ca_w_q", [4, 1024, 512]), ("ca_w_kv", [4, 1024, 1024]), ("ca_w_o", [4, 512, 1024]),
         ("ffn2_norm", [4, 1024]), ("ffn2_w_gu", [4, 1024, 5632]), ("ffn2_w_down", [4, 2816, 1024]),
         ("final_norm", [1, 1024])]


def build(n_layers=4, stages=("ffn1", "proj", "fox", "dsa", "sb", "merge", "ca", "ffn2"), debug=False, attn_probe=False):
    nc = bass.Bass("TRN2", target_bir_lowering=False)
    x_in = nc.dram_tensor("x", [S, D], F32, kind="ExternalInput").ap()
    mem_in = nc.dram_tensor("mem", [256, D], F32, kind="ExternalInput").ap()
    pos_in = nc.dram_tensor("pos", [1, S], I32, kind="ExternalInput").ap()
    W = {}
    for name, shp in ([] if attn_probe else WSPEC):
        shp = [min(shp[0], n_layers)] + list(shp[1:])
        W[name] = nc.dram_tensor(name, shp, F32, kind="ExternalInput").ap()
    CD = {}
    for name, n in CONST_SHAPES.items():
        CD[name] = nc.dram_tensor("c_" + name, [128, n], F32, kind="ExternalInput").ap()
    out_d = nc.dram_tensor("out", [S, D], F32, kind="ExternalOutput").ap()
    dbgk = "ExternalOutput" if debug else "Internal"
    X = nc.dram_tensor("Xs", [S, D], F32, kind="Internal").ap()
    AT = nc.dram_tensor("ATs", [DFF, S], BF16, kind="Internal").ap()
    PJT = nc.dram_tensor("PJTs", [PJ_ROWS, S], BF16, kind=("ExternalInput" if attn_probe else dbgk)).ap()
    VT = nc.dram_tensor("VTs", [S, V_COLS], BF16, kind=("ExternalInput" if attn_probe else "Internal")).ap()
    OT = nc.dram_tensor("OTs", [O_ROWS, S], BF16, kind=dbgk).ap()

    with contextlib.ExitStack() as es:
        mk = MK(nc, es)

        uid = [0]

        def sb(stack, name, shape, dt):
            uid[0] += 1
            t = stack.enter_context(nc.sbuf_tensor("%s_%d" % (name, uid[0]), shape, dt))
            return t, Buf(name)

        Xb = [Buf("X%d" % t) for t in range(NT)]
        ATb = Buf("AT")
        PJb = Buf("PJT")
        VTb = Buf("VT")
        OTb = Buf("OT")
        xstate = {"src": x_in}

        PS = []
        for i in range(7):
            PS.append((es.enter_context(nc.psum_tensor("ps%d" % i, [128, 512], F32)), Buf("ps%d" % i)))
        psT, psTb = es.enter_context(nc.psum_tensor("psT", [128, 1024], BF16)), Buf("psT")

        def cload(name, dt):
            n = CONST_SHAPES[name]
            t, b = sb(es, "k_" + name, [128, n], dt)
            mk.dma("gpsimd", t[:], CD[name][:, :], writes=[b])
            return t, b

        ident, identb = cload("ident", BF16)
        rotm, rotb = cload("rot", BF16)
        trile, trileb = cload("trile", F32)
        trige, trigeb = cload("trige", BF16)
        cmask, cmaskb = cload("cmask", BF16)
        smask, smaskb = cload("smask", BF16)
        negm, negmb = cload("negm", F32)
        invf, invfb = cload("invf", F32)
        onesb, onesbb = sb(es, "onesb", [128, 128], BF16)
        onesf, onesfb = sb(es, "onesf", [128, 128], F32)
        mk.op("vector", lambda e: e.memset(onesb[:], 1.0), writes=[onesbb])
        mk.op("vector", lambda e: e.memset(onesf[:], 1.0), writes=[onesfb])
        Cc, Ccb = sb(es, "Cc", [128, NT, 6], F32)
        Ee, Eeb = sb(es, "Ee", [128, NT, 6], F32)
        widx, widxb = sb(es, "widx", [128, NT, 8], F32)
        gB, gBb = sb(es, "gB", [128, D], F32)
        small, smallb = sb(es, "small", [128, 8], F32)
        mk.keep()

        def rstd_of(xt, xtb, junk, junkb):
            with mk.small():
                mk.op("scalar", lambda e: e.activation(out=junk[:], in_=xt[:], func=AF.Square, accum_out=small[:, 0:1]),
                      reads=[xtb], writes=[junkb, smallb])
                mk.op("vector", lambda e: e.tensor_scalar(out=small[:, 1:2], in0=small[:, 0:1], scalar1=1.0 / D, scalar2=1e-6,
                                                          op0=ALU.mult, op1=ALU.add), reads=[smallb], writes=[smallb])
                mk.op("scalar", lambda e: e.activation(out=small[:, 1:2], in_=small[:, 1:2], func=AF.Sqrt),
                      reads=[smallb], writes=[smallb])
                mk.op("vector", lambda e: e.reciprocal(out=small[:, 2:3], in_=small[:, 1:2]), reads=[smallb], writes=[smallb])

        def load_gain(gain_ap):
            mk.dma("sync", gB[:], gain_ap.to_broadcast([128, D]), writes=[gBb])

        def norm_pass(st_unused, gain_ap, hT, hTb, t0=0, nt=NT):
            load_gain(gain_ap)
            with contextlib.ExitStack() as st:
                xr = Rot([sb(st, "nx%d" % i, [128, D], F32) for i in range(3)])
                hn = Rot([sb(st, "nh%d" % i, [128, D], BF16) for i in range(2)])
                junk, junkb = sb(st, "njunk", [128, D], BF16)
                src = xstate["src"]
                for t in range(t0, t0 + nt):
                    xt, xtb = xr.next()
                    mk.dma("sync", xt[:], src[t * 128:(t + 1) * 128, :], reads=[Xb[t]], writes=[xtb])
                    rstd_of(xt, xtb, junk, junkb)
                    h, hb = hn.next()
                    with mk.small():
                        mk.op("vector", lambda e: e.scalar_tensor_tensor(out=h[:], in0=xt[:], scalar=small[:, 2:3], in1=gB[:],
                                                                         op0=ALU.mult, op1=ALU.mult),
                              reads=[xtb, smallb, gBb], writes=[hb])
                    for kc in range(8):
                        mk.op("tensor", lambda e: e.transpose(psT[:, kc * 128:(kc + 1) * 128], h[:, kc * 128:(kc + 1) * 128], ident[:]),
                              reads=[hb, identb], writes=[psTb], skip_same=True)
                    mk.op("scalar", lambda e: e.activation(out=hT[:, :, (t - t0) * 128:(t - t0 + 1) * 128],
                                                           in_=psT[:, :].rearrange("p (k t) -> p k t", k=8), func=AF.Identity),
                          reads=[psTb], writes=[hTb])
                mk.barrier()

        def wchunk(w2d, c0, n, dst, dstb, kcs=8, q="gpsimd"):
            mk.dma(q, dst, w2d[:, c0:c0 + n].rearrange("(k p) n -> p k n", p=128), writes=[dstb])

        psrot = Rot(PS[0:4])

        def ffn(l, which):
            gain = W["ffn%d_norm" % which][l:l + 1, :]
            wgu = W["ffn%d_w_gu" % which][l]
            wdn = W["ffn%d_w_down" % which][l]
            with contextlib.ExitStack() as st:
                hT, hTb = sb(st, "hT", [128, 8, S], BF16)
                norm_pass(st, gain, hT, hTb)
                wr = Rot([sb(st, "wgu%d" % i, [128, 2, 8, 128], BF16) for i in range(3)])
                ar = Rot([sb(st, "aTj%d" % i, [128, S], BF16) for i in range(2)])
                sr = Rot([sb(st, "sg%d" % i, [128, 512], F32) for i in range(2)])
                wl = []

                def loadw(j):
                    w, wb = wr.next()
                    wchunk(wgu, j * 128, 128, w[:, 0], wb)
                    mk.dma("gpsimd", w[:, 1], wgu[:, DFF + j * 128:DFF + (j + 1) * 128].rearrange("(k p) n -> p k n", p=128),
                           writes=[wb])
                    wl.append((w, wb))
                loadw(0)
                loadw(1)
                for j in range(NJ):
                    if j + 2 < NJ:
                        loadw(j + 2)
                    w, wb = wl[j]
                    a, ab = ar.next()
                    for Q in range(NQ):
                        pg, pgb = psrot.next()
                        pu, pub = psrot.next()
                        for kc in range(8):
                            mk.op("tensor", lambda e: e.matmul(pg[:, :], lhsT=w[:, 0, kc, :], rhs=hT[:, kc, Q * 512:(Q + 1) * 512],
                                                               start=(kc == 0), stop=(kc == 7)),
                                  reads=[wb, hTb], writes=[pgb], skip_same=True)
                        for kc in range(8):
                            mk.op("tensor", lambda e: e.matmul(pu[:, :], lhsT=w[:, 1, kc, :], rhs=hT[:, kc, Q * 512:(Q + 1) * 512],
                                                               start=(kc == 0), stop=(kc == 7)),
                                  reads=[wb, hTb], writes=[pub], skip_same=True)
                        sg, sgb = sr.next()
                        mk.op("scalar", lambda e: e.activation(out=sg[:], in_=pg[:, :], func=AF.Silu), reads=[pgb], writes=[sgb])
                        mk.op("vector", lambda e: e.tensor_tensor(out=a[:, Q * 512:(Q + 1) * 512], in0=sg[:], in1=pu[:, :], op=ALU.mult),
                              reads=[sgb, pub], writes=[ab])
                    mk.dma("sync", AT[j * 128:(j + 1) * 128, :], a[:], reads=[ab], writes=[ATb], semb=ab)
                mk.barrier(recycle=True)
            with contextlib.ExitStack() as st:
                wd, wdb = sb(st, "wd", [128, NJ, D], BF16)
                for j in range(NJ):
                    mk.dma("gpsimd", wd[:, j, :], wdn[j * 128:(j + 1) * 128, :], writes=[wdb])
                aq = Rot([sb(st, "aq%d" % i, [128, NJ, 512], BF16) for i in range(2)])
                xr = Rot([sb(st, "dx%d" % i, [128, D], F32) for i in range(3)])
                src = xstate["src"]
                for Q in range(NQ):
                    a, ab = aq.next()
                    mk.dma("sync", a[:], AT[:, Q * 512:(Q + 1) * 512].rearrange("(j p) t -> p j t", p=128), reads=[ATb], writes=[ab])
                    for sub in range(4):
                        t = Q * 4 + sub
                        xt, xtb = xr.next()
                        mk.dma("sync", xt[:], src[t * 128:(t + 1) * 128, :], reads=[Xb[t]], writes=[xtb])
                        for half in range(2):
                            ps, psb = psrot.next()
                            for j in range(NJ):
                                mk.op("tensor", lambda e: e.matmul(ps[:, :], lhsT=a[:, j, sub * 128:(sub + 1) * 128],
                                                                   rhs=wd[:, j, half * 512:(half + 1) * 512],
                                                                   start=(j == 0), stop=(j == NJ - 1)),
                                      reads=[ab, wdb], writes=[psb], skip_same=True)
                            mk.op("vector", lambda e: e.scalar_tensor_tensor(out=xt[:, half * 512:(half + 1) * 512], in0=ps[:, :], scalar=0.5,
                                                                             in1=xt[:, half * 512:(half + 1) * 512],
                                                                             op0=ALU.mult, op1=ALU.add),
                                  reads=[psb, xtb], writes=[xtb])
                        mk.dma("sync", X[t * 128:(t + 1) * 128, :], xt[:], reads=[xtb], writes=[Xb[t]], semb=xtb)
                xstate["src"] = X
                mk.barrier(recycle=True)

        def proj(l):
            win = W["w_in"][l]
            with contextlib.ExitStack() as st:
                hT, hTb = sb(st, "hT", [128, 8, S], BF16)
                norm_pass(st, W["mix_norm"][l:l + 1, :], hT, hTb)
                cosT, cosb = sb(st, "cosT", [128, S], F32)
                sinT, sinb = sb(st, "sinT", [128, S], F32)
                st_r = contextlib.ExitStack()
                ang, angb = sb(st_r, "ang", [128, S], F32)
                tmpf, tmpfb = sb(st_r, "tmpf", [128, S], F32)
                posi, posib = sb(st_r, "posi", [128, S], I32)
                with mk.small():
                    mk.dma("sync", posi[:], pos_in.to_broadcast([128, S]), writes=[posib])
                    mk.op("vector", lambda e: e.tensor_copy(out=ang[:], in_=posi[:]), reads=[posib], writes=[angb])
                    mk.op("vector", lambda e: e.tensor_scalar(out=ang[:], in0=ang[:], scalar1=invf[:, 0:1], scalar2=None, op0=ALU.mult),
                          reads=[angb, invfb], writes=[angb])
                    TWO_PI = 2.0 * math.pi
                    C1 = 6.28125
                    C2 = TWO_PI - C1
                    for (dst, dstb, shift) in ((sinT, sinb, 0.0), (cosT, cosb, math.pi / 2)):
                        mk.op("vector", lambda e: e.tensor_scalar(out=tmpf[:], in0=ang[:], scalar1=shift, scalar2=1.0 / TWO_PI,
                                                                  op0=ALU.add, op1=ALU.mult), reads=[angb], writes=[tmpfb])
                        mk.op("vector", lambda e: e.tensor_copy(out=posi[:], in_=tmpf[:]), reads=[tmpfb], writes=[posib])
                        mk.op("vector", lambda e: e.tensor_copy(out=tmpf[:], in_=posi[:]), reads=[posib], writes=[tmpfb])
                        mk.op("vector", lambda e: e.scalar_tensor_tensor(out=dst[:], in0=tmpf[:], scalar=-C1, in1=ang[:],
                                                                         op0=ALU.mult, op1=ALU.add), reads=[tmpfb, angb], writes=[dstb])
                        mk.op("vector", lambda e: e.scalar_tensor_tensor(out=dst[:], in0=tmpf[:], scalar=-C2, in1=dst[:],
                                                                         op0=ALU.mult, op1=ALU.add), reads=[tmpfb, dstb], writes=[dstb])
                        mk.op("vector", lambda e: e.tensor_scalar(out=dst[:], in0=dst[:], scalar1=shift, scalar2=math.pi,
                                                                  op0=ALU.add, op1=ALU.min), reads=[dstb], writes=[dstb])
                        mk.op("vector", lambda e: e.tensor_scalar(out=dst[:], in0=dst[:], scalar1=-math.pi, scalar2=None,
                                                                  op0=ALU.max), reads=[dstb], writes=[dstb])
                        mk.op("scalar", lambda e: e.activation(out=dst[:], in_=dst[:], func=AF.Sin), reads=[dstb], writes=[dstb])
                mk.barrier()
                st_r.close()
                chunks = []
                def add_group(c0, r0, width, rope):
                    o = 0
                    while o < width:
                        m = min(128, width - o)
                        chunks.append((c0 + o, r0 + o, m, rope))
                        o += m
                add_group(C_FQ, R_FQ, 384, False)
                add_group(C_FK, R_FK, 384, False)
                add_group(C_DQ, R_DQ, 320, True)
                add_group(C_DK, R_DK, 320, True)
                add_group(C_IQ, R_IQ, 512, True)
                add_group(C_IK, R_IK, 64, True)
                add_group(C_SQ, R_SQ, 320, False)
                add_group(C_SK, R_SK, 320, False)
                wr = Rot([sb(st, "pw%d" % i, [128, 8, 128], BF16) for i in range(3)])
                sr = Rot([sb(st, "pst%d" % i, [128, S], BF16) for i in range(2)])
                xbr = Rot([sb(st, "pxb%d" % i, [128, 512], BF16) for i in range(2)])
                t1r = Rot([sb(st, "pt1%d" % i, [128, 512], F32) for i in range(2)])
                t2r = Rot([sb(st, "pt2%d" % i, [128, 512], F32) for i in range(2)])
                wl = []

                def loadw(i):
                    c0, r0, m, rope = chunks[i]
                    w, wb = wr.next()
                    wchunk(win, c0, m, w[:, :, 0:m], wb)
                    wl.append((w, wb))
                loadw(0)
                loadw(1)
                for i, (c0, r0, m, rope) in enumerate(chunks):
                    if i + 2 < len(chunks):
                        loadw(i + 2)
                    w, wb = wl[i]
                    stg, stgb = sr.next()
                    for Q in range(NQ):
                        qs = slice(Q * 512, (Q + 1) * 512)
                        ps, psb = psrot.next()
                        for kc in range(8):
                            mk.op("tensor", lambda e: e.matmul(ps[0:m, :], lhsT=w[:, kc, 0:m], rhs=hT[:, kc, qs],
                                                               start=(kc == 0), stop=(kc == 7)),
                                  reads=[wb, hTb], writes=[psb], skip_same=True)
                        if not rope:
                            mk.op("scalar", lambda e: e.activation(out=stg[0:m, qs], in_=ps[0:m, :], func=AF.Identity),
                                  reads=[psb], writes=[stgb])
                        else:
                            xb_, xbb = xbr.next()
                            mk.op("scalar", lambda e: e.activation(out=xb_[0:m, :], in_=ps[0:m, :], func=AF.Identity),
                                  reads=[psb], writes=[xbb])
                            pr, prb = psrot.next()
                            mk.op("tensor", lambda e: e.matmul(pr[0:m, :], lhsT=rotm[0:m, 0:m], rhs=xb_[0:m, :], start=True, stop=True),
                                  reads=[rotb, xbb], writes=[prb], skip_same=True)
                            t1, t1b = t1r.next()
                            t2, t2b = t2r.next()
                            mk.op("gpsimd", lambda e: e.tensor_tensor(out=t1[0:m, :], in0=xb_[0:m, :], in1=cosT[0:m, qs], op=ALU.mult),
                                  reads=[xbb, cosb], writes=[t1b])
                            mk.op("vector", lambda e: e.tensor_tensor(out=t2[0:m, :], in0=pr[0:m, :], in1=sinT[0:m, qs], op=ALU.mult),
                                  reads=[prb, sinb], writes=[t2b])
                            mk.op("gpsimd", lambda e: e.tensor_tensor(out=stg[0:m, qs], in0=t1[0:m, :], in1=t2[0:m, :], op=ALU.add),
                                  reads=[t1b, t2b], writes=[stgb])
                    mk.dma("sync", PJT[r0:r0 + m, :], stg[0:m, :], reads=[stgb], writes=[PJb], semb=stgb)
                wv, wvb = sb(st, "wv", [128, 8, 1024], BF16)
                wchunk(win, C_FV, 384, wv[:, :, 0:384], wvb)
                wchunk(win, C_DV, 320, wv[:, :, 384:704], wvb)
                wchunk(win, C_SV, 320, wv[:, :, 704:1024], wvb)
                wf, wfb = sb(st, "wf", [128, 8, 16], BF16)
                mk.op("vector", lambda e: e.memset(wf[:], 0.0), writes=[wfb])
                wchunk(win, C_FF, 6, wf[:, :, 0:6], wfb)
                wchunk(win, C_IW, 8, wf[:, :, 8:16], wfb)
                vst = Rot([sb(st, "vst%d" % i, [128, 16, 65], BF16) for i in range(2)])
                fw, fwb = sb(st, "fw", [128, NT, 16], F32)
                for t in range(NT):
                    ts_ = slice(t * 128, (t + 1) * 128)
                    v, vb = vst.next()
                    mk.op("gpsimd", lambda e: e.memset(v[:], 1.0), writes=[vb])
                    for half in range(2):
                        ps, psb = psrot.next()
                        for kc in range(8):
                            mk.op("tensor", lambda e: e.matmul(ps[:, :], lhsT=hT[:, kc, ts_], rhs=wv[:, kc, half * 512:(half + 1) * 512],
                                                               start=(kc == 0), stop=(kc == 7)),
                                  reads=[hTb, wvb], writes=[psb], skip_same=True)
                        mk.op("scalar", lambda e: e.activation(out=v[:, half * 8:(half + 1) * 8, 0:64],
                                                               in_=ps[:, :].rearrange("p (h d) -> p h d", h=8), func=AF.Identity),
                              reads=[psb], writes=[vb])
                    mk.dma("sync", VT[ts_, :], v[:].rearrange("p h d -> p (h d)"), reads=[vb], writes=[VTb], semb=vb)
                    ps, psb = psrot.next()
                    for kc in range(8):
                        mk.op("tensor", lambda e: e.matmul(ps[:, 0:16], lhsT=hT[:, kc, ts_], rhs=wf[:, kc, :],
                                                           start=(kc == 0), stop=(kc == 7)),
                              reads=[hTb, wfb], writes=[psb], skip_same=True)
                    mk.op("vector", lambda e: e.tensor_copy(out=fw[:, t, :], in_=ps[:, 0:16]), reads=[psb], writes=[fwb])
                with mk.small():
                    bt, btb = sb(st, "bt", [128, 6], F32)
                    mk.dma("sync", bt[:], W["b_fgate"][l:l + 1, :].to_broadcast([128, 6]), writes=[btb])
                    lf, lfb = sb(st, "lf", [128, NT, 6], F32)
                    for h in range(6):
                        mk.op("vector", lambda e: e.tensor_scalar(out=lf[:, :, h], in0=fw[:, :, h], scalar1=bt[:, h:h + 1], scalar2=None,
                                                                  op0=ALU.add), reads=[fwb, btb], writes=[lfb])
                    mk.op("scalar", lambda e: e.activation(out=lf[:], in_=lf[:], func=AF.Exp, scale=-1.0), reads=[lfb], writes=[lfb])
                    mk.op("scalar", lambda e: e.activation(out=lf[:], in_=lf[:], func=AF.Ln, bias=1.0), reads=[lfb], writes=[lfb])
                    p1, p1b = psrot.next()
                    p2, p2b = psrot.next()
                    lf2 = lf[:].rearrange("p t h -> p (t h)")
                    mk.op("tensor", lambda e: e.matmul(p1[:, 0:192], lhsT=trile[:], rhs=lf2, start=True, stop=True),
                          reads=[trileb, lfb], writes=[p1b], skip_same=True)
                    mk.op("tensor", lambda e: e.matmul(p2[:, 0:192], lhsT=onesf[:], rhs=lf2, start=True, stop=True),
                          reads=[onesfb, lfb], writes=[p2b], skip_same=True)
                    tot, totb = sb(st, "tot", [128, NT, 6], F32)
                    mk.op("vector", lambda e: e.tensor_copy(out=tot[:].rearrange("p t h -> p (t h)"), in_=p2[:, 0:192]),
                          reads=[p2b], writes=[totb])
                    mk.op("vector", lambda e: e.tensor_copy(out=Ee[:, 0, :], in_=tot[:, 0, :]), reads=[totb], writes=[Eeb])
                    for t in range(1, NT):
                        mk.op("vector", lambda e: e.tensor_tensor(out=Ee[:, t, :], in0=Ee[:, t - 1, :], in1=tot[:, t, :], op=ALU.add),
                              reads=[Eeb, totb], writes=[Eeb])
                    mk.op("vector", lambda e: e.tensor_tensor(out=Cc[:].rearrange("p t h -> p (t h)"), in0=p1[:, 0:192],
                                                              in1=Ee[:].rearrange("p t h -> p (t h)"), op=ALU.add),
                          reads=[p1b, Eeb], writes=[Ccb])
                    mk.op("vector", lambda e: e.tensor_tensor(out=Cc[:], in0=Cc[:], in1=tot[:], op=ALU.subtract),
                          reads=[Ccb, totb], writes=[Ccb])
                    mk.op("vector", lambda e: e.tensor_scalar(out=widx[:], in0=fw[:, :, 8:16], scalar1=(8.0 ** -0.5) * (64.0 ** -0.5),
                                                              scalar2=None, op0=ALU.mult), reads=[fwb], writes=[widxb])
                mk.barrier(recycle=True)

        def load_kv(st, r_k, v_off, nh):
            kT, kTb = sb(st, "kT", [128, 3, S], BF16)
            nfull = (nh * 64) // 128
            if nfull:
                mk.dma("sync", kT[:, 0:nfull, :], PJT[r_k:r_k + nfull * 128, :].rearrange("(c p) t -> p c t", p=128),
                       reads=[PJb], writes=[kTb])
            if nh % 2:
                mk.dma("sync", kT[0:64, nfull, :], PJT[r_k + nfull * 128:r_k + nfull * 128 + 64, :], reads=[PJb], writes=[kTb])
            vt, vtb = sb(st, "vt", [128, NT, nh, 65], BF16)
            for g in range(4):
                mk.dma("sync", vt[:, g * 8:(g + 1) * 8].rearrange("p t h d -> p t (h d)"),
                       VT[g * 1024:(g + 1) * 1024, v_off:v_off + nh * 65].rearrange("(t p) c -> p t c", p=128),
                       reads=[VTb], writes=[vtb])
            return kT, kTb, vt, vtb

        def load_q(st, name, r_q, nh, Q=None):
            n = S if Q is None else 512
            cs = slice(0, S) if Q is None else slice(Q * 512, (Q + 1) * 512)
            return n, cs

        def attn_softmax_head(st_tiles, qT, qTb, qoff, kT, kTb, vt, vtb, h, Q, bias_fn, mask_fn, ostg, ostgb):
            ptr, rcs, nbs = st_tiles
            ck, pb = h // 2, (h % 2) * 64
            nkb = 4 * Q + 4
            pO, pOb = PS[4 + (Q % 2)]
            pss = [None] * nkb

            def issue_s(kb):
                ps, psb = psrot.next()
                mk.op("tensor", lambda e: e.matmul(ps[:, :], lhsT=kT[pb:pb + 64, ck, kb * 128:(kb + 1) * 128],
                                                   rhs=qT[pb:pb + 64, ck, qoff:qoff + 512], start=True, stop=True),
                      reads=[kTb, qTb], writes=[psb], skip_same=True)
                pss[kb] = (ps, psb)
            issue_s(0)
            for kb in range(nkb):
                if kb + 1 < nkb:
                    issue_s(kb + 1)
                ps, psb = pss[kb]
                pt, ptb = ptr.next()
                bias_ap, bias_bufs = bias_fn(kb)
                if bias_ap is None:
                    mk.op("scalar", lambda e: e.activation(out=pt[:], in_=ps[:, :], func=AF.Exp, scale=0.125), reads=[psb], writes=[ptb])
                else:
                    mk.op("scalar", lambda e: e.activation(out=pt[:], in_=ps[:, :], func=AF.Exp, scale=0.125, bias=bias_ap),
                          reads=[psb] + bias_bufs, writes=[ptb])
                m = mask_fn(kb)
                if m is not None:
                    m_ap, m_bufs = m
                    mk.op("gpsimd", lambda e: e.tensor_tensor(out=pt[:], in0=pt[:], in1=m_ap, op=ALU.mult),
                          reads=[ptb] + m_bufs, writes=[ptb])
                mk.op("tensor", lambda e: e.matmul(pO[0:65, :], lhsT=vt[:, kb, h, :], rhs=pt[:], start=(kb == 0), stop=(kb == nkb - 1)),
                      reads=[vtb, ptb], writes=[pOb], skip_same=True)
            rc, rcb = rcs.next()
            nb, nbb = nbs.next()
            mk.op("vector", lambda e: e.reciprocal(out=rc[64:65, :], in_=pO[64:65, :]), reads=[pOb], writes=[rcb])
            pB, pBb = PS[6]
            mk.op("tensor", lambda e: e.matmul(pB[0:64, :], lhsT=onesf[64:65, 0:64], rhs=rc[64:65, :], start=True, stop=True),
                  reads=[onesfb, rcb], writes=[pBb], skip_same=True)
            mk.op("scalar", lambda e: e.activation(out=nb[0:64, :], in_=pB[0:64, :], func=AF.Identity), reads=[pBb], writes=[nbb])
            mk.op("vector", lambda e: e.tensor_tensor(out=ostg[0:64, Q * 512:(Q + 1) * 512], in0=pO[0:64, :], in1=nb[0:64, :], op=ALU.mult),
                  reads=[pOb, nbb], writes=[ostgb])

        def attn_tiles(st):
            ptr = Rot([sb(st, "pt%d" % i, [128, 512], BF16) for i in range(4)])
            rcs = Rot([sb(st, "rc%d" % i, [128, 512], F32) for i in range(2)])
            nbs = Rot([sb(st, "nb%d" % i, [64, 512], F32) for i in range(2)])
            return ptr, rcs, nbs

        def fox(l):
            with contextlib.ExitStack() as st:
                kT, kTb, vt, vtb = load_kv(st, R_FK, V_F, 6)
                qT, qTb = sb(st, "qT", [128, 3, S], BF16)
                mk.dma("sync", qT[:], PJT[R_FQ:R_FQ + 384, :].rearrange("(c p) t -> p c t", p=128), reads=[PJb], writes=[qTb])
                tiles = attn_tiles(st)
                osr = Rot([sb(st, "ostg%d" % i, [64, S], BF16) for i in range(2)])
                bqr = Rot([sb(st, "bq%d" % i, [128, NT], F32) for i in range(2)])
                for h in range(6):
                    ostg, ostgb = osr.next()
                    for Q in range(NQ):
                        nkb = 4 * Q + 4
                        bq, bqb = bqr.next()
                        mk.op("vector", lambda e: e.tensor_scalar(out=bq[:, 0:nkb], in0=Cc[:, 0:nkb, h], scalar1=Ee[:, 4 * Q + 3, h:h + 1],
                                                                  scalar2=None, op0=ALU.subtract), reads=[Ccb, Eeb], writes=[bqb])
                        attn_softmax_head(tiles, qT, qTb, Q * 512, kT, kTb, vt, vtb, h, Q,
                                          lambda kb: (bq[:, kb:kb + 1], [bqb]),
                                          lambda kb: ((cmask[:, (kb - 4 * Q) * 512:(kb - 4 * Q + 1) * 512], [cmaskb]) if kb >= 4 * Q else None),
                                          ostg, ostgb)
                    mk.dma("sync", OT[O_F + h * 64:O_F + (h + 1) * 64, :], ostg[:], reads=[ostgb], writes=[OTb], semb=ostgb)
                mk.barrier(recycle=True)

        def dsa(l):
            with contextlib.ExitStack() as st:
                kT, kTb, vt, vtb = load_kv(st, R_DK, V_D, 5)
                kix, kixb = sb(st, "kix", [128, S], BF16)
                mk.dma("sync", kix[0:64, :], PJT[R_IK:R_IK + 64, :], reads=[PJb], writes=[kixb])
                mk.dma("sync", kix[64:128, :], PJT[R_IK:R_IK + 64, :], reads=[PJb], writes=[kixb])
                tiles = attn_tiles(st)
                qr = Rot([sb(st, "dq%d" % i, [128, 3, 512], BF16) for i in range(2)])
                qir = Rot([sb(st, "dqi%d" % i, [128, 4, 512], BF16) for i in range(2)])
                sc, scb = sb(st, "sc", [128, S], F32)
                wk, wkb = sb(st, "wk", [128, S], F32)
                m8, m8b = sb(st, "m8", [128, 8], F32)
                bmid, bmidb = sb(st, "bmid", [128, 1], F32)
                bcnt, bcntb = sb(st, "bcnt", [128, 1], F32)
                bd, bdb = sb(st, "bd", [128, 1], F32)
                mqr = Rot([sb(st, "mq%d" % i, [128, S], BF16) for i in range(2)])
                mT, mTb = sb(st, "mT", [128, NT, 512], BF16)
                rr = Rot([sb(st, "rl%d" % i, [128, 512], F32) for i in range(3)])
                osr = Rot([sb(st, "ostg%d" % i, [64, 5, 512], BF16) for i in range(2)])
                for Q in range(NQ):
                    qs = slice(Q * 512, (Q + 1) * 512)
                    q, qb_ = qr.next()
                    mk.dma("sync", q[:, 0:2, :], PJT[R_DQ:R_DQ + 256, qs].rearrange("(c p) t -> p c t", p=128), reads=[PJb], writes=[qb_])
                    mk.dma("sync", q[0:64, 2, :], PJT[R_DQ + 256:R_DQ + 320, qs], reads=[PJb], writes=[qb_])
                    qi, qib = qir.next()
                    mk.dma("sync", qi[:], PJT[R_IQ:R_IQ + 512, qs].rearrange("(c p) t -> p c t", p=128), reads=[PJb], writes=[qib])
                    mk.op("gpsimd", lambda e: e.memset(mT[:, 4 * Q:4 * Q + 4, :], 0.0), writes=[mTb])
                    for qsub in range(4):
                        qb = 4 * Q + qsub
                        nk = (qb + 1) * 128
                        for ks in range((nk + 511) // 512):
                            w_ = min(512, nk - ks * 512)
                            for hi in range(8):
                                ps, psb = psrot.next()
                                pbi = (hi % 2) * 64
                                mk.op("tensor", lambda e: e.matmul(ps[:, 0:w_], lhsT=qi[pbi:pbi + 64, hi // 2, qsub * 128:(qsub + 1) * 128],
                                                                   rhs=kix[pbi:pbi + 64, ks * 512:ks * 512 + w_], start=True, stop=True),
                                      reads=[qib, kixb], writes=[psb], skip_same=True)
                                r, rb = rr.next()
                                mk.op("scalar", lambda e: e.activation(out=r[:, 0:w_], in_=ps[:, 0:w_], func=AF.Relu), reads=[psb], writes=[rb])
                                if hi == 0:
                                    mk.op("vector", lambda e: e.tensor_scalar(out=sc[:, ks * 512:ks * 512 + w_], in0=r[:, 0:w_],
                                                                              scalar1=widx[:, qb, 0:1], scalar2=None, op0=ALU.mult),
                                          reads=[rb, widxb], writes=[scb])
                                else:
                                    mk.op("vector", lambda e: e.scalar_tensor_tensor(out=sc[:, ks * 512:ks * 512 + w_], in0=r[:, 0:w_],
                                                                                     scalar=widx[:, qb, hi:hi + 1],
                                                                                     in1=sc[:, ks * 512:ks * 512 + w_],
                                                                                     op0=ALU.mult, op1=ALU.add),
                                          reads=[rb, widxb, scb], writes=[scb])
                        mk.op("gpsimd", lambda e: e.tensor_tensor(out=sc[:, qb * 128:nk], in0=sc[:, qb * 128:nk], in1=negm[:], op=ALU.add),
                              reads=[scb, negmb], writes=[scb])
                        with mk.small():
                            mq, mqb = mqr.next()
                            if qb >= 2:
                                mk.op("vector", lambda e: e.max(out=m8[:], in_=sc[:, 0:nk]), reads=[scb], writes=[m8b])
                                w = TOPK_W / 2
                                mk.op("vector", lambda e: e.tensor_scalar(out=bmid[:], in0=m8[:, 0:1], scalar1=-w, scalar2=None, op0=ALU.add),
                                      reads=[m8b], writes=[bmidb])
                                for it in range(TOPK_IT):
                                    mk.op("vector", lambda e: e.tensor_scalar(out=mq[:, 0:nk], in0=sc[:, 0:nk], scalar1=bmid[:, 0:1], scalar2=0.0,
                                                                              op0=ALU.is_ge, op1=ALU.add, accum_out=bcnt[:, 0:1]),
                                          reads=[scb, bmidb], writes=[mqb, bcntb])
                                    last = (it == TOPK_IT - 1)
                                    wn = w if last else w / 2
                                    mk.op("vector", lambda e: e.tensor_scalar(out=bd[:], in0=bcnt[:], scalar1=255.5, scalar2=(wn if last else 2 * wn),
                                                                              op0=ALU.is_ge, op1=ALU.mult), reads=[bcntb], writes=[bdb])
                                    mk.op("vector", lambda e: e.scalar_tensor_tensor(out=bmid[:], in0=bd[:], scalar=-wn, in1=bmid[:],
                                                                                     op0=ALU.add, op1=ALU.add), reads=[bdb, bmidb], writes=[bmidb])
                                    w = wn
                                mk.op("vector", lambda e: e.tensor_scalar(out=mq[:, 0:nk], in0=sc[:, 0:nk], scalar1=bmid[:, 0:1], scalar2=None,
                                                                          op0=ALU.is_ge), reads=[scb, bmidb], writes=[mqb])
                            else:
                                mk.op("vector", lambda e: e.tensor_scalar(out=mq[:, 0:nk], in0=sc[:, 0:nk], scalar1=-1.0e29, scalar2=None,
                                                                          op0=ALU.is_ge), reads=[scb], writes=[mqb])
                        for g0 in range(0, qb + 1, 8):
                            g1 = min(qb + 1, g0 + 8)
                            for kb in range(g0, g1):
                                mk.op("tensor", lambda e: e.transpose(psT[:, (kb - g0) * 128:(kb - g0 + 1) * 128], mq[:, kb * 128:(kb + 1) * 128], ident[:]),
                                      reads=[mqb, identb], writes=[psTb], skip_same=True)
                            mk.op("scalar", lambda e: e.activation(out=mT[:, g0:g1, qsub * 128:(qsub + 1) * 128],
                                                                   in_=psT[:, 0:(g1 - g0) * 128].rearrange("p (k t) -> p k t", k=g1 - g0),
                                                                   func=AF.Identity), reads=[psTb], writes=[mTb])
                    ostg, ostgb = osr.next()
                    for h in range(5):
                        attn_softmax_head_dsa(tiles, q, qb_, kT, kTb, vt, vtb, h, Q, mT, mTb, ostg, ostgb)
                    mk.dma("sync", OT[O_D:O_D + 320, qs].rearrange("(h p) t -> p h t", p=64), ostg[:], reads=[ostgb], writes=[OTb], semb=ostgb)
                mk.barrier(recycle=True)

        def attn_softmax_head_dsa(tiles, q, qb_, kT, kTb, vt, vtb, h, Q, mT, mTb, ostg, ostgb):
            class V:
                def __getitem__(self, idx):
                    return ostg[idx[0], h, :]
            attn_softmax_head(tiles, q, qb_, 0, kT, kTb, vt, vtb, h, Q,
                              lambda kb: (None, []),
                              lambda kb: (mT[:, kb, :], [mTb]),
                              V(), ostgb)

        def sbk(l):
            prev = mk.SAME_ENGINE_SYNC
            mk.SAME_ENGINE_SYNC = False
            try:
                _sbk(l)
            finally:
                mk.SAME_ENGINE_SYNC = prev

        def _sbk(l):
            with contextlib.ExitStack() as st:
                kT, kTb, vt, vtb = load_kv(st, R_SK, V_S, 5)
                qT, qTb = sb(st, "qT", [128, 3, S], BF16)
                mk.dma("sync", qT[:, 0:2, :], PJT[R_SQ:R_SQ + 256, :].rearrange("(c p) t -> p c t", p=128), reads=[PJb], writes=[qTb])
                mk.dma("sync", qT[0:64, 2, :], PJT[R_SQ + 256:R_SQ + 320, :], reads=[PJb], writes=[qTb])
                etr = Rot([sb(st, "et%d" % i, [128, 512], F32) for i in range(2)])
                ltr = Rot([sb(st, "lt%d" % i, [128, 512], BF16) for i in range(3)])
                t1r = Rot([sb(st, "st1%d" % i, [128, 512], F32) for i in range(3)])
                t2r = Rot([sb(st, "st2%d" % i, [128, 512], F32) for i in range(2)])
                atr = Rot([sb(st, "at%d" % i, [128, 512], BF16) for i in range(3)])
                rsr = Rot([sb(st, "rs%d" % i, [128, 512], F32) for i in range(2)])
                osr = Rot([sb(st, "ostg%d" % i, [64, S], BF16) for i in range(2)])
                rot5 = Rot(PS[0:4] + [PS[6]])
                for h in range(SB_H):
                    ck, pb = h // 2, (h % 2) * 64
                    ostg, ostgb = osr.next()
                    for Q in range(SB_NQ):
                        qs = slice(Q * 512, (Q + 1) * 512)
                        nkb = 4 * Q + 4
                        rs, rsb = rsr.next()
                        mk.op("gpsimd", lambda e: e.memset(rs[:], 0.0), writes=[rsb])
                        pO, pOb = PS[4 + (Q % 2)]
                        stA = {}

                        def stage_a(kb):
                            d = kb - 4 * Q
                            pz, pzb = rot5.next()
                            mk.op("tensor", lambda e: e.matmul(pz[:, :], lhsT=kT[pb:pb + 64, ck, kb * 128:(kb + 1) * 128],
                                                               rhs=qT[pb:pb + 64, ck, qs], start=True, stop=True),
                                  reads=[kTb, qTb], writes=[pzb], skip_same=True)
                            et, etb = etr.next()
                            lt, ltb = ltr.next()
                            mk.op("scalar", lambda e: e.activation(out=et[:], in_=pz[:, :], func=AF.Exp, scale=0.125), reads=[pzb], writes=[etb])
                            mk.op("scalar", lambda e: e.activation(out=lt[:], in_=et[:], func=AF.Ln, bias=1.0), reads=[etb], writes=[ltb])
                            if d >= 0:
                                mk.op("gpsimd", lambda e: e.tensor_tensor(out=lt[:], in0=lt[:], in1=smask[:, d * 512:(d + 1) * 512], op=ALU.mult),
                                      reads=[ltb, smaskb], writes=[ltb])
                            pc, pcb = rot5.next()
                            mk.op("tensor", lambda e: e.matmul(pc[:, :], lhsT=trige[:], rhs=lt[:], start=True, stop=True),
                                  reads=[trigeb, ltb], writes=[pcb], skip_same=True)
                            prt = None
                            if kb > 0:
                                pr, prb = rot5.next()
                                mk.op("tensor", lambda e: e.matmul(pr[:, :], lhsT=onesb[:], rhs=lt[:], start=True, stop=True),
                                      reads=[onesbb, ltb], writes=[prb], skip_same=True)
                                prt = (pr, prb)
                            t1, t1b = t1r.next()
                            mk.op("scalar", lambda e: e.activation(out=t1[:], in_=pz[:, :], func=AF.Identity, scale=0.125),
                                  reads=[pzb], writes=[t1b])
                            stA[kb] = (d, pc, pcb, prt, t1, t1b)

                        def stage_b(kb):
                            d, pc, pcb, prt, t1, t1b = stA.pop(kb)
                            mk.op("gpsimd", lambda e: e.tensor_tensor(out=t1[:], in0=t1[:], in1=rs[:], op=ALU.subtract),
                                  reads=[t1b, rsb], writes=[t1b])
                            t2, t2b = t2r.next()
                            mk.op("vector", lambda e: e.tensor_tensor(out=t2[:], in0=t1[:], in1=pc[:, :], op=ALU.subtract),
                                  reads=[t1b, pcb], writes=[t2b])
                            at, atb = atr.next()
                            mk.op("scalar", lambda e: e.activation(out=at[:], in_=t2[:], func=AF.Exp), reads=[t2b], writes=[atb])
                            if d >= 0:
                                mk.op("gpsimd", lambda e: e.tensor_tensor(out=at[:], in0=at[:], in1=smask[:, d * 512:(d + 1) * 512], op=ALU.mult),
                                      reads=[atb, smaskb], writes=[atb])
                            if prt is not None:
                                pr, prb = prt
                                mk.op("vector", lambda e: e.tensor_tensor(out=rs[:], in0=pr[:, :], in1=rs[:], op=ALU.add),
                                      reads=[rsb, prb], writes=[rsb])
                            mk.op("tensor", lambda e: e.matmul(pO[0:65, :], lhsT=vt[:, kb, h, 0:65], rhs=at[:],
                                                               start=(kb == nkb - 1), stop=(kb == 0)),
                                  reads=[vtb, atb], writes=[pOb], skip_same=True)

                        kbs = list(range(nkb - 1, -1, -1))
                        stage_a(kbs[0])
                        for i, kb in enumerate(kbs):
                            if i + 1 < len(kbs):
                                stage_a(kbs[i + 1])
                            stage_b(kb)
                        mk.op("scalar", lambda e: e.activation(out=ostg[0:64, qs], in_=pO[0:64, :], func=AF.Identity), reads=[pOb], writes=[ostgb])
                    mk.dma("sync", OT[O_S + h * 64:O_S + (h + 1) * 64, :], ostg[:], reads=[ostgb], writes=[OTb], semb=ostgb)
                mk.barrier(recycle=True)

        def merge(l):
            win = W["w_in"][l]
            with contextlib.ExitStack() as st:
                hT, hTb = sb(st, "hT2", [128, 8, S // 2], BF16)
                gw, gwb = sb(st, "gw", [128, 8, 3072], BF16)
                for b in range(3):
                    wchunk(win, C_G + b * 1024, 1024, gw[:, :, b * 1024:(b + 1) * 1024], gwb)
                wbo, wbob = sb(st, "wbo", [128, 9, D], BF16)
                mk.dma("gpsimd", wbo[:, 0:3, :], W["w_fox_out"][l].rearrange("(c p) n -> p c n", p=128), writes=[wbob])
                for b, nm in ((1, "w_dsa_out"), (2, "w_sb_out")):
                    mk.dma("gpsimd", wbo[:, 3 * b:3 * b + 2, :], W[nm][l][0:256, :].rearrange("(c p) n -> p c n", p=128), writes=[wbob])
                    mk.dma("gpsimd", wbo[0:64, 3 * b + 2, :], W[nm][l][256:320, :], writes=[wbob])
                wo, wob = sb(st, "wo", [128, 8, D], BF16)
                wchunk(W["w_out"][l], 0, D, wo[:], wob)
                oqr = Rot([sb(st, "oq%d" % i, [128, 9, 512], BF16) for i in range(1)])
                sgr = Rot([sb(st, "msg%d" % i, [128, 512], F32) for i in range(3)])
                tmr = Rot([sb(st, "mtm%d" % i, [128, 512], F32) for i in range(3)])
                mTr = Rot([sb(st, "mmT%d" % i, [128, 8, 512], BF16) for i in range(2)])
                xr = Rot([sb(st, "mx%d" % i, [128, D], F32) for i in range(2)])
                KK = [128, 128, 128, 128, 128, 64, 128, 128, 64]
                for Q in range(NQ):
                    if Q % 4 == 0:
                        norm_pass(st, W["mix_norm"][l:l + 1, :], hT, hTb, t0=(Q // 4) * 16, nt=16)
                    qs = slice(Q * 512, (Q + 1) * 512)
                    ql = slice((Q % 4) * 512, (Q % 4 + 1) * 512)
                    oq, oqb = oqr.next()
                    mk.dma("sync", oq[:, 0:5, :], OT[0:640, qs].rearrange("(c p) t -> p c t", p=128), reads=[OTb], writes=[oqb])
                    mk.dma("sync", oq[0:64, 5, :], OT[640:704, qs], reads=[OTb], writes=[oqb])
                    mk.dma("sync", oq[:, 6:8, :], OT[768:1024, qs].rearrange("(c p) t -> p c t", p=128), reads=[OTb], writes=[oqb])
                    mk.dma("sync", oq[0:64, 8, :], OT[1024:1088, qs], reads=[OTb], writes=[oqb])
                    mT, mTb = mTr.next()
                    for fc in range(8):
                        tms = []
                        for b in range(3):
                            pg, pgb = psrot.next()
                            for kc in range(8):
                                mk.op("tensor", lambda e: e.matmul(pg[:, :], lhsT=gw[:, kc, b * 1024 + fc * 128:b * 1024 + (fc + 1) * 128],
                                                                   rhs=hT[:, kc, ql], start=(kc == 0), stop=(kc == 7)),
                                      reads=[gwb, hTb], writes=[pgb], skip_same=True)
                            sg, sgb = sgr.next()
                            mk.op("scalar", lambda e: e.activation(out=sg[:], in_=pg[:, :], func=AF.Sigmoid), reads=[pgb], writes=[sgb])
                            pp, ppb = psrot.next()
                            for c in range(3):
                                kk = KK[3 * b + c]
                                mk.op("tensor", lambda e: e.matmul(pp[:, :], lhsT=wbo[0:kk, 3 * b + c, fc * 128:(fc + 1) * 128],
                                                                   rhs=oq[0:kk, 3 * b + c, :], start=(c == 0), stop=(c == 2)),
                                      reads=[wbob, oqb], writes=[ppb], skip_same=True)
                            tm, tmb = tmr.next()
                            mk.op("vector", lambda e: e.tensor_tensor(out=tm[:], in0=sg[:], in1=pp[:, :], op=ALU.mult),
                                  reads=[sgb, ppb], writes=[tmb])
                            tms.append((tm, tmb))
                        (a0, a0b), (a1, a1b), (a2, a2b) = tms
                        mk.op("gpsimd", lambda e: e.tensor_tensor(out=a0[:], in0=a0[:], in1=a1[:], op=ALU.add), reads=[a0b, a1b], writes=[a0b])
                        mk.op("gpsimd", lambda e: e.tensor_tensor(out=mT[:, fc, :], in0=a0[:], in1=a2[:], op=ALU.add),
                              reads=[a0b, a2b], writes=[mTb])
                    for sub in range(4):
                        t = Q * 4 + sub
                        xt, xtb = xr.next()
                        mk.dma("sync", xt[:], X[t * 128:(t + 1) * 128, :], reads=[Xb[t]], writes=[xtb])
                        for half in range(2):
                            ps, psb = psrot.next()
                            for kc in range(8):
                                mk.op("tensor", lambda e: e.matmul(ps[:, :], lhsT=mT[:, kc, sub * 128:(sub + 1) * 128],
                                                                   rhs=wo[:, kc, half * 512:(half + 1) * 512], start=(kc == 0), stop=(kc == 7)),
                                      reads=[mTb, wob], writes=[psb], skip_same=True)
                            mk.op("vector", lambda e: e.tensor_tensor(out=xt[:, half * 512:(half + 1) * 512], in0=ps[:, :],
                                                                      in1=xt[:, half * 512:(half + 1) * 512], op=ALU.add),
                                  reads=[psb, xtb], writes=[xtb])
                        mk.dma("sync", X[t * 128:(t + 1) * 128, :], xt[:], reads=[xtb], writes=[Xb[t]], semb=xtb)
                mk.barrier(recycle=True)

        def ca(l):
            with contextlib.ExitStack() as st:
                hT, hTb = sb(st, "hT", [128, 8, S], BF16)
                norm_pass(st, W["ca_norm"][l:l + 1, :], hT, hTb)
                load_gain(W["mem_norm"][l:l + 1, :])
                memT, memTb = sb(st, "memT", [128, 8, 256], BF16)
                mx, mxb = sb(st, "cmx", [128, D], F32)
                mh, mhb = sb(st, "cmh", [128, D], BF16)
                junk, junkb = sb(st, "cjunk", [128, D], BF16)
                for mb in range(2):
                    mk.dma("sync", mx[:], mem_in[mb * 128:(mb + 1) * 128, :], writes=[mxb])
                    rstd_of(mx, mxb, junk, junkb)
                    with mk.small():
                        mk.op("vector", lambda e: e.scalar_tensor_tensor(out=mh[:], in0=mx[:], scalar=small[:, 2:3], in1=gB[:],
                                                                         op0=ALU.mult, op1=ALU.mult), reads=[mxb, smallb, gBb], writes=[mhb])
                    for kc in range(8):
                        mk.op("tensor", lambda e: e.transpose(psT[:, kc * 128:(kc + 1) * 128], mh[:, kc * 128:(kc + 1) * 128], ident[:]),
                              reads=[mhb, identb], writes=[psTb], skip_same=True)
                    mk.op("scalar", lambda e: e.activation(out=memT[:, :, mb * 128:(mb + 1) * 128],
                                                           in_=psT[:, :].rearrange("p (k t) -> p k t", k=8), func=AF.Identity),
                          reads=[psTb], writes=[memTb])
                wkv, wkvb = sb(st, "wkv", [128, 8, D], BF16)
                wchunk(W["ca_w_kv"][l], 0, D, wkv[:], wkvb)
                wq, wqb = sb(st, "wq", [128, 8, 512], BF16)
                wchunk(W["ca_w_q"][l], 0, 512, wq[:], wqb)
                wo, wob = sb(st, "cwo", [128, 4, D], BF16)
                mk.dma("gpsimd", wo[:], W["ca_w_o"][l].rearrange("(c p) n -> p c n", p=128), writes=[wob])
                kTm, kTmb = sb(st, "kTm", [128, 4, 256], BF16)
                vm, vmb = sb(st, "vm", [128, 2, 512], BF16)
                for h in range(4):
                    ps, psb = psrot.next()
                    for kc in range(8):
                        mk.op("tensor", lambda e: e.matmul(ps[:, 0:256], lhsT=wkv[:, kc, h * 128:(h + 1) * 128], rhs=memT[:, kc, :],
                                                           start=(kc == 0), stop=(kc == 7)), reads=[wkvb, memTb], writes=[psb], skip_same=True)
                    mk.op("scalar", lambda e: e.activation(out=kTm[:, h, :], in_=ps[:, 0:256], func=AF.Identity), reads=[psb], writes=[kTmb])
                for mb in range(2):
                    ps, psb = psrot.next()
                    for kc in range(8):
                        mk.op("tensor", lambda e: e.matmul(ps[:, :], lhsT=memT[:, kc, mb * 128:(mb + 1) * 128], rhs=wkv[:, kc, 512:1024],
                                                           start=(kc == 0), stop=(kc == 7)), reads=[wkvb, memTb], writes=[psb], skip_same=True)
                    mk.op("scalar", lambda e: e.activation(out=vm[:, mb, :], in_=ps[:, :], func=AF.Identity), reads=[psb], writes=[vmb])
                qcr = Rot([sb(st, "qc%d" % i, [128, 512], BF16) for i in range(2)])
                ptr = Rot([sb(st, "cpt%d" % i, [128, 512], BF16) for i in range(3)])
                rdr = Rot([sb(st, "crd%d" % i, [128, 512], F32) for i in range(2)])
                ocr = Rot([sb(st, "oc%d" % i, [128, 4, 512], BF16) for i in range(2)])
                xr = Rot([sb(st, "cx%d" % i, [128, D], F32) for i in range(3)])
                sc_ = 128.0 ** -0.5
                for Q in range(NQ):
                    qs = slice(Q * 512, (Q + 1) * 512)
                    oc, ocb = ocr.next()
                    for h in range(4):
                        ps, psb = psrot.next()
                        for kc in range(8):
                            mk.op("tensor", lambda e: e.matmul(ps[:, :], lhsT=wq[:, kc, h * 128:(h + 1) * 128], rhs=hT[:, kc, qs],
                                                               start=(kc == 0), stop=(kc == 7)), reads=[wqb, hTb], writes=[psb], skip_same=True)
                        qc, qcb = qcr.next()
                        mk.op("scalar", lambda e: e.activation(out=qc[:], in_=ps[:, :], func=AF.Identity), reads=[psb], writes=[qcb])
                        pO, pOb = PS[4]
                        pD, pDb = PS[5]
                        for mb in range(2):
                            pS, pSb = psrot.next()
                            mk.op("tensor", lambda e: e.matmul(pS[:, :], lhsT=kTm[:, h, mb * 128:(mb + 1) * 128], rhs=qc[:], start=True, stop=True),
                                  reads=[kTmb, qcb], writes=[pSb], skip_same=True)
                            pt, ptb = ptr.next()
                            mk.op("scalar", lambda e: e.activation(out=pt[:], in_=pS[:, :], func=AF.Exp, scale=sc_), reads=[pSb], writes=[ptb])
                            mk.op("tensor", lambda e: e.matmul(pO[:, :], lhsT=vm[:, mb, h * 128:(h + 1) * 128], rhs=pt[:], start=(mb == 0), stop=(mb == 1)),
                                  reads=[vmb, ptb], writes=[pOb], skip_same=True)
                            mk.op("tensor", lambda e: e.matmul(pD[:, :], lhsT=onesb[:], rhs=pt[:], start=(mb == 0), stop=(mb == 1)),
                                  reads=[onesbb, ptb], writes=[pDb], skip_same=True)
                        rd, rdb = rdr.next()
                        mk.op("vector", lambda e: e.reciprocal(out=rd[:], in_=pD[:, :]), reads=[pDb], writes=[rdb])
                        mk.op("vector", lambda e: e.tensor_tensor(out=oc[:, h, :], in0=pO[:, :], in1=rd[:], op=ALU.mult),
                              reads=[pOb, rdb], writes=[ocb])
                    for sub in range(4):
                        t = Q * 4 + sub
                        xt, xtb = xr.next()
                        mk.dma("sync", xt[:], X[t * 128:(t + 1) * 128, :], reads=[Xb[t]], writes=[xtb])
                        for half in range(2):
                            ps, psb = psrot.next()
                            for h in range(4):
                                mk.op("tensor", lambda e: e.matmul(ps[:, :], lhsT=oc[:, h, sub * 128:(sub + 1) * 128],
                                                                   rhs=wo[:, h, half * 512:(half + 1) * 512], start=(h == 0), stop=(h == 3)),
                                      reads=[ocb, wob], writes=[psb], skip_same=True)
                            mk.op("vector", lambda e: e.tensor_tensor(out=xt[:, half * 512:(half + 1) * 512], in0=ps[:, :],
                                                                      in1=xt[:, half * 512:(half + 1) * 512], op=ALU.add),
                                  reads=[psb, xtb], writes=[xtb])
                        mk.dma("sync", X[t * 128:(t + 1) * 128, :], xt[:], reads=[xtb], writes=[Xb[t]], semb=xtb)
                mk.barrier(recycle=True)

        def final(raw):
            with contextlib.ExitStack() as st:
                xr = Rot([sb(st, "fx%d" % i, [128, D], F32) for i in range(3)])
                junk, junkb = sb(st, "fjunk", [128, D], BF16)
                if not raw:
                    load_gain(W["final_norm"][0:1, :])
                src = xstate["src"]
                outb = Buf("out")
                for t in range(NT):
                    xt, xtb = xr.next()
                    mk.dma("sync", xt[:], src[t * 128:(t + 1) * 128, :], reads=[Xb[t]], writes=[xtb])
                    if not raw:
                        rstd_of(xt, xtb, junk, junkb)
                        with mk.small():
                            mk.op("vector", lambda e: e.scalar_tensor_tensor(out=xt[:], in0=xt[:], scalar=small[:, 2:3], in1=gB[:],
                                                                             op0=ALU.mult, op1=ALU.mult),
                                  reads=[xtb, smallb, gBb], writes=[xtb])
                    mk.dma("sync", out_d[t * 128:(t + 1) * 128, :], xt[:], reads=[xtb], writes=[outb], semb=xtb)
                mk.barrier(recycle=True)

        for l in range(n_layers):
            if "ffn1" in stages:
                ffn(l, 1)
            if "proj" in stages:
                proj(l)
            if "fox" in stages:
                fox(l)
            if "dsa" in stages:
                dsa(l)
            if "sb" in stages:
                sbk(l)
            if "merge" in stages:
                merge(l)
            if "ca" in stages:
                ca(l)
            if "ffn2" in stages:
                ffn(l, 2)
        if not attn_probe:
            final(raw=debug)
        else:
            mk.barrier()
    return nc


def make_in_maps(inputs, n_layers=4):
    consts = host_consts()
    maps = []
    shared = {}
    for name, shp in WSPEC:
        shared[name] = np.ascontiguousarray(np.asarray(inputs[name], dtype=np.float32).reshape(shp)[:n_layers])
    for k, v in consts.items():
        shared["c_" + k] = np.ascontiguousarray(v.astype(np.float32))
    for c in range(8):
        b = c // 2
        m = dict(shared)
        m["x"] = np.ascontiguousarray(np.asarray(inputs["x"][b], dtype=np.float32))
        m["mem"] = np.ascontiguousarray(np.asarray(inputs["mem"][b], dtype=np.float32))
        m["pos"] = np.ascontiguousarray(np.asarray(inputs["positions"][b], dtype=np.int32).reshape(1, S))
        maps.append(m)
    return maps


def kernel(**inputs):
    nc = build()
    res = run_bass_kernel_spmd(nc, make_in_maps(inputs), core_ids=list(range(8)))
    out = np.stack([np.asarray(res.results[2 * b]["out"], dtype=np.float32) for b in range(4)], axis=0)
    return out
```

```python
import contextlib
import math
import numpy as np
import concourse.bass as bass
import concourse.mybir as mybir
from concourse.bass_utils import run_bass_kernel_spmd

F32 = mybir.dt.float32
BF16 = mybir.dt.bfloat16
I32 = mybir.dt.int32
ALU = mybir.AluOpType
AF = mybir.ActivationFunctionType

S = 4096
D = 1024
NT = 32
NQ = 8
DFF = 2816
NJ = 22
DIN = 6734
NEG = -1.0e30
TOPK_W = 128.0
TOPK_IT = 28
SB_H = 5
SB_NQ = 8
SB_DBG = 5
SB_V = 'bc'
C_FQ, C_FK, C_FV, C_FF = 0, 384, 768, 1152
C_DQ, C_DK, C_DV = 1158, 1478, 1798
C_IQ, C_IK, C_IW = 2118, 2630, 2694
C_SQ, C_SK, C_SV = 2702, 3022, 3342
C_G = 3662
R_FQ, R_FK, R_DQ, R_DK, R_IQ, R_IK, R_SQ, R_SK = 0, 384, 768, 1088, 1408, 1920, 1984, 2304
PJ_ROWS = 2624
V_F, V_D, V_S = 0, 390, 715
V_COLS = 1040
O_F, O_D, O_S = 0, 384, 768
O_ROWS = 1152


class Sem:
    __slots__ = ("h", "count", "is_dma")

    def __init__(self, h, is_dma):
        self.h = h
        self.count = 0
        self.is_dma = is_dma


class Buf:
    __slots__ = ("name", "w", "r", "dsem")

    def __init__(self, name):
        self.name = name
        self.w = None
        self.r = {}
        self.dsem = None


class MK:
    SAME_ENGINE_SYNC = False

    @contextlib.contextmanager
    def small(self):
        prev = self.SAME_ENGINE_SYNC
        self.SAME_ENGINE_SYNC = True
        try:
            yield
        finally:
            self.SAME_ENGINE_SYNC = prev

    def __init__(self, nc, es):
        self.nc = nc
        self.es = es
        self.eng = {}
        self.esem = {}
        self.waited = {}
        self.dsems = []
        self.phase_sems = []
        self.free_sems = []
        self.uid = 0
        for name in ("tensor", "vector", "scalar", "gpsimd", "sync"):
            self.eng[name] = getattr(nc, name)
            self.esem[name] = Sem(es.enter_context(nc.semaphore("s_" + name)), False)
            self.waited[name] = {}

    def buf(self, name="b"):
        return Buf(name)

    def _dsem(self, b):
        if b.dsem is None:
            if self.free_sems:
                b.dsem = self.free_sems.pop()
            else:
                self.uid += 1
                b.dsem = Sem(self.es.enter_context(self.nc.semaphore("d%d" % self.uid)), True)
                self.dsems.append(b.dsem)
            self.phase_sems.append(b.dsem)
        return b.dsem

    def keep(self):
        self.phase_sems = []

    def _waits(self, en, reads, writes, skip_same):
        need = {}
        for b in reads:
            if b.w is not None:
                s, v = b.w
                if need.get(s, 0) < v:
                    need[s] = v
        for b in writes:
            if b.w is not None:
                s, v = b.w
                if need.get(s, 0) < v:
                    need[s] = v
            for s, v in b.r.items():
                if need.get(s, 0) < v:
                    need[s] = v
        wd = self.waited[en]
        own = self.esem[en]
        for s, v in need.items():
            if s.is_dma:
                v = s.count
            if s is own and (skip_same or not self.SAME_ENGINE_SYNC):
                continue
            if wd.get(s, 0) >= v:
                continue
            self.eng[en].wait_ge(s.h, v)
            wd[s] = v

    def op(self, en, fn, reads=(), writes=(), skip_same=False):
        self._waits(en, reads, writes, skip_same)
        inst = fn(self.eng[en])
        s = self.esem[en]
        s.count += 1
        inst.then_inc(s.h, 1)
        for b in reads:
            b.r[s] = s.count
        ev = (s, s.count)
        for b in writes:
            b.w = ev
            b.r = {}
        return inst

    def dma(self, q, out, in_, reads=(), writes=(), semb=None):
        self._waits(q, reads, writes, True)
        if semb is None:
            semb = (list(writes) + list(reads))[0]
        s = self._dsem(semb)
        inst = self.eng[q].dma_start(out=out, in_=in_)
        s.count += 16
        inst.then_inc(s.h, 16)
        for b in reads:
            b.r[s] = s.count
        ev = (s, s.count)
        for b in writes:
            b.w = ev
            b.r = {}
        return inst

    def barrier(self, recycle=False):
        if recycle:
            self.free_sems.extend(self.phase_sems)
            self.phase_sems = []
        for en in self.eng:
            wd = self.waited[en]
            for on, s in self.esem.items():
                if on == en or s.count == 0:
                    continue
                if wd.get(s, 0) < s.count:
                    self.eng[en].wait_ge(s.h, s.count)
                    wd[s] = s.count
            for s in self.dsems:
                if s.count and wd.get(s, 0) < s.count:
                    self.eng[en].wait_ge(s.h, s.count)
                    wd[s] = s.count


class Rot:
    def __init__(self, items):
        self.items = items
        self.i = 0

    def next(self):
        it = self.items[self.i % len(self.items)]
        self.i += 1
        return it


def host_consts():
    c = {}
    p = np.arange(128)[:, None]
    f = np.arange(128)[None, :]
    c["ident"] = (p == f).astype(np.float32)
    rot = np.zeros((128, 128), np.float32)
    for m in range(128):
        if (m % 64) < 32:
            rot[m + 32, m] = -1.0
        else:
            rot[m - 32, m] = 1.0
    c["rot"] = rot
    c["trile"] = (p <= f).astype(np.float32)
    c["trige"] = (p >= f).astype(np.float32)
    ff = np.arange(512)[None, None, :]
    dd = np.arange(4)[None, :, None]
    pp = np.arange(128)[:, None, None]
    c["cmask"] = ((128 * dd + pp) <= ff).astype(np.float32).reshape(128, 2048)
    c["smask"] = ((128 * dd + pp) < ff).astype(np.float32).reshape(128, 2048)
    c["negm"] = np.where(f <= p, 0.0, NEG).astype(np.float32)
    c["invf"] = (10000.0 ** (-(np.arange(128) % 32).astype(np.float64) / 32.0)).astype(np.float32).reshape(128, 1)
    return c


CONST_SHAPES = {"ident": 128, "rot": 128, "trile": 128, "trige": 128, "cmask": 2048, "smask": 2048,
                "negm": 128, "invf": 1}

WSPEC = [("ffn1_norm", [4, 1024]), ("ffn1_w_gu", [4, 1024, 5632]), ("ffn1_w_down", [4, 2816, 1024]),
         ("mix_norm", [4, 1024]), ("w_in", [4, 1024, 6734]), ("b_fgate", [4, 6]),
         ("w_fox_out", [4, 384, 1024]), ("w_dsa_out", [4, 320, 1024]), ("w_sb_out", [4, 320, 1024]),
         ("w_out", [4, 1024, 1024]), ("ca_norm", [4, 1024]), ("mem_norm", [4, 1024]),
         ("ca_w_q", [4, 1024, 512]), ("ca_w_kv", [4, 1024, 1024]), ("ca_w_o", [4, 512, 1024]),
         ("ffn2_norm", [4, 1024]), ("ffn2_w_gu", [4, 1024, 5632]), ("ffn2_w_down", [4, 2816, 1024]),
         ("final_norm", [1, 1024])]


def build(n_layers=4, stages=("ffn1", "proj", "fox", "dsa", "sb", "merge", "ca", "ffn2"), debug=False, attn_probe=False):
    nc = bass.Bass("TRN2", target_bir_lowering=False)
    x_in = nc.dram_tensor("x", [S, D], F32, kind="ExternalInput").ap()
    mem_in = nc.dram_tensor("mem", [256, D], F32, kind="ExternalInput").ap()
    pos_in = nc.dram_tensor("pos", [1, S], I32, kind="ExternalInput").ap()
    W = {}
    for name, shp in ([] if attn_probe else WSPEC):
        shp = [min(shp[0], n_layers)] + list(shp[1:])
        W[name] = nc.dram_tensor(name, shp, F32, kind="ExternalInput").ap()
    CD = {}
    for name, n in CONST_SHAPES.items():
        CD[name] = nc.dram_tensor("c_" + name, [128, n], F32, kind="ExternalInput").ap()
    out_d = nc.dram_tensor("out", [S, D], F32, kind="ExternalOutput").ap()
    dbgk = "ExternalOutput" if debug else "Internal"
    X = nc.dram_tensor("Xs", [S, D], F32, kind="Internal").ap()
    AT = nc.dram_tensor("ATs", [DFF, S], BF16, kind="Internal").ap()
    PJT = nc.dram_tensor("PJTs", [PJ_ROWS, S], BF16, kind=("ExternalInput" if attn_probe else dbgk)).ap()
    VT = nc.dram_tensor("VTs", [S, V_COLS], BF16, kind=("ExternalInput" if attn_probe else "Internal")).ap()
    OT = nc.dram_tensor("OTs", [O_ROWS, S], BF16, kind=dbgk).ap()

    with contextlib.ExitStack() as es:
        mk = MK(nc, es)

        uid = [0]

        def sb(stack, name, shape, dt):
            uid[0] += 1
            t = stack.enter_context(nc.sbuf_tensor("%s_%d" % (name, uid[0]), shape, dt))
            return t, Buf(name)

        Xb = [Buf("X%d" % t) for t in range(NT)]
        ATb = Buf("AT")
        PJb = Buf("PJT")
        VTb = Buf("VT")
        OTb = Buf("OT")
        xstate = {"src": x_in}

        PS = []
        for i in range(7):
            PS.append((es.enter_context(nc.psum_tensor("ps%d" % i, [128, 512], F32)), Buf("ps%d" % i)))
        psT, psTb = es.enter_context(nc.psum_tensor("psT", [128, 1024], BF16)), Buf("psT")

        def cload(name, dt):
            n = CONST_SHAPES[name]
            t, b = sb(es, "k_" + name, [128, n], dt)
            mk.dma("gpsimd", t[:], CD[name][:, :], writes=[b])
            return t, b

        ident, identb = cload("ident", BF16)
        rotm, rotb = cload("rot", BF16)
        trile, trileb = cload("trile", F32)
        trige, trigeb = cload("trige", BF16)
        cmask, cmaskb = cload("cmask", BF16)
        smask, smaskb = cload("smask", BF16)
        negm, negmb = cload("negm", F32)
        invf, invfb = cload("invf", F32)
        onesb, onesbb = sb(es, "onesb", [128, 128], BF16)
        onesf, onesfb = sb(es, "onesf", [128, 128], F32)
        mk.op("vector", lambda e: e.memset(onesb[:], 1.0), writes=[onesbb])
        mk.op("vector", lambda e: e.memset(onesf[:], 1.0), writes=[onesfb])
        Cc, Ccb = sb(es, "Cc", [128, NT, 6], F32)
        Ee, Eeb = sb(es, "Ee", [128, NT, 6], F32)
        widx, widxb = sb(es, "widx", [128, NT, 8], F32)
        gB, gBb = sb(es, "gB", [128, D], F32)
        small, smallb = sb(es, "small", [128, 8], F32)
        mk.keep()

        def rstd_of(xt, xtb, junk, junkb):
            with mk.small():
                mk.op("scalar", lambda e: e.activation(out=junk[:], in_=xt[:], func=AF.Square, accum_out=small[:, 0:1]),
                      reads=[xtb], writes=[junkb, smallb])
                mk.op("vector", lambda e: e.tensor_scalar(out=small[:, 1:2], in0=small[:, 0:1], scalar1=1.0 / D, scalar2=1e-6,
                                                          op0=ALU.mult, op1=ALU.add), reads=[smallb], writes=[smallb])
                mk.op("scalar", lambda e: e.activation(out=small[:, 1:2], in_=small[:, 1:2], func=AF.Sqrt),
                      reads=[smallb], writes=[smallb])
                mk.op("vector", lambda e: e.reciprocal(out=small[:, 2:3], in_=small[:, 1:2]), reads=[smallb], writes=[smallb])

        def load_gain(gain_ap):
            mk.dma("sync", gB[:], gain_ap.to_broadcast([128, D]), writes=[gBb])

        def norm_pass(st_unused, gain_ap, hT, hTb, t0=0, nt=NT):
            load_gain(gain_ap)
            with contextlib.ExitStack() as st:
                xr = Rot([sb(st, "nx%d" % i, [128, D], F32) for i in range(3)])
                hn = Rot([sb(st, "nh%d" % i, [128, D], BF16) for i in range(2)])
                junk, junkb = sb(st, "njunk", [128, D], BF16)
                src = xstate["src"]
                for t in range(t0, t0 + nt):
                    xt, xtb = xr.next()
                    mk.dma("sync", xt[:], src[t * 128:(t + 1) * 128, :], reads=[Xb[t]], writes=[xtb])
                    rstd_of(xt, xtb, junk, junkb)
                    h, hb = hn.next()
                    with mk.small():
                        mk.op("vector", lambda e: e.scalar_tensor_tensor(out=h[:], in0=xt[:], scalar=small[:, 2:3], in1=gB[:],
                                                                         op0=ALU.mult, op1=ALU.mult),
                              reads=[xtb, smallb, gBb], writes=[hb])
                    for kc in range(8):
                        mk.op("tensor", lambda e: e.transpose(psT[:, kc * 128:(kc + 1) * 128], h[:, kc * 128:(kc + 1) * 128], ident[:]),
                              reads=[hb, identb], writes=[psTb], skip_same=True)
                    mk.op("scalar", lambda e: e.activation(out=hT[:, :, (t - t0) * 128:(t - t0 + 1) * 128],
                                                           in_=psT[:, :].rearrange("p (k t) -> p k t", k=8), func=AF.Identity),
                          reads=[psTb], writes=[hTb])
                mk.barrier()

        def wchunk(w2d, c0, n, dst, dstb, kcs=8, q="gpsimd"):
            mk.dma(q, dst, w2d[:, c0:c0 + n].rearrange("(k p) n -> p k n", p=128), writes=[dstb])

        psrot = Rot(PS[0:4])

        def ffn(l, which):
            gain = W["ffn%d_norm" % which][l:l + 1, :]
            wgu = W["ffn%d_w_gu" % which][l]
            wdn = W["ffn%d_w_down" % which][l]
            with contextlib.ExitStack() as st:
                hT, hTb = sb(st, "hT", [128, 8, S], BF16)
                norm_pass(st, gain, hT, hTb)
                wr = Rot([sb(st, "wgu%d" % i, [128, 2, 8, 128], BF16) for i in range(3)])
                ar = Rot([sb(st, "aTj%d" % i, [128, S], BF16) for i in range(2)])
                sr = Rot([sb(st, "sg%d" % i, [128, 512], F32) for i in range(2)])
                wl = []

                def loadw(j):
                    w, wb = wr.next()
                    wchunk(wgu, j * 128, 128, w[:, 0], wb)
                    mk.dma("gpsimd", w[:, 1], wgu[:, DFF + j * 128:DFF + (j + 1) * 128].rearrange("(k p) n -> p k n", p=128),
                           writes=[wb])
                    wl.append((w, wb))
                loadw(0)
                loadw(1)
                for j in range(NJ):
                    if j + 2 < NJ:
                        loadw(j + 2)
                    w, wb = wl[j]
                    a, ab = ar.next()
                    for Q in range(NQ):
                        pg, pgb = psrot.next()
                        pu, pub = psrot.next()
                        for kc in range(8):
                            mk.op("tensor", lambda e: e.matmul(pg[:, :], lhsT=w[:, 0, kc, :], rhs=hT[:, kc, Q * 512:(Q + 1) * 512],
                                                               start=(kc == 0), stop=(kc == 7)),
                                  reads=[wb, hTb], writes=[pgb], skip_same=True)
                        for kc in range(8):
                            mk.op("tensor", lambda e: e.matmul(pu[:, :], lhsT=w[:, 1, kc, :], rhs=hT[:, kc, Q * 512:(Q + 1) * 512],
                                                               start=(kc == 0), stop=(kc == 7)),
                                  reads=[wb, hTb], writes=[pub], skip_same=True)
                        sg, sgb = sr.next()
                        mk.op("scalar", lambda e: e.activation(out=sg[:], in_=pg[:, :], func=AF.Silu), reads=[pgb], writes=[sgb])
                        mk.op("vector", lambda e: e.tensor_tensor(out=a[:, Q * 512:(Q + 1) * 512], in0=sg[:], in1=pu[:, :], op=ALU.mult),
                              reads=[sgb, pub], writes=[ab])
                    mk.dma("sync", AT[j * 128:(j + 1) * 128, :], a[:], reads=[ab], writes=[ATb], semb=ab)
                mk.barrier(recycle=True)
            with contextlib.ExitStack() as st:
                wd, wdb = sb(st, "wd", [128, NJ, D], BF16)
                for j in range(NJ):
                    mk.dma("gpsimd", wd[:, j, :], wdn[j * 128:(j + 1) * 128, :], writes=[wdb])
                aq = Rot([sb(st, "aq%d" % i, [128, NJ, 512], BF16) for i in range(2)])
                xr = Rot([sb(st, "dx%d" % i, [128, D], F32) for i in range(3)])
                src = xstate["src"]
                aql = []

                def load_aq(Q):
                    a, ab = aq.next()
                    mk.dma("sync", a[:], AT[:, Q * 512:(Q + 1) * 512].rearrange("(j p) t -> p j t", p=128), reads=[ATb], writes=[ab])
                    aql.append((a, ab))
                load_aq(0)
                for Q in range(NQ):
                    if Q + 1 < NQ:
                        load_aq(Q + 1)
                    a, ab = aql[Q]
                    for sub in range(4):
                        t = Q * 4 + sub
                        xt, xtb = xr.next()
                        mk.dma("sync", xt[:], src[t * 128:(t + 1) * 128, :], reads=[Xb[t]], writes=[xtb])
                        for half in range(2):
                            ps, psb = psrot.next()
                            for j in range(NJ):
                                mk.op("tensor", lambda e: e.matmul(ps[:, :], lhsT=a[:, j, sub * 128:(sub + 1) * 128],
                                                                   rhs=wd[:, j, half * 512:(half + 1) * 512],
                                                                   start=(j == 0), stop=(j == NJ - 1)),
                                      reads=[ab, wdb], writes=[psb], skip_same=True)
                            mk.op("vector", lambda e: e.scalar_tensor_tensor(out=xt[:, half * 512:(half + 1) * 512], in0=ps[:, :], scalar=0.5,
                                                                             in1=xt[:, half * 512:(half + 1) * 512],
                                                                             op0=ALU.mult, op1=ALU.add),
                                  reads=[psb, xtb], writes=[xtb])
                        mk.dma("sync", X[t * 128:(t + 1) * 128, :], xt[:], reads=[xtb], writes=[Xb[t]], semb=xtb)
                xstate["src"] = X
                mk.barrier(recycle=True)

        def proj(l):
            win = W["w_in"][l]
            with contextlib.ExitStack() as st:
                hT, hTb = sb(st, "hT", [128, 8, S], BF16)
                norm_pass(st, W["mix_norm"][l:l + 1, :], hT, hTb)
                cosT, cosb = sb(st, "cosT", [128, S], F32)
                sinT, sinb = sb(st, "sinT", [128, S], F32)
                st_r = contextlib.ExitStack()
                ang, angb = sb(st_r, "ang", [128, S], F32)
                tmpf, tmpfb = sb(st_r, "tmpf", [128, S], F32)
                posi, posib = sb(st_r, "posi", [128, S], I32)
                with mk.small():
                    mk.dma("sync", posi[:], pos_in.to_broadcast([128, S]), writes=[posib])
                    mk.op("vector", lambda e: e.tensor_copy(out=ang[:], in_=posi[:]), reads=[posib], writes=[angb])
                    mk.op("vector", lambda e: e.tensor_scalar(out=ang[:], in0=ang[:], scalar1=invf[:, 0:1], scalar2=None, op0=ALU.mult),
                          reads=[angb, invfb], writes=[angb])
                    TWO_PI = 2.0 * math.pi
                    C1 = 6.28125
                    C2 = TWO_PI - C1
                    for (dst, dstb, shift) in ((sinT, sinb, 0.0), (cosT, cosb, math.pi / 2)):
                        mk.op("vector", lambda e: e.tensor_scalar(out=tmpf[:], in0=ang[:], scalar1=shift, scalar2=1.0 / TWO_PI,
                                                                  op0=ALU.add, op1=ALU.mult), reads=[angb], writes=[tmpfb])
                        mk.op("vector", lambda e: e.tensor_copy(out=posi[:], in_=tmpf[:]), reads=[tmpfb], writes=[posib])
                        mk.op("vector", lambda e: e.tensor_copy(out=tmpf[:], in_=posi[:]), reads=[posib], writes=[tmpfb])
                        mk.op("vector", lambda e: e.scalar_tensor_tensor(out=dst[:], in0=tmpf[:], scalar=-C1, in1=ang[:],
                                                                         op0=ALU.mult, op1=ALU.add), reads=[tmpfb, angb], writes=[dstb])
                        mk.op("vector", lambda e: e.scalar_tensor_tensor(out=dst[:], in0=tmpf[:], scalar=-C2, in1=dst[:],
                                                                         op0=ALU.mult, op1=ALU.add), reads=[tmpfb, dstb], writes=[dstb])
                        mk.op("vector", lambda e: e.tensor_scalar(out=dst[:], in0=dst[:], scalar1=shift, scalar2=math.pi,
                                                                  op0=ALU.add, op1=ALU.min), reads=[dstb], writes=[dstb])
                        mk.op("vector", lambda e: e.tensor_scalar(out=dst[:], in0=dst[:], scalar1=-math.pi, scalar2=None,
                                                                  op0=ALU.max), reads=[dstb], writes=[dstb])
                        mk.op("scalar", lambda e: e.activation(out=dst[:], in_=dst[:], func=AF.Sin), reads=[dstb], writes=[dstb])
                mk.barrier()
                st_r.close()
                chunks = []
                def add_group(c0, r0, width, rope):
                    o = 0
                    while o < width:
                        m = min(128, width - o)
                        chunks.append((c0 + o, r0 + o, m, rope))
                        o += m
                add_group(C_FQ, R_FQ, 384, False)
                add_group(C_FK, R_FK, 384, False)
                add_group(C_DQ, R_DQ, 320, True)
                add_group(C_DK, R_DK, 320, True)
                add_group(C_IQ, R_IQ, 512, True)
                add_group(C_IK, R_IK, 64, True)
                add_group(C_SQ, R_SQ, 320, False)
                add_group(C_SK, R_SK, 320, False)
                wr = Rot([sb(st, "pw%d" % i, [128, 8, 128], BF16) for i in range(3)])
                sr = Rot([sb(st, "pst%d" % i, [128, S], BF16) for i in range(2)])
                xbr = Rot([sb(st, "pxb%d" % i, [128, 512], BF16) for i in range(2)])
                t1r = Rot([sb(st, "pt1%d" % i, [128, 512], F32) for i in range(2)])
                t2r = Rot([sb(st, "pt2%d" % i, [128, 512], F32) for i in range(2)])
                wl = []

                def loadw(i):
                    c0, r0, m, rope = chunks[i]
                    w, wb = wr.next()
                    wchunk(win, c0, m, w[:, :, 0:m], wb)
                    wl.append((w, wb))
                loadw(0)
                loadw(1)
                for i, (c0, r0, m, rope) in enumerate(chunks):
                    if i + 2 < len(chunks):
                        loadw(i + 2)
                    w, wb = wl[i]
                    stg, stgb = sr.next()
                    for Q in range(NQ):
                        qs = slice(Q * 512, (Q + 1) * 512)
                        ps, psb = psrot.next()
                        for kc in range(8):
                            mk.op("tensor", lambda e: e.matmul(ps[0:m, :], lhsT=w[:, kc, 0:m], rhs=hT[:, kc, qs],
                                                               start=(kc == 0), stop=(kc == 7)),
                                  reads=[wb, hTb], writes=[psb], skip_same=True)
                        if not rope:
                            mk.op("scalar", lambda e: e.activation(out=stg[0:m, qs], in_=ps[0:m, :], func=AF.Identity),
                                  reads=[psb], writes=[stgb])
                        else:
                            xb_, xbb = xbr.next()
                            mk.op("scalar", lambda e: e.activation(out=xb_[0:m, :], in_=ps[0:m, :], func=AF.Identity),
                                  reads=[psb], writes=[xbb])
                            pr, prb = psrot.next()
                            mk.op("tensor", lambda e: e.matmul(pr[0:m, :], lhsT=rotm[0:m, 0:m], rhs=xb_[0:m, :], start=True, stop=True),
                                  reads=[rotb, xbb], writes=[prb], skip_same=True)
                            t1, t1b = t1r.next()
                            t2, t2b = t2r.next()
                            mk.op("gpsimd", lambda e: e.tensor_tensor(out=t1[0:m, :], in0=xb_[0:m, :], in1=cosT[0:m, qs], op=ALU.mult),
                                  reads=[xbb, cosb], writes=[t1b])
                            mk.op("vector", lambda e: e.tensor_tensor(out=t2[0:m, :], in0=pr[0:m, :], in1=sinT[0:m, qs], op=ALU.mult),
                                  reads=[prb, sinb], writes=[t2b])
                            mk.op("gpsimd", lambda e: e.tensor_tensor(out=stg[0:m, qs], in0=t1[0:m, :], in1=t2[0:m, :], op=ALU.add),
                                  reads=[t1b, t2b], writes=[stgb])
                    mk.dma("sync", PJT[r0:r0 + m, :], stg[0:m, :], reads=[stgb], writes=[PJb], semb=stgb)
                wv, wvb = sb(st, "wv", [128, 8, 1024], BF16)
                wchunk(win, C_FV, 384, wv[:, :, 0:384], wvb)
                wchunk(win, C_DV, 320, wv[:, :, 384:704], wvb)
                wchunk(win, C_SV, 320, wv[:, :, 704:1024], wvb)
                wf, wfb = sb(st, "wf", [128, 8, 16], BF16)
                mk.op("vector", lambda e: e.memset(wf[:], 0.0), writes=[wfb])
                wchunk(win, C_FF, 6, wf[:, :, 0:6], wfb)
                wchunk(win, C_IW, 8, wf[:, :, 8:16], wfb)
                vst = Rot([sb(st, "vst%d" % i, [128, 16, 65], BF16) for i in range(2)])
                fw, fwb = sb(st, "fw", [128, NT, 16], F32)
                for t in range(NT):
                    ts_ = slice(t * 128, (t + 1) * 128)
                    v, vb = vst.next()
                    mk.op("gpsimd", lambda e: e.memset(v[:], 1.0), writes=[vb])
                    for half in range(2):
                        ps, psb = psrot.next()
                        for kc in range(8):
                            mk.op("tensor", lambda e: e.matmul(ps[:, :], lhsT=hT[:, kc, ts_], rhs=wv[:, kc, half * 512:(half + 1) * 512],
                                                               start=(kc == 0), stop=(kc == 7)),
                                  reads=[hTb, wvb], writes=[psb], skip_same=True)
                        mk.op("scalar", lambda e: e.activation(out=v[:, half * 8:(half + 1) * 8, 0:64],
                                                               in_=ps[:, :].rearrange("p (h d) -> p h d", h=8), func=AF.Identity),
                              reads=[psb], writes=[vb])
                    mk.dma("sync", VT[ts_, :], v[:].rearrange("p h d -> p (h d)"), reads=[vb], writes=[VTb], semb=vb)
                    ps, psb = psrot.next()
                    for kc in range(8):
                        mk.op("tensor", lambda e: e.matmul(ps[:, 0:16], lhsT=hT[:, kc, ts_], rhs=wf[:, kc, :],
                                                           start=(kc == 0), stop=(kc == 7)),
                              reads=[hTb, wfb], writes=[psb], skip_same=True)
                    mk.op("vector", lambda e: e.tensor_copy(out=fw[:, t, :], in_=ps[:, 0:16]), reads=[psb], writes=[fwb])
                with mk.small():
                    bt, btb = sb(st, "bt", [128, 6], F32)
                    mk.dma("sync", bt[:], W["b_fgate"][l:l + 1, :].to_broadcast([128, 6]), writes=[btb])
                    lf, lfb = sb(st, "lf", [128, NT, 6], F32)
                    for h in range(6):
                        mk.op("vector", lambda e: e.tensor_scalar(out=lf[:, :, h], in0=fw[:, :, h], scalar1=bt[:, h:h + 1], scalar2=None,
                                                                  op0=ALU.add), reads=[fwb, btb], writes=[lfb])
                    mk.op("scalar", lambda e: e.activation(out=lf[:], in_=lf[:], func=AF.Exp, scale=-1.0), reads=[lfb], writes=[lfb])
                    mk.op("scalar", lambda e: e.activation(out=lf[:], in_=lf[:], func=AF.Ln, bias=1.0), reads=[lfb], writes=[lfb])
                    p1, p1b = psrot.next()
                    p2, p2b = psrot.next()
                    lf2 = lf[:].rearrange("p t h -> p (t h)")
                    mk.op("tensor", lambda e: e.matmul(p1[:, 0:192], lhsT=trile[:], rhs=lf2, start=True, stop=True),
                          reads=[trileb, lfb], writes=[p1b], skip_same=True)
                    mk.op("tensor", lambda e: e.matmul(p2[:, 0:192], lhsT=onesf[:], rhs=lf2, start=True, stop=True),
                          reads=[onesfb, lfb], writes=[p2b], skip_same=True)
                    tot, totb = sb(st, "tot", [128, NT, 6], F32)
                    mk.op("vector", lambda e: e.tensor_copy(out=tot[:].rearrange("p t h -> p (t h)"), in_=p2[:, 0:192]),
                          reads=[p2b], writes=[totb])
                    mk.op("vector", lambda e: e.tensor_copy(out=Ee[:, 0, :], in_=tot[:, 0, :]), reads=[totb], writes=[Eeb])
                    for t in range(1, NT):
                        mk.op("vector", lambda e: e.tensor_tensor(out=Ee[:, t, :], in0=Ee[:, t - 1, :], in1=tot[:, t, :], op=ALU.add),
                              reads=[Eeb, totb], writes=[Eeb])
                    mk.op("vector", lambda e: e.tensor_tensor(out=Cc[:].rearrange("p t h -> p (t h)"), in0=p1[:, 0:192],
                                                              in1=Ee[:].rearrange("p t h -> p (t h)"), op=ALU.add),
                          reads=[p1b, Eeb], writes=[Ccb])
                    mk.op("vector", lambda e: e.tensor_tensor(out=Cc[:], in0=Cc[:], in1=tot[:], op=ALU.subtract),
                          reads=[Ccb, totb], writes=[Ccb])
                    mk.op("vector", lambda e: e.tensor_scalar(out=widx[:], in0=fw[:, :, 8:16], scalar1=(8.0 ** -0.5) * (64.0 ** -0.5),
                                                              scalar2=None, op0=ALU.mult), reads=[fwb], writes=[widxb])
                mk.barrier(recycle=True)

        def load_kv(st, r_k, v_off, nh):
            kT, kTb = sb(st, "kT", [128, 3, S], BF16)
            nfull = (nh * 64) // 128
            if nfull:
                mk.dma("sync", kT[:, 0:nfull, :], PJT[r_k:r_k + nfull * 128, :].rearrange("(c p) t -> p c t", p=128),
                       reads=[PJb], writes=[kTb])
            if nh % 2:
                mk.dma("sync", kT[0:64, nfull, :], PJT[r_k + nfull * 128:r_k + nfull * 128 + 64, :], reads=[PJb], writes=[kTb])
            vt, vtb = sb(st, "vt", [128, NT, nh, 65], BF16)
            for g in range(4):
                mk.dma("sync", vt[:, g * 8:(g + 1) * 8].rearrange("p t h d -> p t (h d)"),
                       VT[g * 1024:(g + 1) * 1024, v_off:v_off + nh * 65].rearrange("(t p) c -> p t c", p=128),
                       reads=[VTb], writes=[vtb])
            return kT, kTb, vt, vtb

        def load_q(st, name, r_q, nh, Q=None):
            n = S if Q is None else 512
            cs = slice(0, S) if Q is None else slice(Q * 512, (Q + 1) * 512)
            return n, cs

        def attn_softmax_head(st_tiles, qT, qTb, qoff, kT, kTb, vt, vtb, h, Q, bias_fn, mask_fn, ostg, ostgb):
            ptr, rcs, nbs = st_tiles
            ck, pb = h // 2, (h % 2) * 64
            nkb = 4 * Q + 4
            pO, pOb = PS[4 + (Q % 2)]
            pss = [None] * nkb

            def issue_s(kb):
                ps, psb = psrot.next()
                mk.op("tensor", lambda e: e.matmul(ps[:, :], lhsT=kT[pb:pb + 64, ck, kb * 128:(kb + 1) * 128],
                                                   rhs=qT[pb:pb + 64, ck, qoff:qoff + 512], start=True, stop=True),
                      reads=[kTb, qTb], writes=[psb], skip_same=True)
                pss[kb] = (ps, psb)
            issue_s(0)
            for kb in range(nkb):
                if kb + 1 < nkb:
                    issue_s(kb + 1)
                ps, psb = pss[kb]
                pt, ptb = ptr.next()
                bias_ap, bias_bufs = bias_fn(kb)
                if bias_ap is None:
                    mk.op("scalar", lambda e: e.activation(out=pt[:], in_=ps[:, :], func=AF.Exp, scale=0.125), reads=[psb], writes=[ptb])
                else:
                    mk.op("scalar", lambda e: e.activation(out=pt[:], in_=ps[:, :], func=AF.Exp, scale=0.125, bias=bias_ap),
                          reads=[psb] + bias_bufs, writes=[ptb])
                m = mask_fn(kb)
                if m is not None:
                    m_ap, m_bufs = m
                    mk.op("gpsimd", lambda e: e.tensor_tensor(out=pt[:], in0=pt[:], in1=m_ap, op=ALU.mult),
                          reads=[ptb] + m_bufs, writes=[ptb])
                mk.op("tensor", lambda e: e.matmul(pO[0:65, :], lhsT=vt[:, kb, h, :], rhs=pt[:], start=(kb == 0), stop=(kb == nkb - 1)),
                      reads=[vtb, ptb], writes=[pOb], skip_same=True)
            rc, rcb = rcs.next()
            nb, nbb = nbs.next()
            mk.op("vector", lambda e: e.reciprocal(out=rc[64:65, :], in_=pO[64:65, :]), reads=[pOb], writes=[rcb])
            pB, pBb = PS[6]
            mk.op("tensor", lambda e: e.matmul(pB[0:64, :], lhsT=onesf[64:65, 0:64], rhs=rc[64:65, :], start=True, stop=True),
                  reads=[onesfb, rcb], writes=[pBb], skip_same=True)
            mk.op("scalar", lambda e: e.activation(out=nb[0:64, :], in_=pB[0:64, :], func=AF.Identity), reads=[pBb], writes=[nbb])
            mk.op("vector", lambda e: e.tensor_tensor(out=ostg[0:64, Q * 512:(Q + 1) * 512], in0=pO[0:64, :], in1=nb[0:64, :], op=ALU.mult),
                  reads=[pOb, nbb], writes=[ostgb])

        def attn_tiles(st):
            ptr = Rot([sb(st, "pt%d" % i, [128, 512], BF16) for i in range(4)])
            rcs = Rot([sb(st, "rc%d" % i, [128, 512], F32) for i in range(2)])
            nbs = Rot([sb(st, "nb%d" % i, [64, 512], F32) for i in range(2)])
            return ptr, rcs, nbs

        def fox(l):
            with contextlib.ExitStack() as st:
                kT, kTb, vt, vtb = load_kv(st, R_FK, V_F, 6)
                qT, qTb = sb(st, "qT", [128, 3, S], BF16)
                mk.dma("sync", qT[:], PJT[R_FQ:R_FQ + 384, :].rearrange("(c p) t -> p c t", p=128), reads=[PJb], writes=[qTb])
                tiles = attn_tiles(st)
                osr = Rot([sb(st, "ostg%d" % i, [64, S], BF16) for i in range(2)])
                bqr = Rot([sb(st, "bq%d" % i, [128, NT], F32) for i in range(2)])
                for h in range(6):
                    ostg, ostgb = osr.next()
                    for Q in range(NQ):
                        nkb = 4 * Q + 4
                        bq, bqb = bqr.next()
                        mk.op("vector", lambda e: e.tensor_scalar(out=bq[:, 0:nkb], in0=Cc[:, 0:nkb, h], scalar1=Ee[:, 4 * Q + 3, h:h + 1],
                                                                  scalar2=None, op0=ALU.subtract), reads=[Ccb, Eeb], writes=[bqb])
                        attn_softmax_head(tiles, qT, qTb, Q * 512, kT, kTb, vt, vtb, h, Q,
                                          lambda kb: (bq[:, kb:kb + 1], [bqb]),
                                          lambda kb: ((cmask[:, (kb - 4 * Q) * 512:(kb - 4 * Q + 1) * 512], [cmaskb]) if kb >= 4 * Q else None),
                                          ostg, ostgb)
                    mk.dma("sync", OT[O_F + h * 64:O_F + (h + 1) * 64, :], ostg[:], reads=[ostgb], writes=[OTb], semb=ostgb)
                mk.barrier(recycle=True)

        def dsa(l):
            with contextlib.ExitStack() as st:
                kT, kTb, vt, vtb = load_kv(st, R_DK, V_D, 5)
                kix, kixb = sb(st, "kix", [128, S], BF16)
                mk.dma("sync", kix[0:64, :], PJT[R_IK:R_IK + 64, :], reads=[PJb], writes=[kixb])
                mk.dma("sync", kix[64:128, :], PJT[R_IK:R_IK + 64, :], reads=[PJb], writes=[kixb])
                tiles = attn_tiles(st)
                qr = Rot([sb(st, "dq%d" % i, [128, 3, 512], BF16) for i in range(2)])
                qir = Rot([sb(st, "dqi%d" % i, [128, 4, 512], BF16) for i in range(2)])
                sc, scb = sb(st, "sc", [128, S], F32)
                wk, wkb = sb(st, "wk", [128, S], F32)
                m8, m8b = sb(st, "m8", [128, 8], F32)
                bmid, bmidb = sb(st, "bmid", [128, 1], F32)
                bcnt, bcntb = sb(st, "bcnt", [128, 1], F32)
                bd, bdb = sb(st, "bd", [128, 1], F32)
                mqr = Rot([sb(st, "mq%d" % i, [128, S], BF16) for i in range(2)])
                mT, mTb = sb(st, "mT", [128, NT, 512], BF16)
                rr = Rot([sb(st, "rl%d" % i, [128, 512], F32) for i in range(3)])
                osr = Rot([sb(st, "ostg%d" % i, [64, 5, 512], BF16) for i in range(2)])
                for Q in range(NQ):
                    qs = slice(Q * 512, (Q + 1) * 512)
                    q, qb_ = qr.next()
                    mk.dma("sync", q[:, 0:2, :], PJT[R_DQ:R_DQ + 256, qs].rearrange("(c p) t -> p c t", p=128), reads=[PJb], writes=[qb_])
                    mk.dma("sync", q[0:64, 2, :], PJT[R_DQ + 256:R_DQ + 320, qs], reads=[PJb], writes=[qb_])
                    qi, qib = qir.next()
                    mk.dma("sync", qi[:], PJT[R_IQ:R_IQ + 512, qs].rearrange("(c p) t -> p c t", p=128), reads=[PJb], writes=[qib])
                    mk.op("gpsimd", lambda e: e.memset(mT[:, 4 * Q:4 * Q + 4, :], 0.0), writes=[mTb])
                    for qsub in range(4):
                        qb = 4 * Q + qsub
                        nk = (qb + 1) * 128
                        for ks in range((nk + 511) // 512):
                            w_ = min(512, nk - ks * 512)
                            for hi in range(8):
                                ps, psb = psrot.next()
                                pbi = (hi % 2) * 64
                                mk.op("tensor", lambda e: e.matmul(ps[:, 0:w_], lhsT=qi[pbi:pbi + 64, hi // 2, qsub * 128:(qsub + 1) * 128],
                                                                   rhs=kix[pbi:pbi + 64, ks * 512:ks * 512 + w_], start=True, stop=True),
                                      reads=[qib, kixb], writes=[psb], skip_same=True)
                                r, rb = rr.next()
                                mk.op("scalar", lambda e: e.activation(out=r[:, 0:w_], in_=ps[:, 0:w_], func=AF.Relu), reads=[psb], writes=[rb])
                                if hi == 0:
                                    mk.op("vector", lambda e: e.tensor_scalar(out=sc[:, ks * 512:ks * 512 + w_], in0=r[:, 0:w_],
                                                                              scalar1=widx[:, qb, 0:1], scalar2=None, op0=ALU.mult),
                                          reads=[rb, widxb], writes=[scb])
                                else:
                                    mk.op("vector", lambda e: e.scalar_tensor_tensor(out=sc[:, ks * 512:ks * 512 + w_], in0=r[:, 0:w_],
                                                                                     scalar=widx[:, qb, hi:hi + 1],
                                                                                     in1=sc[:, ks * 512:ks * 512 + w_],
                                                                                     op0=ALU.mult, op1=ALU.add),
                                          reads=[rb, widxb, scb], writes=[scb])
                        mk.op("gpsimd", lambda e: e.tensor_tensor(out=sc[:, qb * 128:nk], in0=sc[:, qb * 128:nk], in1=negm[:], op=ALU.add),
                              reads=[scb, negmb], writes=[scb])
                        with mk.small():
                            mq, mqb = mqr.next()
                            if qb >= 2:
                                mk.op("vector", lambda e: e.max(out=m8[:], in_=sc[:, 0:nk]), reads=[scb], writes=[m8b])
                                w = TOPK_W / 2
                                mk.op("vector", lambda e: e.tensor_scalar(out=bmid[:], in0=m8[:, 0:1], scalar1=-w, scalar2=None, op0=ALU.add),
                                      reads=[m8b], writes=[bmidb])
                                for it in range(TOPK_IT):
                                    mk.op("vector", lambda e: e.tensor_scalar(out=mq[:, 0:nk], in0=sc[:, 0:nk], scalar1=bmid[:, 0:1], scalar2=0.0,
                                                                              op0=ALU.is_ge, op1=ALU.add, accum_out=bcnt[:, 0:1]),
                                          reads=[scb, bmidb], writes=[mqb, bcntb])
                                    last = (it == TOPK_IT - 1)
                                    wn = w if last else w / 2
                                    mk.op("vector", lambda e: e.tensor_scalar(out=bd[:], in0=bcnt[:], scalar1=255.5, scalar2=(wn if last else 2 * wn),
                                                                              op0=ALU.is_ge, op1=ALU.mult), reads=[bcntb], writes=[bdb])
                                    mk.op("vector", lambda e: e.scalar_tensor_tensor(out=bmid[:], in0=bd[:], scalar=-wn, in1=bmid[:],
                                                                                     op0=ALU.add, op1=ALU.add), reads=[bdb, bmidb], writes=[bmidb])
                                    w = wn
                                mk.op("vector", lambda e: e.tensor_scalar(out=mq[:, 0:nk], in0=sc[:, 0:nk], scalar1=bmid[:, 0:1], scalar2=None,
                                                                          op0=ALU.is_ge), reads=[scb, bmidb], writes=[mqb])
                            else:
                                mk.op("vector", lambda e: e.tensor_scalar(out=mq[:, 0:nk], in0=sc[:, 0:nk], scalar1=-1.0e29, scalar2=None,
                                                                          op0=ALU.is_ge), reads=[scb], writes=[mqb])
                        for g0 in range(0, qb + 1, 8):
                            g1 = min(qb + 1, g0 + 8)
                            for kb in range(g0, g1):
                                mk.op("tensor", lambda e: e.transpose(psT[:, (kb - g0) * 128:(kb - g0 + 1) * 128], mq[:, kb * 128:(kb + 1) * 128], ident[:]),
                                      reads=[mqb, identb], writes=[psTb], skip_same=True)
                            mk.op("scalar", lambda e: e.activation(out=mT[:, g0:g1, qsub * 128:(qsub + 1) * 128],
                                                                   in_=psT[:, 0:(g1 - g0) * 128].rearrange("p (k t) -> p k t", k=g1 - g0),
                                                                   func=AF.Identity), reads=[psTb], writes=[mTb])
                    ostg, ostgb = osr.next()
                    for h in range(5):
                        attn_softmax_head_dsa(tiles, q, qb_, kT, kTb, vt, vtb, h, Q, mT, mTb, ostg, ostgb)
                    mk.dma("sync", OT[O_D:O_D + 320, qs].rearrange("(h p) t -> p h t", p=64), ostg[:], reads=[ostgb], writes=[OTb], semb=ostgb)
                mk.barrier(recycle=True)

        def attn_softmax_head_dsa(tiles, q, qb_, kT, kTb, vt, vtb, h, Q, mT, mTb, ostg, ostgb):
            class V:
                def __getitem__(self, idx):
                    return ostg[idx[0], h, :]
            attn_softmax_head(tiles, q, qb_, 0, kT, kTb, vt, vtb, h, Q,
                              lambda kb: (None, []),
                              lambda kb: (mT[:, kb, :], [mTb]),
                              V(), ostgb)

        def sbk(l):
            prev = mk.SAME_ENGINE_SYNC
            mk.SAME_ENGINE_SYNC = False
            try:
                _sbk(l)
            finally:
                mk.SAME_ENGINE_SYNC = prev

        def _sbk(l):
            with contextlib.ExitStack() as st:
                kT, kTb, vt, vtb = load_kv(st, R_SK, V_S, 5)
                qT, qTb = sb(st, "qT", [128, 3, S], BF16)
                mk.dma("sync", qT[:, 0:2, :], PJT[R_SQ:R_SQ + 256, :].rearrange("(c p) t -> p c t", p=128), reads=[PJb], writes=[qTb])
                mk.dma("sync", qT[0:64, 2, :], PJT[R_SQ + 256:R_SQ + 320, :], reads=[PJb], writes=[qTb])
                etr = Rot([sb(st, "et%d" % i, [128, 512], F32) for i in range(2)])
                ltr = Rot([sb(st, "lt%d" % i, [128, 512], BF16) for i in range(3)])
                t1r = Rot([sb(st, "st1%d" % i, [128, 512], F32) for i in range(3)])
                t2r = Rot([sb(st, "st2%d" % i, [128, 512], F32) for i in range(2)])
                atr = Rot([sb(st, "at%d" % i, [128, 512], BF16) for i in range(3)])
                rsr = Rot([sb(st, "rs%d" % i, [128, 512], F32) for i in range(2)])
                osr = Rot([sb(st, "ostg%d" % i, [64, S], BF16) for i in range(2)])
                rot5 = Rot(PS[0:4] + [PS[6]])
                for h in range(SB_H):
                    ck, pb = h // 2, (h % 2) * 64
                    ostg, ostgb = osr.next()
                    for Q in range(SB_NQ):
                        qs = slice(Q * 512, (Q + 1) * 512)
                        nkb = 4 * Q + 4
                        rs, rsb = rsr.next()
                        mk.op("gpsimd", lambda e: e.memset(rs[:], 0.0), writes=[rsb])
                        pO, pOb = PS[4 + (Q % 2)]
                        stA = {}

                        def stage_a(kb):
                            d = kb - 4 * Q
                            pz, pzb = rot5.next()
                            mk.op("tensor", lambda e: e.matmul(pz[:, :], lhsT=kT[pb:pb + 64, ck, kb * 128:(kb + 1) * 128],
                                                               rhs=qT[pb:pb + 64, ck, qs], start=True, stop=True),
                                  reads=[kTb, qTb], writes=[pzb], skip_same=True)
                            et, etb = etr.next()
                            lt, ltb = ltr.next()
                            mk.op("scalar", lambda e: e.activation(out=et[:], in_=pz[:, :], func=AF.Exp, scale=0.125), reads=[pzb], writes=[etb])
                            mk.op("scalar", lambda e: e.activation(out=lt[:], in_=et[:], func=AF.Ln, bias=1.0), reads=[etb], writes=[ltb])
                            if d >= 0:
                                mk.op("gpsimd", lambda e: e.tensor_tensor(out=lt[:], in0=lt[:], in1=smask[:, d * 512:(d + 1) * 512], op=ALU.mult),
                                      reads=[ltb, smaskb], writes=[ltb])
                            pc, pcb = rot5.next()
                            mk.op("tensor", lambda e: e.matmul(pc[:, :], lhsT=trige[:], rhs=lt[:], start=True, stop=True),
                                  reads=[trigeb, ltb], writes=[pcb], skip_same=True)
                            prt = None
                            if kb > 0:
                                pr, prb = rot5.next()
                                mk.op("tensor", lambda e: e.matmul(pr[:, :], lhsT=onesb[:], rhs=lt[:], start=True, stop=True),
                                      reads=[onesbb, ltb], writes=[prb], skip_same=True)
                                prt = (pr, prb)
                            t1, t1b = t1r.next()
                            mk.op("scalar", lambda e: e.activation(out=t1[:], in_=pz[:, :], func=AF.Identity, scale=0.125),
                                  reads=[pzb], writes=[t1b])
                            stA[kb] = (d, pc, pcb, prt, t1, t1b)

                        def stage_b(kb):
                            d, pc, pcb, prt, t1, t1b = stA.pop(kb)
                            mk.op("gpsimd", lambda e: e.tensor_tensor(out=t1[:], in0=t1[:], in1=rs[:], op=ALU.subtract),
                                  reads=[t1b, rsb], writes=[t1b])
                            t2, t2b = t2r.next()
                            mk.op("vector", lambda e: e.tensor_tensor(out=t2[:], in0=t1[:], in1=pc[:, :], op=ALU.subtract),
                                  reads=[t1b, pcb], writes=[t2b])
                            at, atb = atr.next()
                            mk.op("scalar", lambda e: e.activation(out=at[:], in_=t2[:], func=AF.Exp), reads=[t2b], writes=[atb])
                            if d >= 0:
                                mk.op("gpsimd", lambda e: e.tensor_tensor(out=at[:], in0=at[:], in1=smask[:, d * 512:(d + 1) * 512], op=ALU.mult),
                                      reads=[atb, smaskb], writes=[atb])
                            if prt is not None:
                                pr, prb = prt
                                mk.op("vector", lambda e: e.tensor_tensor(out=rs[:], in0=pr[:, :], in1=rs[:], op=ALU.add),
                                      reads=[rsb, prb], writes=[rsb])
                            mk.op("tensor", lambda e: e.matmul(pO[0:65, :], lhsT=vt[:, kb, h, 0:65], rhs=at[:],
                                                               start=(kb == nkb - 1), stop=(kb == 0)),
                                  reads=[vtb, atb], writes=[pOb], skip_same=True)

                        kbs = list(range(nkb - 1, -1, -1))
                        stage_a(kbs[0])
                        for i, kb in enumerate(kbs):
                            if i + 1 < len(kbs):
                                stage_a(kbs[i + 1])
                            stage_b(kb)
                        mk.op("scalar", lambda e: e.activation(out=ostg[0:64, qs], in_=pO[0:64, :], func=AF.Identity), reads=[pOb], writes=[ostgb])
                    mk.dma("sync", OT[O_S + h * 64:O_S + (h + 1) * 64, :], ostg[:], reads=[ostgb], writes=[OTb], semb=ostgb)
                mk.barrier(recycle=True)

        def merge(l):
            win = W["w_in"][l]
            with contextlib.ExitStack() as st:
                hT, hTb = sb(st, "hT2", [128, 8, S // 2], BF16)
                gw, gwb = sb(st, "gw", [128, 8, 3072], BF16)
                for b in range(3):
                    wchunk(win, C_G + b * 1024, 1024, gw[:, :, b * 1024:(b + 1) * 1024], gwb)
                wbo, wbob = sb(st, "wbo", [128, 9, D], BF16)
                mk.dma("gpsimd", wbo[:, 0:3, :], W["w_fox_out"][l].rearrange("(c p) n -> p c n", p=128), writes=[wbob])
                for b, nm in ((1, "w_dsa_out"), (2, "w_sb_out")):
                    mk.dma("gpsimd", wbo[:, 3 * b:3 * b + 2, :], W[nm][l][0:256, :].rearrange("(c p) n -> p c n", p=128), writes=[wbob])
                    mk.dma("gpsimd", wbo[0:64, 3 * b + 2, :], W[nm][l][256:320, :], writes=[wbob])
                wo, wob = sb(st, "wo", [128, 8, D], BF16)
                wchunk(W["w_out"][l], 0, D, wo[:], wob)
                oqr = Rot([sb(st, "oq%d" % i, [128, 9, 512], BF16) for i in range(1)])
                sgr = Rot([sb(st, "msg%d" % i, [128, 512], F32) for i in range(3)])
                tmr = Rot([sb(st, "mtm%d" % i, [128, 512], F32) for i in range(3)])
                mTr = Rot([sb(st, "mmT%d" % i, [128, 8, 512], BF16) for i in range(2)])
                xr = Rot([sb(st, "mx%d" % i, [128, D], F32) for i in range(2)])
                KK = [128, 128, 128, 128, 128, 64, 128, 128, 64]
                for Q in range(NQ):
                    if Q % 4 == 0:
                        norm_pass(st, W["mix_norm"][l:l + 1, :], hT, hTb, t0=(Q // 4) * 16, nt=16)
                    qs = slice(Q * 512, (Q + 1) * 512)
                    ql = slice((Q % 4) * 512, (Q % 4 + 1) * 512)
                    oq, oqb = oqr.next()
                    mk.dma("sync", oq[:, 0:5, :], OT[0:640, qs].rearrange("(c p) t -> p c t", p=128), reads=[OTb], writes=[oqb])
                    mk.dma("sync", oq[0:64, 5, :], OT[640:704, qs], reads=[OTb], writes=[oqb])
                    mk.dma("sync", oq[:, 6:8, :], OT[768:1024, qs].rearrange("(c p) t -> p c t", p=128), reads=[OTb], writes=[oqb])
                    mk.dma("sync", oq[0:64, 8, :], OT[1024:1088, qs], reads=[OTb], writes=[oqb])
                    mT, mTb = mTr.next()
                    for fc in range(8):
                        tms = []
                        for b in range(3):
                            pg, pgb = psrot.next()
                            for kc in range(8):
                                mk.op("tensor", lambda e: e.matmul(pg[:, :], lhsT=gw[:, kc, b * 1024 + fc * 128:b * 1024 + (fc + 1) * 128],
                                                                   rhs=hT[:, kc, ql], start=(kc == 0), stop=(kc == 7)),
                                      reads=[gwb, hTb], writes=[pgb], skip_same=True)
                            sg, sgb = sgr.next()
                            mk.op("scalar", lambda e: e.activation(out=sg[:], in_=pg[:, :], func=AF.Sigmoid), reads=[pgb], writes=[sgb])
                            pp, ppb = psrot.next()
                            for c in range(3):
                                kk = KK[3 * b + c]
                                mk.op("tensor", lambda e: e.matmul(pp[:, :], lhsT=wbo[0:kk, 3 * b + c, fc * 128:(fc + 1) * 128],
                                                                   rhs=oq[0:kk, 3 * b + c, :], start=(c == 0), stop=(c == 2)),
                                      reads=[wbob, oqb], writes=[ppb], skip_same=True)
                            tm, tmb = tmr.next()
                            mk.op("vector", lambda e: e.tensor_tensor(out=tm[:], in0=sg[:], in1=pp[:, :], op=ALU.mult),
                                  reads=[sgb, ppb], writes=[tmb])
                            tms.append((tm, tmb))
                        (a0, a0b), (a1, a1b), (a2, a2b) = tms
                        mk.op("gpsimd", lambda e: e.tensor_tensor(out=a0[:], in0=a0[:], in1=a1[:], op=ALU.add), reads=[a0b, a1b], writes=[a0b])
                        mk.op("gpsimd", lambda e: e.tensor_tensor(out=mT[:, fc, :], in0=a0[:], in1=a2[:], op=ALU.add),
                              reads=[a0b, a2b], writes=[mTb])
                    for sub in range(4):
                        t = Q * 4 + sub
                        xt, xtb = xr.next()
                        mk.dma("sync", xt[:], X[t * 128:(t + 1) * 128, :], reads=[Xb[t]], writes=[xtb])
                        for half in range(2):
                            ps, psb = psrot.next()
                            for kc in range(8):
                                mk.op("tensor", lambda e: e.matmul(ps[:, :], lhsT=mT[:, kc, sub * 128:(sub + 1) * 128],
                                                                   rhs=wo[:, kc, half * 512:(half + 1) * 512], start=(kc == 0), stop=(kc == 7)),
                                      reads=[mTb, wob], writes=[psb], skip_same=True)
                            mk.op("vector", lambda e: e.tensor_tensor(out=xt[:, half * 512:(half + 1) * 512], in0=ps[:, :],
                                                                      in1=xt[:, half * 512:(half + 1) * 512], op=ALU.add),
                                  reads=[psb, xtb], writes=[xtb])
                        mk.dma("sync", X[t * 128:(t + 1) * 128, :], xt[:], reads=[xtb], writes=[Xb[t]], semb=xtb)
                mk.barrier(recycle=True)

        def ca(l):
            with contextlib.ExitStack() as st:
                hT, hTb = sb(st, "hT", [128, 8, S], BF16)
                norm_pass(st, W["ca_norm"][l:l + 1, :], hT, hTb)
                load_gain(W["mem_norm"][l:l + 1, :])
                memT, memTb = sb(st, "memT", [128, 8, 256], BF16)
                mx, mxb = sb(st, "cmx", [128, D], F32)
                mh, mhb = sb(st, "cmh", [128, D], BF16)
                junk, junkb = sb(st, "cjunk", [128, D], BF16)
                for mb in range(2):
                    mk.dma("sync", mx[:], mem_in[mb * 128:(mb + 1) * 128, :], writes=[mxb])
                    rstd_of(mx, mxb, junk, junkb)
                    with mk.small():
                        mk.op("vector", lambda e: e.scalar_tensor_tensor(out=mh[:], in0=mx[:], scalar=small[:, 2:3], in1=gB[:],
                                                                         op0=ALU.mult, op1=ALU.mult), reads=[mxb, smallb, gBb], writes=[mhb])
                    for kc in range(8):
                        mk.op("tensor", lambda e: e.transpose(psT[:, kc * 128:(kc + 1) * 128], mh[:, kc * 128:(kc + 1) * 128], ident[:]),
                              reads=[mhb, identb], writes=[psTb], skip_same=True)
                    mk.op("scalar", lambda e: e.activation(out=memT[:, :, mb * 128:(mb + 1) * 128],
                                                           in_=psT[:, :].rearrange("p (k t) -> p k t", k=8), func=AF.Identity),
                          reads=[psTb], writes=[memTb])
                wkv, wkvb = sb(st, "wkv", [128, 8, D], BF16)
                wchunk(W["ca_w_kv"][l], 0, D, wkv[:], wkvb)
                wq, wqb = sb(st, "wq", [128, 8, 512], BF16)
                wchunk(W["ca_w_q"][l], 0, 512, wq[:], wqb)
                wo, wob = sb(st, "cwo", [128, 4, D], BF16)
                mk.dma("gpsimd", wo[:], W["ca_w_o"][l].rearrange("(c p) n -> p c n", p=128), writes=[wob])
                kTm, kTmb = sb(st, "kTm", [128, 4, 256], BF16)
                vm, vmb = sb(st, "vm", [128, 2, 512], BF16)
                for h in range(4):
                    ps, psb = psrot.next()
                    for kc in range(8):
                        mk.op("tensor", lambda e: e.matmul(ps[:, 0:256], lhsT=wkv[:, kc, h * 128:(h + 1) * 128], rhs=memT[:, kc, :],
                                                           start=(kc == 0), stop=(kc == 7)), reads=[wkvb, memTb], writes=[psb], skip_same=True)
                    mk.op("scalar", lambda e: e.activation(out=kTm[:, h, :], in_=ps[:, 0:256], func=AF.Identity), reads=[psb], writes=[kTmb])
                for mb in range(2):
                    ps, psb = psrot.next()
                    for kc in range(8):
                        mk.op("tensor", lambda e: e.matmul(ps[:, :], lhsT=memT[:, kc, mb * 128:(mb + 1) * 128], rhs=wkv[:, kc, 512:1024],
                                                           start=(kc == 0), stop=(kc == 7)), reads=[wkvb, memTb], writes=[psb], skip_same=True)
                    mk.op("scalar", lambda e: e.activation(out=vm[:, mb, :], in_=ps[:, :], func=AF.Identity), reads=[psb], writes=[vmb])
                qcr = Rot([sb(st, "qc%d" % i, [128, 512], BF16) for i in range(2)])
                ptr = Rot([sb(st, "cpt%d" % i, [128, 512], BF16) for i in range(3)])
                rdr = Rot([sb(st, "crd%d" % i, [128, 512], F32) for i in range(2)])
                ocr = Rot([sb(st, "oc%d" % i, [128, 4, 512], BF16) for i in range(2)])
                xr = Rot([sb(st, "cx%d" % i, [128, D], F32) for i in range(3)])
                sc_ = 128.0 ** -0.5
                for Q in range(NQ):
                    qs = slice(Q * 512, (Q + 1) * 512)
                    oc, ocb = ocr.next()
                    for h in range(4):
                        ps, psb = psrot.next()
                        for kc in range(8):
                            mk.op("tensor", lambda e: e.matmul(ps[:, :], lhsT=wq[:, kc, h * 128:(h + 1) * 128], rhs=hT[:, kc, qs],
                                                               start=(kc == 0), stop=(kc == 7)), reads=[wqb, hTb], writes=[psb], skip_same=True)
                        qc, qcb = qcr.next()
                        mk.op("scalar", lambda e: e.activation(out=qc[:], in_=ps[:, :], func=AF.Identity), reads=[psb], writes=[qcb])
                        pO, pOb = PS[4]
                        pD, pDb = PS[5]
                        for mb in range(2):
                            pS, pSb = psrot.next()
                            mk.op("tensor", lambda e: e.matmul(pS[:, :], lhsT=kTm[:, h, mb * 128:(mb + 1) * 128], rhs=qc[:], start=True, stop=True),
                                  reads=[kTmb, qcb], writes=[pSb], skip_same=True)
                            pt, ptb = ptr.next()
                            mk.op("scalar", lambda e: e.activation(out=pt[:], in_=pS[:, :], func=AF.Exp, scale=sc_), reads=[pSb], writes=[ptb])
                            mk.op("tensor", lambda e: e.matmul(pO[:, :], lhsT=vm[:, mb, h * 128:(h + 1) * 128], rhs=pt[:], start=(mb == 0), stop=(mb == 1)),
                                  reads=[vmb, ptb], writes=[pOb], skip_same=True)
                            mk.op("tensor", lambda e: e.matmul(pD[:, :], lhsT=onesb[:], rhs=pt[:], start=(mb == 0), stop=(mb == 1)),
                                  reads=[onesbb, ptb], writes=[pDb], skip_same=True)
                        rd, rdb = rdr.next()
                        mk.op("vector", lambda e: e.reciprocal(out=rd[:], in_=pD[:, :]), reads=[pDb], writes=[rdb])
                        mk.op("vector", lambda e: e.tensor_tensor(out=oc[:, h, :], in0=pO[:, :], in1=rd[:], op=ALU.mult),
                              reads=[pOb, rdb], writes=[ocb])
                    for sub in range(4):
                        t = Q * 4 + sub
                        xt, xtb = xr.next()
                        mk.dma("sync", xt[:], X[t * 128:(t + 1) * 128, :], reads=[Xb[t]], writes=[xtb])
                        for half in range(2):
                            ps, psb = psrot.next()
                            for h in range(4):
                                mk.op("tensor", lambda e: e.matmul(ps[:, :], lhsT=oc[:, h, sub * 128:(sub + 1) * 128],
                                                                   rhs=wo[:, h, half * 512:(half + 1) * 512], start=(h == 0), stop=(h == 3)),
                                      reads=[ocb, wob], writes=[psb], skip_same=True)
                            mk.op("vector", lambda e: e.tensor_tensor(out=xt[:, half * 512:(half + 1) * 512], in0=ps[:, :],
                                                                      in1=xt[:, half * 512:(half + 1) * 512], op=ALU.add),
                                  reads=[psb, xtb], writes=[xtb])
                        mk.dma("sync", X[t * 128:(t + 1) * 128, :], xt[:], reads=[xtb], writes=[Xb[t]], semb=xtb)
                mk.barrier(recycle=True)

        def final(raw):
            with contextlib.ExitStack() as st:
                xr = Rot([sb(st, "fx%d" % i, [128, D], F32) for i in range(3)])
                junk, junkb = sb(st, "fjunk", [128, D], BF16)
                if not raw:
                    load_gain(W["final_norm"][0:1, :])
                src = xstate["src"]
                outb = Buf("out")
                for t in range(NT):
                    xt, xtb = xr.next()
                    mk.dma("sync", xt[:], src[t * 128:(t + 1) * 128, :], reads=[Xb[t]], writes=[xtb])
                    if not raw:
                        rstd_of(xt, xtb, junk, junkb)
                        with mk.small():
                            mk.op("vector", lambda e: e.scalar_tensor_tensor(out=xt[:], in0=xt[:], scalar=small[:, 2:3], in1=gB[:],
                                                                             op0=ALU.mult, op1=ALU.mult),
                                  reads=[xtb, smallb, gBb], writes=[xtb])
                    mk.dma("sync", out_d[t * 128:(t + 1) * 128, :], xt[:], reads=[xtb], writes=[outb], semb=xtb)
                mk.barrier(recycle=True)

        for l in range(n_layers):
            if "ffn1" in stages:
                ffn(l, 1)
            if "proj" in stages:
                proj(l)
            if "fox" in stages:
                fox(l)
            if "dsa" in stages:
                dsa(l)
            if "sb" in stages:
                sbk(l)
            if "merge" in stages:
                merge(l)
            if "ca" in stages:
                ca(l)
            if "ffn2" in stages:
                ffn(l, 2)
        if not attn_probe:
            final(raw=debug)
        else:
            mk.barrier()
    return nc


def make_in_maps(inputs, n_layers=4):
    consts = host_consts()
    maps = []
    shared = {}
    for name, shp in WSPEC:
        shared[name] = np.ascontiguousarray(np.asarray(inputs[name], dtype=np.float32).reshape(shp)[:n_layers])
    for k, v in consts.items():
        shared["c_" + k] = np.ascontiguousarray(v.astype(np.float32))
    for c in range(8):
        b = c // 2
        m = dict(shared)
        m["x"] = np.ascontiguousarray(np.asarray(inputs["x"][b], dtype=np.float32))
        m["mem"] = np.ascontiguousarray(np.asarray(inputs["mem"][b], dtype=np.float32))
        m["pos"] = np.ascontiguousarray(np.asarray(inputs["positions"][b], dtype=np.int32).reshape(1, S))
        maps.append(m)
    return maps


def kernel(**inputs):
    nc = build()
    res = run_bass_kernel_spmd(nc, make_in_maps(inputs), core_ids=list(range(8)))
    out = np.stack([np.asarray(res.results[2 * b]["out"], dtype=np.float32) for b in range(4)], axis=0)
    return out
```
